# Optimizing a Trainium2 kernel written in Bass

```python
import math
import jax, jax.numpy as jnp
from jax import lax
import numpy as np

D_MODEL = 1024
BATCH = 1
SEQ = 16384
DEPTH = 1

CHUNK = 64
Q_BLOCK = 128
EPS = 1e-6

DA_HEADS = 4
DA_HEAD_DIM = 64
DA_V_DIM = 2 * DA_HEAD_DIM
DA_WIDTH = DA_HEADS * DA_V_DIM
ALIBI_MAX = 8.0

RET_HEADS = 4
RET_QK_DIM = 64
RET_V_DIM = 128
RET_WIDTH = RET_HEADS * RET_V_DIM

MIX_WIDTH = DA_WIDTH + RET_WIDTH

DA_Q_COLS = DA_HEADS * 2 * DA_HEAD_DIM
DA_K_COLS = DA_HEADS * 2 * DA_HEAD_DIM
DA_V_COLS = DA_WIDTH
RET_Q_COLS = RET_HEADS * RET_QK_DIM
RET_K_COLS = RET_HEADS * RET_QK_DIM
RET_V_COLS = RET_WIDTH
RET_G_COLS = RET_WIDTH
IN_COLS = DA_Q_COLS + DA_K_COLS + DA_V_COLS + RET_Q_COLS + RET_K_COLS + RET_V_COLS + RET_G_COLS
IN_SPLITS = (DA_Q_COLS,
             DA_Q_COLS + DA_K_COLS,
             DA_Q_COLS + DA_K_COLS + DA_V_COLS,
             DA_Q_COLS + DA_K_COLS + DA_V_COLS + RET_Q_COLS,
             DA_Q_COLS + DA_K_COLS + DA_V_COLS + RET_Q_COLS + RET_K_COLS,
             DA_Q_COLS + DA_K_COLS + DA_V_COLS + RET_Q_COLS + RET_K_COLS + RET_V_COLS)

MOE_GROUPS = 4
MOE_EXPERTS_PER_GROUP = 8
MOE_EXPERTS = MOE_GROUPS * MOE_EXPERTS_PER_GROUP
MOE_TOP_K = 2
MOE_HIDDEN = 512
MOE_BLOCK = 128

kernel_name = "hybrid_diffattn_retention_hmoe"


def rmsnorm(x, g):
    xf = x.astype(jnp.float32)
    y = xf * lax.rsqrt(jnp.mean(xf * xf, axis=-1, keepdims=True) + EPS)
    return (y * g.astype(jnp.float32)).astype(x.dtype)


def head_rmsnorm(x):
    xf = x.astype(jnp.float32)
    return xf * lax.rsqrt(jnp.mean(xf * xf, axis=-1, keepdims=True) + EPS)


def alibi_slopes(n_heads):
    return jnp.exp2(-ALIBI_MAX * jnp.arange(1, n_heads + 1, dtype=jnp.float32) / n_heads)


def diff_attention(q, k, v, lam, subln_g, lambda_init):
    B, S = q.shape[0], q.shape[1]
    nb = S // Q_BLOCK
    scale = DA_HEAD_DIM ** -0.5
    slopes = alibi_slopes(DA_HEADS)
    k_pos = jnp.arange(S)
    k_chunk = k_pos // CHUNK
    qb = q.reshape(B, nb, Q_BLOCK, DA_HEADS, 2, DA_HEAD_DIM).transpose(1, 0, 2, 3, 4, 5)

    def one_block(args):
        i, q_blk = args
        q_pos = i * Q_BLOCK + jnp.arange(Q_BLOCK)
        s = jnp.einsum('bqhmd,bkhmd->bhmqk', q_blk, k,
                       preferred_element_type=jnp.float32) * scale
        dist = jnp.abs(q_pos[:, None] - k_pos[None, :]).astype(jnp.float32)
        bias = -slopes[:, None, None, None] * dist[None, None]
        allowed = k_chunk[None, :] <= (q_pos // CHUNK)[:, None]
        s = jnp.where(allowed, s + bias, -jnp.inf)
        p = jax.nn.softmax(s, axis=-1)
        a = p[:, :, 0] - lam * p[:, :, 1]
        return jnp.einsum('bhqk,bkhe->bqhe', a.astype(v.dtype), v)

    o = lax.map(one_block, (jnp.arange(nb), qb))
    o = o.transpose(1, 0, 2, 3, 4).reshape(B, S, DA_HEADS, DA_V_DIM)
    o = rmsnorm(o, subln_g) * (1.0 - lambda_init)
    return o.reshape(B, S, DA_WIDTH)


def retention(q, k, v, g):
    B, S = q.shape[0], q.shape[1]
    nc = S // CHUNK
    f32 = jnp.float32
    log_gamma = jnp.log1p(-jnp.exp2(-5.0 - jnp.arange(RET_HEADS, dtype=f32)))
    qc = q.astype(f32).reshape(B, nc, CHUNK, RET_HEADS, RET_QK_DIM)
    kc = (k.astype(f32) * RET_QK_DIM ** -0.5).reshape(B, nc, CHUNK, RET_HEADS, RET_QK_DIM)
    vc = v.astype(f32).reshape(B, nc, CHUNK, RET_HEADS, RET_V_DIM)
    pos = jnp.arange(CHUNK, dtype=f32)
    rel = pos[:, None] - pos[None, :]
    intra_decay = jnp.where(rel >= 0, jnp.exp(log_gamma[:, None, None] * jnp.maximum(rel, 0.0)), 0.0)
    s = jnp.einsum('bnchd,bnshd->bnhcs', qc, kc) * intra_decay
    intra = jnp.einsum('bnhcs,bnshe->bnche', s, vc)
    k_decay = jnp.exp(log_gamma[:, None] * (CHUNK - 1 - pos)[None, :])
    kv = jnp.einsum('bnshd,hs,bnshe->bnhde', kc, k_decay, vc)
    chunk_decay = jnp.exp(log_gamma * CHUNK)[:, None, None]

    def step(state, kv_n):
        return state * chunk_decay + kv_n, state

    init = jnp.zeros((B, RET_HEADS, RET_QK_DIM, RET_V_DIM), f32)
    _, prev = lax.scan(step, init, kv.transpose(1, 0, 2, 3, 4))
    prev = prev.transpose(1, 0, 2, 3, 4)
    q_decay = jnp.exp(log_gamma[:, None] * (pos + 1.0)[None, :])
    cross = jnp.einsum('bnchd,hc,bnhde->bnche', qc, q_decay, prev)
    o = head_rmsnorm((intra + cross).reshape(B, S, RET_HEADS, RET_V_DIM))
    o = jax.nn.silu(g.astype(f32)) * o
    return o.reshape(B, S, RET_WIDTH).astype(q.dtype)


def hier_moe(xn, wg, bg, we, be, w_gate, w_up, w_down):
    B, S, D = xn.shape
    T = B * S
    f32 = jnp.float32
    xt = xn.reshape(T, D)
    g_prob = jax.nn.softmax((xt @ wg + bg).astype(f32), axis=-1)
    g_top_p, g_top_i = lax.top_k(g_prob, 1)
    grp = g_top_i[:, 0]
    e_logits = (xt @ we + be).astype(f32).reshape(T, MOE_GROUPS, MOE_EXPERTS_PER_GROUP)
    idx = jnp.broadcast_to(grp[:, None, None], (T, 1, MOE_EXPERTS_PER_GROUP))
    e_logits = jnp.take_along_axis(e_logits, idx, axis=1)[:, 0]
    top_val, top_idx = lax.top_k(e_logits, MOE_TOP_K)
    weights = g_top_p * jax.nn.softmax(top_val, axis=-1)
    expert_id = grp[:, None] * MOE_EXPERTS_PER_GROUP + top_idx

    N = T * MOE_TOP_K
    e_flat = expert_id.reshape(N).astype(jnp.int32)
    w_flat = weights.reshape(N)
    tok_flat = jnp.repeat(jnp.arange(T, dtype=jnp.int32), MOE_TOP_K)
    order = jnp.argsort(e_flat)
    sorted_e = e_flat[order]
    counts = jnp.zeros((MOE_EXPERTS,), jnp.int32).at[e_flat].add(1)
    start = jnp.cumsum(counts) - counts
    padded = (counts + MOE_BLOCK - 1) // MOE_BLOCK * MOE_BLOCK
    pad_end = jnp.cumsum(padded)
    pad_start = pad_end - padded
    dest = pad_start[sorted_e] + jnp.arange(N, dtype=jnp.int32) - start[sorted_e]
    n_blocks = (N + MOE_BLOCK - 1) // MOE_BLOCK + MOE_EXPERTS
    nbuf = n_blocks * MOE_BLOCK
    buf_tok = jnp.full((nbuf,), T, jnp.int32).at[dest].set(tok_flat[order])
    buf_w = jnp.zeros((nbuf,), f32).at[dest].set(w_flat[order])
    block_e = jnp.minimum(jnp.searchsorted(pad_end, jnp.arange(n_blocks, dtype=jnp.int32) * MOE_BLOCK,
                                           side='right'), MOE_EXPERTS - 1)
    x_pad = jnp.concatenate([xt, jnp.zeros((1, D), xt.dtype)], axis=0)
    xb = x_pad[buf_tok].reshape(n_blocks, MOE_BLOCK, D)

    def expert_block(args):
        xblk, e = args
        h = jax.nn.silu(xblk @ w_gate[e]) * (xblk @ w_up[e])
        return h @ w_down[e]

    yb = lax.map(expert_block, (xb, block_e)).reshape(nbuf, D)
    y = jnp.zeros((T + 1, D), f32).at[buf_tok].add(yb.astype(f32) * buf_w[:, None])[:T]
    return y.reshape(B, S, D).astype(xn.dtype)


def setup_inputs(seed: int = 0) -> dict:
    key = jax.random.key(seed)
    ks = jax.random.split(key, 20)
    f32 = jnp.float32
    L, D = DEPTH, D_MODEL

    def nrm(k, shape, scale):
        return jax.random.normal(k, shape, f32) * scale

    return {
        "x": nrm(ks[0], (BATCH, SEQ, D), 1.0),
        "attn_norm_g": 1.0 + nrm(ks[1], (L, D), 0.02),
        "w_in": nrm(ks[2], (L, D, IN_COLS), D ** -0.5),
        "da_lambda_q1": nrm(ks[3], (L, DA_HEAD_DIM), 0.1),
        "da_lambda_k1": nrm(ks[4], (L, DA_HEAD_DIM), 0.1),
        "da_lambda_q2": nrm(ks[5], (L, DA_HEAD_DIM), 0.1),
        "da_lambda_k2": nrm(ks[6], (L, DA_HEAD_DIM), 0.1),
        "da_subln_g": 1.0 + nrm(ks[7], (L, DA_V_DIM), 0.02),
        "w_out": nrm(ks[8], (L, MIX_WIDTH, D), MIX_WIDTH ** -0.5),
        "ffn_norm_g": 1.0 + nrm(ks[9], (L, D), 0.02),
        "router_group_w": nrm(ks[10], (L, D, MOE_GROUPS), D ** -0.5),
        "router_group_b": nrm(ks[11], (L, MOE_GROUPS), 0.01),
        "router_expert_w": nrm(ks[12], (L, D, MOE_EXPERTS), D ** -0.5),
        "router_expert_b": nrm(ks[13], (L, MOE_EXPERTS), 0.01),
        "expert_w_gate": nrm(ks[14], (L, MOE_EXPERTS, D, MOE_HIDDEN), D ** -0.5),
        "expert_w_up": nrm(ks[15], (L, MOE_EXPERTS, D, MOE_HIDDEN), D ** -0.5),
        "expert_w_down": nrm(ks[16], (L, MOE_EXPERTS, MOE_HIDDEN, D), MOE_HIDDEN ** -0.5),
        "final_norm_g": 1.0 + nrm(ks[17], (D,), 0.02),
    }


def reference(x, attn_norm_g, w_in, da_lambda_q1, da_lambda_k1, da_lambda_q2, da_lambda_k2,
              da_subln_g, w_out, ffn_norm_g, router_group_w, router_group_b, router_expert_w,
              router_expert_b, expert_w_gate, expert_w_up, expert_w_down, final_norm_g):
    B, S, _ = x.shape
    h = x
    for l in range(DEPTH):
        xn = rmsnorm(h, attn_norm_g[l])
        proj = xn @ w_in[l]
        q_da, k_da, v_da, q_r, k_r, v_r, g_r = jnp.split(proj, IN_SPLITS, axis=-1)
        lambda_init = 0.8 - 0.6 * math.exp(-0.3 * l)
        lam = (jnp.exp(jnp.sum(da_lambda_q1[l].astype(jnp.float32) * da_lambda_k1[l].astype(jnp.float32)))
               - jnp.exp(jnp.sum(da_lambda_q2[l].astype(jnp.float32) * da_lambda_k2[l].astype(jnp.float32)))
               + lambda_init)
        o_da = diff_attention(q_da.reshape(B, S, DA_HEADS, 2, DA_HEAD_DIM),
                              k_da.reshape(B, S, DA_HEADS, 2, DA_HEAD_DIM),
                              v_da.reshape(B, S, DA_HEADS, DA_V_DIM),
                              lam, da_subln_g[l], lambda_init)
        o_r = retention(q_r.reshape(B, S, RET_HEADS, RET_QK_DIM),
                        k_r.reshape(B, S, RET_HEADS, RET_QK_DIM),
                        v_r.reshape(B, S, RET_HEADS, RET_V_DIM),
                        g_r.reshape(B, S, RET_HEADS, RET_V_DIM))
        mix = jnp.concatenate([o_da.astype(h.dtype), o_r.astype(h.dtype)], axis=-1)
        h = h + mix @ w_out[l]
        xn = rmsnorm(h, ffn_norm_g[l])
        h = h + hier_moe(xn, router_group_w[l], router_group_b[l], router_expert_w[l],
                         router_expert_b[l], expert_w_gate[l], expert_w_up[l], expert_w_down[l])
    return rmsnorm(h, final_norm_g)
```

```python
import math
from contextlib import ExitStack

import numpy as np
import concourse.bass as bass
import concourse.mybir as mybir
from concourse.bass_utils import run_bass_kernel_spmd

F32 = mybir.dt.float32
BF16 = mybir.dt.bfloat16
I32 = mybir.dt.int32
U32 = mybir.dt.uint32
ALU = mybir.AluOpType
AF = mybir.ActivationFunctionType
AX = mybir.AxisListType

NCORES = 8
D = 1024
H = 4
EPS = 1e-6
NEXP = 32
HID = 512
LAMBDA_INIT = 0.8 - 0.6 * math.exp(-0.3 * 0)
ALIBI_CUT = 134.0


class Prog:
    ENG = ("pe", "act", "dve", "pool", "sp")

    def __init__(self, nc, stack):
        self.nc = nc
        self.stack = stack
        self.sem = {e: stack.enter_context(nc.semaphore("sem_" + e)) for e in self.ENG}
        self.cnt = {e: 0 for e in self.ENG}
        self.stream = {e: [] for e in self.ENG}
        self.seen = {e: {} for e in self.ENG}
        self.dsem = {}
        self.dcnt = {}
        self.lastw = {}
        self.readers = {}
        self.bc_value = 0
        self.bcreg = None

    def _dma_sem(self, name):
        if name not in self.dsem:
            self.dsem[name] = self.stack.enter_context(self.nc.semaphore("dma_" + name))
            self.dcnt[name] = 0
        return self.dsem[name]

    def _need(self, eng, tok):
        if tok is None:
            return
        kind, name, val = tok
        if kind == "eng" and name == eng and eng == "pe":
            return
        key = (kind, name)
        if self.seen[eng].get(key, 0) >= val:
            return
        self.seen[eng][key] = val
        sem = self.sem[name] if kind == "eng" else self.dsem[name]
        self.stream[eng].append(("wait", sem, val))

    def _deps(self, eng, reads, writes):
        toks = []
        for k in reads:
            toks.append(self.lastw.get(k))
        for k in writes:
            toks.append(self.lastw.get(k))
            toks.extend(self.readers.get(k, ()))
        best = {}
        for t in toks:
            if t is None:
                continue
            key = (t[0], t[1])
            if key not in best or best[key][2] < t[2]:
                best[key] = t
        for t in best.values():
            self._need(eng, t)

    def _commit(self, tok, reads, writes):
        for k in reads:
            self.readers.setdefault(k, []).append(tok)
        for k in writes:
            self.lastw[k] = tok
            self.readers[k] = []

    def op(self, eng, fn, reads=(), writes=()):
        self._deps(eng, reads, writes)
        self.cnt[eng] += 1
        tok = ("eng", eng, self.cnt[eng])
        self.stream[eng].append(("inst", fn, self.sem[eng], 1))
        self._commit(tok, reads, writes)

    def dma(self, eng, fn, reads=(), writes=(), sem=None):
        assert eng in ("sp", "pool", "act")
        name = sem
        s = self._dma_sem(name)
        self._deps(eng, reads, writes)
        self.dcnt[name] += 16
        tok = ("dma", name, self.dcnt[name])
        self.stream[eng].append(("inst", fn, s, 16))
        self._commit(tok, reads, writes)

    def raw(self, eng, fn, reads=(), writes=()):
        self._deps(eng, reads, writes)
        self.stream[eng].append(("raw", fn))

    def barrier(self):
        toks = [("eng", e, self.cnt[e]) for e in self.ENG if self.cnt[e] > 0]
        toks += [("dma", n, c) for n, c in self.dcnt.items() if c > 0]
        for e in self.ENG:
            for t in toks:
                self._need(e, t)

    def finish_waits(self, eng="sp"):
        best = {}
        for k, tok in list(self.lastw.items()):
            if k[0] == "OUT":
                key = (tok[0], tok[1])
                if key not in best or best[key][2] < tok[2]:
                    best[key] = tok
        for tok in best.values():
            self._need(eng, tok)

    def emit(self, block):
        nc = self.nc
        engobj = {"pe": "tensor", "act": "scalar", "dve": "vector", "pool": "gpsimd", "sp": "sync"}

        def mk(ename):
            def body(e):
                if ename == "pool":
                    self.bcreg = e.to_reg(self.bc_value)
                    run(e)
                else:
                    run(e)

            def run(e):
                for it in self.stream[ename]:
                    if it[0] == "wait":
                        e.wait_ge(it[1], it[2])
                    elif it[0] == "raw":
                        it[1](e)
                    else:
                        it[1](e).then_inc(it[2], it[3])
            return body

        for ename in self.ENG:
            getattr(block, engobj[ename])(mk(ename))


def I(name, *args, **kw):
    return lambda e: getattr(e, name)(*args, **kw)


def host_tables(c, S):
    NO = S // 8
    slopes = np.exp2(-8.0 * np.arange(1, H + 1) / H)
    i = np.arange(NO)
    pos = 64 * (i // 8) + 8 * c + (i % 8)
    qrel = pos % 4096
    a = (qrel // 64) * 64
    b = qrel % 64
    qaug = np.zeros((2, H, NO), np.float32)
    for h in range(H):
        qaug[0, h] = -8.0 * slopes[h] * a
        qaug[1, h] = -8.0 * slopes[h] * b
    p = np.arange(128)
    biask = np.zeros((128, H, 128), np.float32)
    for h in range(H):
        for idx in range(128):
            biask[:, h, idx] = slopes[h] * (128 * (idx - 96) + p)
    fix = np.zeros((128, H, 16), np.float32)
    for col in range(16):
        u, v = col // 8, col % 8
        qt = 64 * u + 8 * c + v
        for h in range(H):
            for pp in range(128):
                if pp // 64 > u:
                    fix[pp, h, col] = -30000.0
                elif pp > qt:
                    fix[pp, h, col] = -2.0 * slopes[h] * (pp - qt) * 8.0
    lg = np.log1p(-np.exp2(-5.0 - np.arange(H, dtype=np.float64)))
    s = np.arange(64)
    v8 = 8 * c + np.arange(8)
    kdec = np.zeros((128, H, 64), np.float32)
    for h in range(H):
        kd = np.exp(lg[h] * (63 - s)) * 0.125
        kdec[:, h, :] = np.concatenate([kd, kd])[:, None]
    dt = np.zeros((128, 2, 2, 8), np.float32)
    qdec8 = np.zeros((128, 2, 8), np.float32)
    sdec = np.zeros((128, 2), np.float32)
    for h in range(H):
        rel = v8[None, :] - s[:, None]
        dd = np.where(rel >= 0, np.exp(lg[h] * np.maximum(rel, 0)), 0.0) * 0.125
        hh, grp = h % 2, h // 2
        dt[:, hh, grp, :] = np.concatenate([dd, dd], 0)
        qdec8[hh * 64:(hh + 1) * 64, grp, :] = np.exp(lg[h] * (v8 + 1.0))[None, :]
        sdec[hh * 64:(hh + 1) * 64, grp] = np.exp(lg[h] * 64)
    return dict(qaug=qaug, biask=biask, fix=fix, kdec=kdec.reshape(128, H * 64), dt=dt,
                qdec8=qdec8, sdec=sdec)


def build(S, stage="full"):
    NO = S // 8
    NT = NO // 128
    NB = S // 512
    NG = S // 4096
    NSB = NT + NEXP
    NBLK = 2 * NSB
    assert NG >= 1
    nc = bass.Bass("TRN2", target_bir_lowering=False)
    st = ExitStack()
    with st:
        def din(name, shape, dt=F32):
            return nc.dram_tensor(name, list(shape), dt, kind="ExternalInput")

        xT = din("xT", [D, S])
        xoT = din("xoT", [D, NO])
        xo = din("xo", [NO, D])
        w_in = din("w_in", [D, 3072])
        g1 = din("g1", [128, 8])
        qaug_d = din("qaug", [2, H, NO])
        biask_d = din("biask", [128, H, 128])
        fix_d = din("fix", [128, H, 16])
        kdec_d = din("kdec", [128, H * 64])
        dt_d = din("dt", [128, 2, 2, 8])
        qdec8_d = din("qdec8", [128, 2, 8])
        sdec_d = din("sdec", [128, 2])
        lamv = din("lamv", [1, 4, 64])
        w_out = din("w_out", [D, D])
        sg = din("sg", [128, 1])
        g2 = din("g2", [1, D])
        gf = din("gf", [1, D])
        wr = din("wr", [D, 36])
        br = din("br", [1, 36])
        wg = din("wg", [NEXP * 128, 8 * HID])
        wu = din("wu", [NEXP * 128, 8 * HID])
        wd = din("wd", [NEXP * 128, 4 * D])
        out = nc.dram_tensor("out", [NO, D], F32, kind="ExternalOutput")
        dbg = None
        if stage != "full":
            dbg = nc.dram_tensor("dbg", [NO, 2048], F32, kind="ExternalOutput")

        kT_s = nc.dram_tensor("kT_s", [H, 2, 64, S], BF16)
        v_s = nc.dram_tensor("v_s", [S, 512], BF16)
        h1_s = nc.dram_tensor("h1_s", [NO, D], F32)
        xn_s = nc.dram_tensor("xn_s", [NO, D], BF16)
        xs_s = nc.dram_tensor("xs_s", [NBLK * 128, D], BF16)
        ys_s = nc.dram_tensor("ys_s", [NBLK * 128, D], F32)

        P = Prog(nc, st)
        P.bc_value = NEXP * 128 - 1

        def sbt(stack, name, shape, dt=F32):
            return stack.enter_context(nc.sbuf_tensor(name, list(shape), dt))

        def sb(name, shape, dt=F32):
            return sbt(st, name, shape, dt)

        def finish():
            P.finish_waits("sp")
            with nc.Block() as block:
                P.emit(block)
            return nc

        ones_bf = sb("ones_bf", [128, 128], BF16)
        ones_f = sb("ones_f", [128, 128], F32)
        ident_bf = sb("ident_bf", [128, 128], BF16)
        ident_f = sb("ident_f", [128, 128], F32)
        iotf = sb("iotf", [128, 128])
        g1_sb = sb("g1_sb", [128, 8])
        biask = sb("biask_sb", [128, H, 128])
        fix = sb("fix_sb", [128, H, 16])
        kdec = sb("kdec_sb", [128, H * 64])
        dtt = sb("dt_sb", [128, 2, 2, 8])
        qdec8 = sb("qdec8_sb", [128, 2, 8])
        sdec = sb("sdec_sb", [128, 2])
        epsc = sb("epsc", [128, 1])
        gsl = sb("gsl", [128, NT, 512], BF16)
        ret = sb("ret", [128, NT, 512], BF16)
        att = sb("att", [128, NT, 8, 128], BF16)
        psum = [st.enter_context(nc.psum_tensor(f"ps{i}", [128, 512], F32)) for i in range(8)]
        PS = [("ps", i) for i in range(8)]

        P.op("pool", I("memset", ones_bf[:], 1.0), writes=["ones_bf"])
        P.op("pool", I("memset", ones_f[:], 1.0), writes=["ones_f"])
        P.op("pool", I("memset", epsc[:], EPS), writes=["epsc"])
        iot = sb("iot", [128, 128], I32)
        P.op("pool", I("iota", iot[:], pattern=[[1, 128]], base=0, channel_multiplier=-1), writes=["iot"])
        P.op("dve", I("tensor_copy", out=iotf[:], in_=iot[:]), reads=["iot"], writes=["iotf"])
        P.op("dve", I("tensor_single_scalar", out=ident_f[:], in_=iotf[:], scalar=0.0, op=ALU.is_equal),
             reads=["iotf"], writes=["ident_f"])
        P.op("dve", I("tensor_copy", out=ident_bf[:], in_=ident_f[:]), reads=["ident_f"], writes=["ident_bf"])
        for (dst, src, key) in ((g1_sb, g1, "g1"), (biask, biask_d, "biask"), (fix, fix_d, "fix"),
                                (kdec, kdec_d, "kdec"), (dtt, dt_d, "dt"), (qdec8, qdec8_d, "qdec8"),
                                (sdec, sdec_d, "sdec")):
            P.dma("sp", I("dma_start", out=dst[:], in_=src.ap()), writes=[key], sem="c_" + key)

        stM = ExitStack()
        st.enter_context(stM)
        qda = sbt(stM, "qda", [128, 8, NO], BF16)
        qr = sbt(stM, "qr", [128, 2, NO], BF16)
        for h in range(H):
            for m in range(2):
                P.dma("pool", I("dma_start", out=qda[64:66, 2 * h + m, :], in_=qaug_d.ap()[:, h, :]),
                      writes=[("qda", 2 * h + m, "aug")], sem=f"c_qa{2 * h + m}")

        w_in_v = w_in.ap().rearrange("(k p) c -> p k c", p=128)

        def load_w(stack, name, ranges):
            ncols = sum(r[1] - r[0] for r in ranges)
            W = sbt(stack, name, [128, 8, ncols], BF16)
            stg = [sbt(stack, f"{name}_st{i}", [128, 512]) for i in range(2)]
            cnt = 0
            d0 = 0
            for (a0, a1) in ranges:
                for c0 in range(a0, a1, 512):
                    c1 = min(a1, c0 + 512)
                    for kc in range(8):
                        sl = cnt % 2
                        cnt += 1
                        key = (name + "_st", sl)
                        P.dma("sp", I("dma_start", out=stg[sl][:, 0:c1 - c0], in_=w_in_v[:, kc, c0:c1]),
                              writes=[key], sem=f"{name}_st{sl}")
                        P.op("dve", I("tensor_scalar", out=W[:, kc, d0:d0 + c1 - c0], in0=stg[sl][:, 0:c1 - c0],
                                      scalar1=g1_sb[:, kc:kc + 1], scalar2=None, op0=ALU.mult),
                             reads=[key, "g1"], writes=[(name, kc)])
                    d0 += c1 - c0
            return W, [(name, kc) for kc in range(8)]

        def mk_front(stack, tag):
            xb = [sbt(stack, f"xb{tag}{i}", [128, 8, 512], BF16) for i in range(2)]
            sq = sbt(stack, f"sq{tag}", [128, 8, 512], BF16)
            rbc = [sbt(stack, f"rbc{tag}{i}", [128, 512]) for i in range(2)]
            rtk = [sbt(stack, f"rtk{tag}{i}", [128, 4]) for i in range(2)]
            lnb = sbt(stack, f"lnb{tag}", [128, 512])

            issued = set()

            def front_load(src_ap_T, blk, par):
                if blk in issued:
                    return
                issued.add(blk)
                xbk = ("xb" + tag, par)
                xv = src_ap_T.rearrange("(k p) t -> p k t", p=128)
                for half in range(2):
                    P.dma("pool", I("dma_start", out=xb[par][:, 4 * half:4 * half + 4, :],
                                    in_=xv[:, 4 * half:4 * half + 4, blk * 512:(blk + 1) * 512]),
                          writes=[(xbk, half)], sem=f"xb{tag}{par}_{half}")

            def front(src_ap_T, blk, par, nblk=None):
                xbk, sqk = ("xb" + tag, par), "sq" + tag
                front_load(src_ap_T, blk, par)
                for half in range(2):
                    P.op("pool" if half else "dve", I("tensor_tensor", out=sq[:, 4 * half:4 * half + 4, :],
                                                      in0=xb[par][:, 4 * half:4 * half + 4, :],
                                                      in1=xb[par][:, 4 * half:4 * half + 4, :], op=ALU.mult),
                         reads=[(xbk, half)], writes=[(sqk, half)])
                for kc in range(8):
                    P.op("pe", I("matmul", psum[7][:, :], lhsT=ones_bf[:, :], rhs=sq[:, kc, :], start=(kc == 0), stop=(kc == 7)),
                         reads=[(sqk, kc // 4), "ones_bf"], writes=[PS[7]])
                P.op("act", I("activation", out=lnb[:], in_=psum[7][:, :], func=AF.Ln, bias=epsc[:, 0:1], scale=1.0 / D),
                     reads=[PS[7], "epsc"], writes=["lnb" + tag])
                P.op("act", I("activation", out=rbc[par][:], in_=lnb[:], func=AF.Exp, scale=-0.5),
                     reads=["lnb" + tag], writes=[("rbc" + tag, par)])
                for tt in range(4):
                    for kc in range(8):
                        P.op("pe", I("matmul", psum[7][:, tt:tt + 1], lhsT=sq[:, kc, tt * 128:(tt + 1) * 128], rhs=ones_bf[:, 0:1],
                                     start=(kc == 0 and tt == 0), stop=(kc == 7), skip_group_check=True),
                             reads=[(sqk, kc // 4), "ones_bf"], writes=[PS[7]])
                P.op("act", I("activation", out=lnb[:, 0:4], in_=psum[7][:, 0:4], func=AF.Ln, bias=epsc[:, 0:1], scale=1.0 / D),
                     reads=[PS[7], "epsc"], writes=["lnb" + tag])
                P.op("act", I("activation", out=rtk[par][:], in_=lnb[:, 0:4], func=AF.Exp, scale=-0.5),
                     reads=["lnb" + tag], writes=[("rtk" + tag, par)])
                return [(xbk, 0), (xbk, 1)]
            front.load = front_load
            return xb, rbc, rtk, front

        pa = 0

        def proj(XB, WK, lhsT_fn, rhs_fn, M, N):
            nonlocal pa
            pb, pk = psum[pa % 2], PS[pa % 2]
            pa += 1
            for kc in range(8):
                P.op("pe", I("matmul", pb[0:M, 0:N], lhsT=lhsT_fn(kc), rhs=rhs_fn(kc), start=(kc == 0), stop=(kc == 7)),
                     reads=XB + [WK[kc]], writes=[pk])
            return pb, pk

        WA, WKA = load_w(stM, "WA", [(512, 1536), (1792, 2560)])

        with ExitStack() as s0:
            W0, WK0 = load_w(s0, "W0", [(0, 512), (1536, 1792), (2560, 3072)])
            xb, rbc, rtk, front = mk_front(s0, "o")
            for blk in range(NO // 512):
                par = blk % 2
                XB = front(xoT.ap(), blk, par)
                for hm in range(8):
                    c0 = (hm // 2) * 128 + (hm % 2) * 64
                    pb, pk = proj(XB, WK0, lambda kc: W0[:, kc, c0:c0 + 64], lambda kc: xb[par][:, kc, :], 64, 512)
                    P.op("dve", I("tensor_tensor", out=qda[0:64, hm, blk * 512:(blk + 1) * 512], in0=pb[0:64, :],
                                  in1=rbc[par][0:64, :], op=ALU.mult),
                         reads=[pk, ("rbco", par)], writes=[("qda", hm, blk)])
                for grp in range(2):
                    c0 = 512 + grp * 128
                    pb, pk = proj(XB, WK0, lambda kc: W0[:, kc, c0:c0 + 128], lambda kc: xb[par][:, kc, :], 128, 512)
                    P.op("dve", I("tensor_tensor", out=qr[:, grp, blk * 512:(blk + 1) * 512], in0=pb[:, :],
                                  in1=rbc[par][:, :], op=ALU.mult),
                         reads=[pk, ("rbco", par)], writes=[("qr", blk)])
                for tt in range(4):
                    pb, pk = proj(XB, WK0, lambda kc: xb[par][:, kc, tt * 128:(tt + 1) * 128], lambda kc: W0[:, kc, 768:1280], 128, 512)
                    P.op("act", I("activation", out=gsl[:, blk * 4 + tt, :], in_=pb[:, :], func=AF.Silu,
                                  scale=rtk[par][:, tt:tt + 1]),
                         reads=[pk, ("rtko", par)], writes=[("gsl", blk * 4 + tt)])
            P.barrier()

        with ExitStack() as sA:
            xb, rbc, rtk, front = mk_front(sA, "a")
            kst = sbt(sA, "kst", [128, 4, 512], BF16)
            vst = sbt(sA, "vst", [128, 4, 512], BF16)
            krT = sbt(sA, "krT", [128, 2, 512], BF16)
            vr = sbt(sA, "vr", [128, 4, 512], BF16)
            krk = sbt(sA, "krk", [128, 4, 256], BF16)
            NPB = 4
            ADp = [sbt(sA, f"ADp{i}", [128, 2, 2, 248], BF16) for i in range(NPB)]
            Qxp = [sbt(sA, f"Qxp{i}", [128, 2, 248], BF16) for i in range(NPB)]
            Sst = sbt(sA, "Sst", [128, 256])
            Sbf = sbt(sA, "Sbf", [128, 256], BF16)
            rtmp = sbt(sA, "rtmp", [128, 512])
            for i in range(NPB):
                P.op("pool", I("memset", ADp[i][:], 0.0), writes=[("ADp", i)])
                P.op("pool", I("memset", Qxp[i][:], 0.0), writes=[("Qxp", i)])
            P.op("pool", I("memset", Sst[:], 0.0), writes=["Sst"])
            P.op("pool", I("memset", Sbf[:], 0.0), writes=[("Sbf", 0)])
            kT_v = kT_s.ap().rearrange("h m d s -> (m d) h s")
            v_v = v_s.ap().rearrange("(n p) c -> p n c", p=128)
            retfirst = {2: True, 3: True}
            Sbf2 = [Sbf, sbt(sA, "Sbf_b", [128, 256], BF16)]
            P.op("pool", I("memset", Sbf2[1][:], 0.0), writes=[("Sbf", 1)])
            for blk in range(NB):
                par = blk % 2
                XB = front(xT.ap(), blk, par)
                pending_prefetch = (blk + 1 < NB)

                def kv_unit(u, blk=blk, par=par, XB=XB):
                    if u < 4:
                        h = u
                        c0 = h * 128
                        pb, pk = proj(XB, WKA, lambda kc: WA[:, kc, c0:c0 + 128], lambda kc: xb[par][:, kc, :], 128, 512)
                        P.op("dve", I("tensor_tensor", out=kst[:, h, :], in0=pb[:, :], in1=rbc[par][:, :], op=ALU.mult),
                             reads=[pk, ("rbca", par)], writes=[("kst", h)])
                        if u == 3:
                            P.dma("sp", I("dma_start", out=kT_v[:, :, blk * 512:(blk + 1) * 512], in_=kst[:, :, :]),
                                  reads=[("kst", h_) for h_ in range(H)], writes=[("kT_s", blk)], sem="kst")
                    else:
                        tt = u - 4
                        pb, pk = proj(XB, WKA, lambda kc: xb[par][:, kc, tt * 128:(tt + 1) * 128], lambda kc: WA[:, kc, 512:1024], 128, 512)
                        P.op("act", I("activation", out=vst[:, tt, :], in_=pb[:, :], func=AF.Copy, scale=rtk[par][:, tt:tt + 1]),
                             reads=[pk, ("rtka", par)], writes=[("vst", tt)])
                        if u == 7:
                            P.dma("sp", I("dma_start", out=v_v[:, blk * 4:(blk + 1) * 4, :], in_=vst[:, :, :]),
                                  reads=[("vst", t_) for t_ in range(4)], writes=[("v_s", blk)], sem="vst")

                for grp in range(2):
                    c0 = 1024 + grp * 128
                    pb, pk = proj(XB, WKA, lambda kc: WA[:, kc, c0:c0 + 128], lambda kc: xb[par][:, kc, :], 128, 512)
                    P.op("dve", I("tensor_tensor", out=krT[:, grp, :], in0=pb[:, :], in1=rbc[par][:, :], op=ALU.mult),
                         reads=[pk, ("rbca", par)], writes=[("krT", grp)])
                for tt in range(4):
                    pb, pk = proj(XB, WKA, lambda kc: xb[par][:, kc, tt * 128:(tt + 1) * 128], lambda kc: WA[:, kc, 1280:1792], 128, 512)
                    P.op("act", I("activation", out=vr[:, tt, :], in_=pb[:, :], func=AF.Copy, scale=rtk[par][:, tt:tt + 1]),
                         reads=[pk, ("rtka", par)], writes=[("vr", tt)])
                for tt in range(4):
                    pbT, pkT = psum[pa % 2], PS[pa % 2]
                    pa += 1
                    pbTb = pbT.bitcast(BF16)
                    for grp in range(2):
                        P.op("pe", I("transpose", out=pbTb[:, grp * 128:(grp + 1) * 128], in_=krT[:, grp, tt * 128:(tt + 1) * 128],
                                     identity=ident_bf[:]), reads=[("krT", grp), "ident_bf"], writes=[pkT])
                    P.op("dve", I("tensor_tensor", out=krk[:, tt, :], in0=pbTb[:, 0:256], in1=kdec[:, :], op=ALU.mult),
                         reads=[pkT, "kdec"], writes=[("krk", tt)])
                for ch in range(8):
                    n = blk * 8 + ch
                    tt, cp = ch // 2, ch % 2
                    T, w = n // 16, n % 16
                    pbi = n % NPB
                    wo = 120 - 8 * w
                    rows = slice(cp * 64, cp * 64 + 64)
                    scur, snxt = n % 2, (n + 1) % 2
                    for h in range(H):
                        hr = slice((h % 2) * 64, (h % 2) * 64 + 64)
                        bk = 4 if h % 2 == 0 else 6
                        P.op("pe", I("matmul", psum[bk][rows, (h // 2) * 8:(h // 2) * 8 + 8],
                                     lhsT=krT[hr, h // 2, ch * 64:ch * 64 + 64], rhs=qr[hr, h // 2, 8 * n:8 * n + 8],
                                     start=True, stop=True, skip_group_check=True),
                             reads=[("krT", h // 2), ("qr", (8 * n) // 512)], writes=[PS[bk]])
                    for hh in range(2):
                        bk = 4 if hh == 0 else 6
                        P.op("dve", I("tensor_tensor", out=ADp[pbi][rows, hh, :, 120:128],
                                      in0=psum[bk][rows, 0:16].rearrange("p (g c) -> p g c", g=2), in1=dtt[rows, hh, :, :],
                                      op=ALU.mult),
                             reads=[PS[bk], "dt"], writes=[("ADp", pbi)])
                    P.op("pool", I("tensor_tensor", out=Qxp[pbi][:, :, 120:128], in0=qr[:, :, 8 * n:8 * n + 8],
                                   in1=qdec8[:, :, :], op=ALU.mult),
                         reads=[("qr", (8 * n) // 512), "qdec8"], writes=[("Qxp", pbi)])
                    for h in range(H):
                        hr = slice((h % 2) * 64, (h % 2) * 64 + 64)
                        P.op("pe", I("matmul", psum[5][hr, (h // 2) * 128:(h // 2) * 128 + 128],
                                     lhsT=krk[rows, tt, h * 64:h * 64 + 64], rhs=vr[rows, tt, h * 128:(h + 1) * 128],
                                     start=True, stop=True, skip_group_check=True),
                             reads=[("krk", tt), ("vr", tt)], writes=[PS[5]])
                    for grp in range(2):
                        P.op("dve", I("scalar_tensor_tensor", out=Sst[:, grp * 128:(grp + 1) * 128],
                                      in0=Sst[:, grp * 128:(grp + 1) * 128], scalar=sdec[:, grp:grp + 1],
                                      in1=psum[5][:, grp * 128:(grp + 1) * 128], op0=ALU.mult, op1=ALU.add),
                             reads=["Sst", PS[5], "sdec"], writes=["Sst"])
                    P.op("pool", I("tensor_copy", out=Sbf2[snxt][:, :], in_=Sst[:, :]), reads=["Sst"], writes=[("Sbf", snxt)])
                    kv_unit(ch)
                    if ch == 0 and pending_prefetch:
                        front.load(xT.ap(), blk + 1, (blk + 1) % 2)
                    if w == 0:
                        retfirst = {2: True, 3: True}
                    for h in range(H):
                        hr = slice((h % 2) * 64, (h % 2) * 64 + 64)
                        bk = 2 + cp
                        P.op("pe", I("matmul", psum[bk][:, h * 128:(h + 1) * 128], lhsT=ADp[pbi][rows, h % 2, h // 2, wo:wo + 128],
                                     rhs=vr[rows, tt, h * 128:(h + 1) * 128], start=retfirst[bk], stop=False, skip_group_check=True),
                             reads=[("ADp", pbi), ("vr", tt)], writes=[PS[bk]])
                        retfirst[bk] = False
                        bk = 2 + (h % 2)
                        P.op("pe", I("matmul", psum[bk][:, h * 128:(h + 1) * 128], lhsT=Qxp[pbi][hr, h // 2, wo:wo + 128],
                                     rhs=Sbf2[scur][hr, (h // 2) * 128:(h // 2) * 128 + 128], start=retfirst[bk], stop=False,
                                     skip_group_check=True),
                             reads=[("Qxp", pbi), ("Sbf", scur)], writes=[PS[bk]])
                        retfirst[bk] = False
                    if w == 15:
                        P.op("act", I("activation", out=rtmp[:, :], in_=psum[3][:, :], func=AF.Copy), reads=[PS[3]], writes=["rtmp"])
                        P.op("dve", I("tensor_tensor", out=ret[:, T, :], in0=psum[2][:, :], in1=rtmp[:, :], op=ALU.add),
                             reads=[PS[2], "rtmp"], writes=[("ret", T)])
            P.barrier()

        if stage == "ret":
            dst = sb("dbg_st", [128, 512])
            dv = dbg.ap().rearrange("(t p) c -> p t c", p=128)
            for t in range(NT):
                P.op("dve", I("tensor_copy", out=dst[:], in_=ret[:, t, :]), reads=[("ret", t)], writes=["dbg_st"])
                P.dma("sp", I("dma_start", out=dv[:, t, 0:512], in_=dst[:]), reads=["dbg_st"], writes=[("OUT", t)], sem="out")
            return finish()

        with ExitStack() as sB:
            KB = 8
            NKB = 3
            kbuf = [sbt(sB, f"kbuf{i}", [128, 2, KB * 128], BF16) for i in range(NKB)]
            vbuf = [sbt(sB, f"vbuf{i}", [128, KB, 132], BF16) for i in range(NKB)]
            eT = [sbt(sB, f"eT{i}", [128, 512], BF16) for i in range(4)]
            rz = sbt(sB, "rz", [128, 8])
            ust = [[sbt(sB, f"ust{g_}{m_}", [128, 516]) for m_ in range(2)] for g_ in range(2)]
            for i in range(NKB):
                P.op("pool", I("memset", kbuf[i][64:66, :, :], 1.0), writes=[("kbuf", i, "ones")])
                P.op("pool", I("memset", vbuf[i][:, :, 128:129], 1.0), writes=[("vbuf", i, "ones")])
            kT_l = kT_s.ap()
            v_l = v_s.ap().rearrange("(n p) c -> p n c", p=128)
            ldc = sc = ec = 0
            SKEW = 2
            ncols_total = [0]
            for h in range(H):
                for G in range(NG):
                    nkt = 32 * (G + 1)
                    steps = []
                    batches = []
                    Wh = ALIBI_CUT / (2.0 ** (-8.0 * (h + 1) / H))
                    chi = {}
                    for kt in range(nkt):
                        jj = kt - 32 * G
                        c0 = 16 * jj if jj >= 0 else 0
                        cq_max = math.floor((Wh + 128 * kt + 127 - 4096 * G) / 64.0)
                        c_hi = min(512, 8 * (cq_max + 1))
                        if c_hi <= c0:
                            continue
                        chi[kt] = min(512, ((c_hi + 127) // 128) * 128)
                    for kb0 in range(0, nkt, KB):
                        kts = [kt for kt in range(kb0, kb0 + KB) if kt in chi]
                        if not kts:
                            continue
                        lb = ldc % NKB
                        ldc += 1
                        batches.append((kb0, lb))
                        for kt in kts:
                            for m in range(2):
                                steps.append((kb0, lb, kt, m))
                    ncols_total[0] += sum(chi[st_[2]] - (16 * max(st_[2] - 32 * G, 0) // 128) * 128 for st_ in steps)
                    loaded = set()
                    started = set()

                    def kv_load(bi):
                        if bi >= len(batches) or bi in loaded:
                            return
                        loaded.add(bi)
                        kb0, lb = batches[bi]
                        P.dma("sp", I("dma_start", out=kbuf[lb][0:64, :, :],
                                      in_=kT_l[h, :, :, kb0 * 128:(kb0 + KB) * 128].rearrange("m d s -> d m s")),
                              reads=[("kT_s", b_) for b_ in range(kb0 // 4, (kb0 + KB) // 4)], writes=[("kbuf", lb)], sem=f"kbuf{lb}")
                        P.dma("sp", I("dma_start", out=vbuf[lb][:, :, 0:128], in_=v_l[:, kb0:kb0 + KB, h * 128:(h + 1) * 128]),
                              reads=[("v_s", b_) for b_ in range(kb0 // 4, (kb0 + KB) // 4)], writes=[("vbuf", lb)], sem=f"vbuf{lb}")

                    bidx = {kb0_: i_ for i_, (kb0_, _) in enumerate(batches)}
                    seenb = set()

                    def s_front(kb0, lb, kt, m, sp_, spk, eb, ek):
                        if kb0 not in seenb:
                            seenb.add(kb0)
                            kv_load(bidx[kb0])
                            kv_load(bidx[kb0] + 1)
                        ce = chi[kt]
                        jj = kt - 32 * G
                        c0 = 16 * jj if jj >= 0 else 0
                        cc0 = (c0 // 128) * 128
                        kk = kt - kb0
                        hm = 2 * h + m
                        P.op("pe", I("matmul", sp_[:, cc0:ce], lhsT=kbuf[lb][0:66, m, kk * 128:(kk + 1) * 128],
                                     rhs=qda[0:66, hm, G * 512 + cc0:G * 512 + ce], start=True, stop=True),
                             reads=[("kbuf", lb), ("kbuf", lb, "ones"), ("qda", hm, G), ("qda", hm, "aug")], writes=[spk])
                        if jj >= 0:
                            P.op("dve", I("tensor_tensor", out=sp_[:, c0:c0 + 16], in0=sp_[:, c0:c0 + 16],
                                          in1=fix[:, h, :], op=ALU.add), reads=[spk, "fix"], writes=[spk])
                            if c0 > cc0:
                                P.op("pool", I("memset", eb[:, cc0:c0], 0.0), writes=[ek])
                        P.op("act", I("activation", out=eb[:, c0:ce], in_=sp_[:, c0:ce], func=AF.Exp,
                                      bias=biask[:, h, kt - 32 * G + 96:kt - 32 * G + 97], scale=0.125),
                             reads=[spk, "biask"], writes=[ek])

                    def s_back(kb0, lb, kt, m, eb, ek):
                        jj = kt - 32 * G
                        c0 = 16 * jj if jj >= 0 else 0
                        cc0 = (c0 // 128) * 128
                        kk = kt - kb0
                        for j in range(cc0 // 128, chi[kt] // 128):
                            bank = 4 + 2 * m + (1 if j == 3 else 0)
                            col = 0 if j == 3 else j * 129
                            first = bank not in started
                            started.add(bank)
                            P.op("pe", I("matmul", psum[bank][:, col:col + 129], lhsT=eb[:, j * 128:(j + 1) * 128],
                                         rhs=vbuf[lb][:, kk, 0:129], start=first, stop=(kt == nkt - 1), skip_group_check=True),
                                 reads=[ek, ("vbuf", lb), ("vbuf", lb, "ones")], writes=[PS[bank]])

                    bufs = []
                    for i in range(len(steps) + SKEW):
                        if i < len(steps):
                            sp_, spk = psum[sc % 4], PS[sc % 4]
                            sc += 1
                            eb, ek = eT[ec % 4], ("eT", ec % 4)
                            ec += 1
                            bufs.append((eb, ek))
                            s_front(*steps[i], sp_, spk, eb, ek)
                        if i - SKEW >= 0:
                            s_back(*steps[i - SKEW], *bufs[i - SKEW])
                    gp_ = (h * NG + G) % 2
                    for m in range(2):
                        P.op("dve", I("tensor_copy", out=ust[gp_][m][:, 0:387], in_=psum[4 + 2 * m][:, 0:387]),
                             reads=[PS[4 + 2 * m]], writes=[("ust", gp_, m, 0)])
                        P.op("act", I("activation", out=ust[gp_][m][:, 387:516], in_=psum[5 + 2 * m][:, 0:129], func=AF.Copy),
                             reads=[PS[5 + 2 * m]], writes=[("ust", gp_, m, 1)])
                    for m in range(2):
                        hm = 2 * h + m
                        for j in range(4):
                            col = j * 129
                            part = 1 if j == 3 else 0
                            P.op("dve", I("reciprocal", out=rz[:, 4 * m + j:4 * m + j + 1], in_=ust[gp_][m][:, col + 128:col + 129]),
                                 reads=[("ust", gp_, m, part)], writes=[("rz", m, j)])
                            P.op("dve", I("tensor_scalar", out=att[:, 4 * G + j, hm, :], in0=ust[gp_][m][:, col:col + 128],
                                          scalar1=rz[:, 4 * m + j:4 * m + j + 1], scalar2=None, op0=ALU.mult),
                                 reads=[("ust", gp_, m, part), ("rz", m, j)], writes=[("att", 4 * G + j)])
            print('attention score columns per core:', ncols_total[0])
            P.barrier()
        stM.close()

        if stage == "attn":
            dst = sb("dbg_st", [128, 1024])
            dv = dbg.ap().rearrange("(t p) c -> p t c", p=128)
            for t in range(NT):
                P.op("dve", I("tensor_copy", out=dst[:], in_=att[:, t, :, :].rearrange("p a b -> p (a b)")),
                     reads=[("att", t)], writes=["dbg_st"])
                P.dma("sp", I("dma_start", out=dv[:, t, 0:1024], in_=dst[:]), reads=["dbg_st"], writes=[("OUT", t)], sem="out")
            return finish()

        sC = ExitStack()
        st.enter_context(sC)
        Wo = sbt(sC, "Wo", [128, 8, D], BF16)
        g2bc = sbt(sC, "g2bc", [128, D])
        gfbc = sbt(sC, "gfbc", [128, D])
        Wr = sbt(sC, "Wr", [128, 8, 36])
        brow = sbt(sC, "brow", [1, 36])
        lam_sb = sbt(sC, "lam_sb", [1, 256])
        lam_t = sbt(sC, "lam_t", [1, 136])
        nlam = sbt(sC, "nlam", [128, 1])
        sgs = sbt(sC, "sgs", [128, 1])
        grow = sbt(sC, "grow", [1, 2 * D])
        iota32 = sbt(sC, "iota32", [128, 64])
        iot2 = sbt(sC, "iot2", [128, 64], I32)
        Ustr = sbt(sC, "Ustr", [128, 128], BF16)
        OH = sbt(sC, "OH", [128, 2 * NT, 32], BF16)
        wts = sbt(sC, "wts", [128, NT, 2])
        dest_f = sbt(sC, "dest_f", [128, 2 * NT])
        dest_i = sbt(sC, "dest_i", [128, 2 * NT], I32)
        be_i = sbt(sC, "be_i", [128, NSB], I32)
        idxw = sbt(sC, "idxw", [128, NSB], I32)
        wost = [sbt(sC, f"wost{i}", [128, D]) for i in range(2)]

        zt = sbt(sC, "zt", [128, D], BF16)
        P.op("pool", I("memset", zt[:], 0.0), writes=["zt"])
        for b in range(NBLK):
            P.dma("sp", I("dma_start", out=xs_s.ap()[b * 128:(b + 1) * 128, :], in_=zt[:]), reads=["zt"], writes=[("xs_z", b)], sem="xsz")
        XSZ = [("xs_z", b) for b in range(NBLK)]
        P.dma("sp", I("dma_start", out=lam_sb[:], in_=lamv.ap().rearrange("a b c -> a (b c)")), writes=["lam_sb"], sem="c_lam")
        P.op("dve", I("tensor_tensor", out=lam_t[0:1, 0:64], in0=lam_sb[0:1, 0:64], in1=lam_sb[0:1, 64:128], op=ALU.mult),
             reads=["lam_sb"], writes=["lam_t"])
        P.op("dve", I("tensor_tensor", out=lam_t[0:1, 64:128], in0=lam_sb[0:1, 128:192], in1=lam_sb[0:1, 192:256], op=ALU.mult),
             reads=["lam_sb"], writes=["lam_t"])
        P.op("dve", I("tensor_reduce", out=lam_t[0:1, 128:130], in_=lam_t[0:1, 0:128].rearrange("p (a b) -> p a b", a=2),
                      axis=AX.X, op=ALU.add), reads=["lam_t"], writes=["lam_t"])
        P.op("act", I("activation", out=lam_t[0:1, 130:132], in_=lam_t[0:1, 128:130], func=AF.Exp), reads=["lam_t"], writes=["lam_t"])
        P.op("dve", I("tensor_tensor", out=lam_t[0:1, 132:133], in0=lam_t[0:1, 131:132], in1=lam_t[0:1, 130:131], op=ALU.subtract),
             reads=["lam_t"], writes=["lam_t"])
        P.op("dve", I("tensor_scalar", out=lam_t[0:1, 133:134], in0=lam_t[0:1, 132:133], scalar1=-LAMBDA_INIT, scalar2=None,
                      op0=ALU.add), reads=["lam_t"], writes=["lam_t"])
        P.op("pe", I("matmul", psum[0][:, 0:1], lhsT=ones_f[0:1, :], rhs=lam_t[0:1, 133:134], start=True, stop=True),
             reads=["lam_t", "ones_f"], writes=[PS[0]])
        P.op("dve", I("tensor_copy", out=nlam[:], in_=psum[0][:, 0:1]), reads=[PS[0]], writes=["nlam"])
        P.dma("sp", I("dma_start", out=sgs[:], in_=sg.ap()), writes=["sgs0"], sem="c_sg")
        P.op("dve", I("tensor_scalar", out=sgs[:], in0=sgs[:], scalar1=1.0 - LAMBDA_INIT, scalar2=None, op0=ALU.mult),
             reads=["sgs0"], writes=["sgs"])
        w_out_v = w_out.ap().rearrange("(k p) c -> p k c", p=128)
        for kc in range(8):
            sl = kc % 2
            P.dma("sp", I("dma_start", out=wost[sl][:], in_=w_out_v[:, kc, :]), writes=[("wost", sl)], sem=f"wost{sl}")
            if kc < 4:
                P.op("dve", I("tensor_scalar", out=Wo[:, kc, :], in0=wost[sl][:], scalar1=sgs[:, 0:1], scalar2=None, op0=ALU.mult),
                     reads=[("wost", sl), "sgs"], writes=[("Wo", kc)])
            else:
                P.op("dve", I("tensor_copy", out=Wo[:, kc, :], in_=wost[sl][:]), reads=[("wost", sl)], writes=[("Wo", kc)])
        WOK = [("Wo", kc) for kc in range(8)]
        P.dma("sp", I("dma_start", out=grow[0:1, 0:D], in_=g2.ap()), writes=["grow_a"], sem="c_g2")
        P.dma("sp", I("dma_start", out=grow[0:1, D:2 * D], in_=gf.ap()), writes=["grow_b"], sem="c_gf")
        for i, (dstt, key) in enumerate(((g2bc, "g2bc"), (gfbc, "gfbc"))):
            for half in range(2):
                P.op("pe", I("matmul", psum[1 + half][:, :], lhsT=ones_f[0:1, :], rhs=grow[0:1, i * D + half * 512:i * D + half * 512 + 512],
                             start=True, stop=True), reads=["grow_a", "grow_b", "ones_f"], writes=[PS[1 + half]])
                P.op("dve", I("tensor_copy", out=dstt[:, half * 512:(half + 1) * 512], in_=psum[1 + half][:, :]),
                     reads=[PS[1 + half]], writes=[(key, half)])
        P.dma("sp", I("dma_start", out=Wr[:], in_=wr.ap().rearrange("(k p) c -> p k c", p=128)), writes=["Wr"], sem="c_wr")
        P.dma("sp", I("dma_start", out=brow[:], in_=br.ap()), writes=["brow"], sem="c_br")
        P.op("pool", I("iota", iot2[:], pattern=[[1, 64]], base=0, channel_multiplier=0), writes=["iot2"])
        P.op("dve", I("tensor_copy", out=iota32[:], in_=iot2[:]), reads=["iot2"], writes=["iota32"])
        P.op("dve", I("tensor_single_scalar", out=Ustr[:], in_=iotf[:], scalar=0.0, op=ALU.is_gt), reads=["iotf"], writes=["Ustr"])

        sT = ExitStack()
        st.enter_context(sT)
        od = [sbt(sT, f"od{i_}", [128, 4, 128]) for i_ in range(2)]
        sqt = [sbt(sT, f"sqt{i_}", [128, 4, 128]) for i_ in range(2)]
        ssr = [sbt(sT, f"ssr{i_}", [128, 16]) for i_ in range(2)]
        mix = [sbt(sT, f"mix{i_}", [128, D], BF16) for i_ in range(2)]
        mixT = [sbt(sT, f"mixT{i_}", [128, 8, 128], BF16) for i_ in range(2)]
        xot = [sbt(sT, f"xot{i_}", [128, D]) for i_ in range(2)]
        h1t = [sbt(sT, f"h1t{i_}", [128, D]) for i_ in range(2)]
        junk = [sbt(sT, f"junk{i_}", [128, D], BF16) for i_ in range(2)]
        xn2f = [sbt(sT, f"xn2f{i_}", [128, D]) for i_ in range(2)]
        xn2b = [sbt(sT, f"xn2b{i_}", [128, D], BF16) for i_ in range(2)]
        xnT = [sbt(sT, f"xnT{i_}", [128, 8, 128]) for i_ in range(2)]
        lg = [sbt(sT, f"lg{i_}", [128, 36]) for i_ in range(2)]
        rt_ = [sbt(sT, f"rt_{i_}", [128, 64]) for i_ in range(2)]
        t8 = [sbt(sT, f"t8{i_}", [128, 8]) for i_ in range(2)]
        i8 = [sbt(sT, f"i8{i_}", [128, 8], U32) for i_ in range(2)]
        psbf = [psum[i].bitcast(BF16) for i in range(8)]
        xo_v = xo.ap().rearrange("(t p) c -> p t c", p=128)
        h1_v = h1_s.ap().rearrange("(t p) c -> p t c", p=128)
        xn_v = xn_s.ap().rearrange("(t p) c -> p t c", p=128)
        out_v = out.ap().rearrange("(t p) c -> p t c", p=128)

        def rstd_from(ss_ap, out_ap, n, key_in, key_out, scale):
            P.op("act", I("activation", out=out_ap, in_=ss_ap, func=AF.Ln, bias=epsc[:, 0:1], scale=scale),
                 reads=[key_in, "epsc"], writes=[key_out])
            P.op("act", I("activation", out=out_ap, in_=out_ap, func=AF.Exp, scale=-0.5), reads=[key_out], writes=[key_out])

        def C1(T):
            pz = T % 2
            attv = att[:, T, :, :].rearrange("p (h m) e -> p h m e", m=2)
            P.op("dve", I("scalar_tensor_tensor", out=od[pz][:], in0=attv[:, :, 1, :], scalar=nlam[:, 0:1], in1=attv[:, :, 0, :],
                          op0=ALU.mult, op1=ALU.add), reads=[("att", T), "nlam"], writes=[("od", pz)])
            P.op("dve", I("tensor_tensor", out=sqt[pz][:], in0=od[pz][:], in1=od[pz][:], op=ALU.mult), reads=[("od", pz)], writes=[("sqt", pz)])
            P.op("dve", I("tensor_reduce", out=ssr[pz][:, 0:4], in_=sqt[pz][:], axis=AX.X, op=ALU.add), reads=[("sqt", pz)], writes=[("ssr", pz)])
            retv = ret[:, T, :].rearrange("p (h e) -> p h e", h=H)
            P.op("dve", I("tensor_tensor", out=sqt[pz][:], in0=retv, in1=retv, op=ALU.mult), reads=[("ret", T), ("ssr", pz)], writes=[("sqt", pz)])
            P.op("dve", I("tensor_reduce", out=ssr[pz][:, 4:8], in_=sqt[pz][:], axis=AX.X, op=ALU.add), reads=[("sqt", pz)], writes=[("ssr", pz)])
            rstd_from(ssr[pz][:, 0:8], ssr[pz][:, 8:16], 8, ("ssr", pz), ("ssr2", pz), 1.0 / 128)
            for h in range(H):
                P.op("dve", I("tensor_scalar", out=mix[pz][:, h * 128:(h + 1) * 128], in0=od[pz][:, h, :], scalar1=ssr[pz][:, 8 + h:9 + h],
                              scalar2=None, op0=ALU.mult), reads=[("od", pz), ("ssr2", pz)], writes=[("mix", pz, h)])
                P.op("dve", I("scalar_tensor_tensor", out=mix[pz][:, 512 + h * 128:512 + (h + 1) * 128], in0=retv[:, h, :],
                              scalar=ssr[pz][:, 12 + h:13 + h], in1=gsl[:, T, h * 128:(h + 1) * 128], op0=ALU.mult, op1=ALU.mult),
                     reads=[("ret", T), ("ssr2", pz), ("gsl", T)], writes=[("mix", pz, 4 + h)])

        def C2(T):
            pz = T % 2
            for kc in range(8):
                P.op("pe", I("transpose", out=psbf[pz][:, kc * 128:(kc + 1) * 128], in_=mix[pz][:, kc * 128:(kc + 1) * 128],
                             identity=ident_bf[:]), reads=[("mix", pz, kc), "ident_bf"], writes=[PS[pz]])
            P.op("act", I("activation", out=mixT[pz][:].rearrange("p a b -> p (a b)"), in_=psbf[pz][:, :], func=AF.Copy),
                 reads=[PS[pz]], writes=[("mixT", pz)])
            P.dma("sp", I("dma_start", out=xot[pz][:], in_=xo_v[:, T, :]), writes=[("xot", pz)], sem=f"xot{pz}")

        def C3(T):
            pz = T % 2
            for half in range(2):
                for kc in range(8):
                    P.op("pe", I("matmul", psum[2 + half][:, :], lhsT=mixT[pz][:, kc, :], rhs=Wo[:, kc, half * 512:(half + 1) * 512],
                                 start=(kc == 0), stop=(kc == 7)), reads=[("mixT", pz), WOK[kc]], writes=[PS[2 + half]])
                P.op("dve", I("tensor_tensor", out=h1t[pz][:, half * 512:(half + 1) * 512], in0=psum[2 + half][:, :],
                              in1=xot[pz][:, half * 512:(half + 1) * 512], op=ALU.add),
                     reads=[PS[2 + half], ("xot", pz)], writes=[("h1t", pz, half)])
            P.dma("sp", I("dma_start", out=h1_v[:, T, :], in_=h1t[pz][:]), reads=[("h1t", pz, 0), ("h1t", pz, 1)], writes=[("h1_s", T)], sem=f"h1st{pz}")
            P.op("act", I("activation", out=junk[pz][:], in_=h1t[pz][:], func=AF.Square, accum_out=rt_[pz][:, 0:1]),
                 reads=[("h1t", pz, 0), ("h1t", pz, 1)], writes=[("junk", pz), ("rt0", pz)])
            rstd_from(rt_[pz][:, 0:1], rt_[pz][:, 1:2], 1, ("rt0", pz), ("rt1", pz), 1.0 / D)
            P.op("dve", I("scalar_tensor_tensor", out=xn2f[pz][:], in0=h1t[pz][:], scalar=rt_[pz][:, 1:2], in1=g2bc[:], op0=ALU.mult, op1=ALU.mult),
                 reads=[("h1t", pz, 0), ("h1t", pz, 1), ("rt1", pz), ("g2bc", 0), ("g2bc", 1)], writes=[("xn2f", pz)])
            P.op("act", I("activation", out=xn2b[pz][:], in_=xn2f[pz][:], func=AF.Copy), reads=[("xn2f", pz)], writes=[("xn2b", pz)])
            P.dma("sp", I("dma_start", out=xn_v[:, T, :], in_=xn2b[pz][:]), reads=[("xn2b", pz)], writes=[("xn_s", T)], sem=f"xnst{pz}")

        def C4(T):
            pz = T % 2
            for kc in range(8):
                bk = 4 + kc // 4
                P.op("pe", I("transpose", out=psum[bk][:, (kc % 4) * 128:(kc % 4 + 1) * 128], in_=xn2f[pz][:, kc * 128:(kc + 1) * 128],
                             identity=ident_f[:]), reads=[("xn2f", pz), "ident_f"], writes=[PS[bk]])
            for half in range(2):
                P.op("act" if half else "dve",
                     I("activation", out=xnT[pz][:, 4 * half:4 * half + 4, :].rearrange("p a b -> p (a b)"), in_=psum[4 + half][:, :], func=AF.Copy)
                     if half else I("tensor_copy", out=xnT[pz][:, 0:4, :].rearrange("p a b -> p (a b)"), in_=psum[4][:, :]),
                     reads=[PS[4 + half]], writes=[("xnT", pz, half)])
            for kc in range(8):
                P.op("pe", I("matmul", psum[6][:, 0:36], lhsT=xnT[pz][:, kc, :], rhs=Wr[:, kc, :], start=(kc == 0), stop=False),
                     reads=[("xnT", pz, kc // 4), "Wr"], writes=[PS[6]])
            P.op("pe", I("matmul", psum[6][:, 0:36], lhsT=ones_f[0:1, :], rhs=brow[0:1, :], start=False, stop=True),
                 reads=["ones_f", "brow"], writes=[PS[6]])
            P.op("dve", I("tensor_copy", out=lg[pz][:], in_=psum[6][:, 0:36]), reads=[PS[6]], writes=[("lg", pz)])

        def C5(T):
            pz = T % 2
            R = ("rt", pz)
            P.op("dve", I("tensor_reduce", out=rt_[pz][:, 2:3], in_=lg[pz][:, 0:4], axis=AX.X, op=ALU.max), reads=[("lg", pz)], writes=[R])
            P.op("dve", I("tensor_scalar", out=rt_[pz][:, 3:4], in0=rt_[pz][:, 2:3], scalar1=-1.0, scalar2=None, op0=ALU.mult), reads=[R], writes=[R])
            P.op("act", I("activation", out=rt_[pz][:, 4:8], in_=lg[pz][:, 0:4], func=AF.Exp, bias=rt_[pz][:, 3:4], scale=1.0, accum_out=rt_[pz][:, 8:9]),
                 reads=[R, ("lg", pz)], writes=[R])
            P.op("dve", I("reciprocal", out=rt_[pz][:, 9:10], in_=rt_[pz][:, 8:9]), reads=[R], writes=[R])
            P.op("dve", I("tensor_scalar", out=rt_[pz][:, 10:14], in0=lg[pz][:, 0:4], scalar1=rt_[pz][:, 2:3], scalar2=None, op0=ALU.is_equal),
                 reads=[R, ("lg", pz)], writes=[R])
            lev = lg[pz][:, 4:36].rearrange("p (g e) -> p g e", g=4)
            P.op("dve", I("tensor_scalar", out=rt_[pz][:, 16:24], in0=lev[:, 0, :], scalar1=rt_[pz][:, 10:11], scalar2=None, op0=ALU.mult),
                 reads=[R, ("lg", pz)], writes=[R])
            for g in range(1, 4):
                P.op("dve", I("scalar_tensor_tensor", out=rt_[pz][:, 16:24], in0=lev[:, g, :], scalar=rt_[pz][:, 10 + g:11 + g],
                              in1=rt_[pz][:, 16:24], op0=ALU.mult, op1=ALU.add), reads=[R, ("lg", pz)], writes=[R])
            P.op("dve", I("tensor_tensor", out=rt_[pz][:, 24:28], in0=rt_[pz][:, 10:14], in1=iota32[:, 0:4], op=ALU.mult),
                 reads=[R, "iota32"], writes=[R])
            P.op("dve", I("tensor_reduce", out=rt_[pz][:, 28:29], in_=rt_[pz][:, 24:28], axis=AX.X, op=ALU.add), reads=[R], writes=[R])
            P.op("dve", I("max", out=t8[pz][:], in_=rt_[pz][:, 16:24]), reads=[R], writes=[("t8", pz)])
            P.op("dve", I("max_index", out=i8[pz][:], in_max=t8[pz][:], in_values=rt_[pz][:, 16:24]), reads=[R, ("t8", pz)], writes=[("i8", pz)])
            P.op("dve", I("tensor_copy", out=rt_[pz][:, 30:32], in_=i8[pz][:, 0:2]), reads=[("i8", pz)], writes=[R])
            for k in range(2):
                P.op("dve", I("scalar_tensor_tensor", out=rt_[pz][:, 32 + k:33 + k], in0=rt_[pz][:, 28:29], scalar=8.0, in1=rt_[pz][:, 30 + k:31 + k],
                              op0=ALU.mult, op1=ALU.add), reads=[R], writes=[R])
                P.op("dve", I("tensor_scalar", out=OH[:, k * NT + T, :], in0=iota32[:, 0:32], scalar1=rt_[pz][:, 32 + k:33 + k], scalar2=None,
                              op0=ALU.is_equal), reads=[R, "iota32"], writes=[("OH", k * NT + T)])
            P.op("dve", I("tensor_tensor", out=rt_[pz][:, 34:35], in0=t8[pz][:, 1:2], in1=t8[pz][:, 0:1], op=ALU.subtract), reads=[("t8", pz), R], writes=[R])
            P.op("act", I("activation", out=rt_[pz][:, 35:36], in_=rt_[pz][:, 34:35], func=AF.Exp), reads=[R], writes=[R])
            P.op("dve", I("tensor_scalar", out=rt_[pz][:, 36:37], in0=rt_[pz][:, 35:36], scalar1=1.0, scalar2=None, op0=ALU.add), reads=[R], writes=[R])
            P.op("dve", I("reciprocal", out=rt_[pz][:, 37:38], in_=rt_[pz][:, 36:37]), reads=[R], writes=[R])
            P.op("dve", I("tensor_tensor", out=wts[:, T, 0:1], in0=rt_[pz][:, 37:38], in1=rt_[pz][:, 9:10], op=ALU.mult), reads=[R], writes=[("wts", T)])
            P.op("dve", I("tensor_tensor", out=wts[:, T, 1:2], in0=wts[:, T, 0:1], in1=rt_[pz][:, 35:36], op=ALU.mult),
                 reads=[R, ("wts", T)], writes=[("wts", T)])

        for i in range(NT + 4):
            if i < NT:
                C1(i)
            if 0 <= i - 1 < NT:
                C2(i - 1)
            if 0 <= i - 2 < NT:
                C3(i - 2)
            if 0 <= i - 3 < NT:
                C4(i - 3)
            if 0 <= i - 4 < NT:
                C5(i - 4)

        if stage == "h1":
            dst = sb("dbg_st", [128, 1024])
            dv = dbg.ap().rearrange("(t p) c -> p t c", p=128)
            for t in range(NT):
                P.dma("sp", I("dma_start", out=dst[:], in_=h1_v[:, t, :]), reads=[("h1_s", t)], writes=["dbg_st"], sem="dbgl")
                P.dma("sp", I("dma_start", out=dv[:, t, 0:1024], in_=dst[:]), reads=["dbg_st"], writes=[("OUT", t)], sem="out")
            dst2 = sb("dbg_st2", [128, 2 * NT, 32])
            P.op("dve", I("tensor_copy", out=dst2[:], in_=OH[:]), reads=[("OH", t) for t in range(2 * NT)], writes=["dbg_st2"])
            P.dma("sp", I("dma_start", out=dv[:, 0, 1024:1024 + 64 * NT].rearrange("p (a b) -> p a b", b=32), in_=dst2[:]),
                  reads=["dbg_st2"], writes=[("OUT", "oh")], sem="out")
            P.dma("sp", I("dma_start", out=dv[:, 1, 1024:1024 + 2 * NT].rearrange("p (a b) -> p a b", b=2), in_=wts[:]),
                  reads=[("wts", t) for t in range(NT)], writes=[("OUT", "w")], sem="out")
            return finish()
        P.barrier()
        sT.close()

        sD = ExitStack()
        st.enter_context(sD)
        NTT = 2 * NT
        HB = NTT // 2
        assert HB * 32 <= 512
        cnt_f = sbt(sD, "cnt_f", [128, 32])
        cnt_i = sbt(sD, "cnt_i", [128, 32], I32)
        pad_f = sbt(sD, "pad_f", [128, 32])
        pend = sbt(sD, "pend", [128, 32])
        pstart = sbt(sD, "pstart", [128, 32])
        dtmp = sbt(sD, "dtmp", [128, HB, 32])
        cmpb = sbt(sD, "cmpb", [128, NSB, 32])
        thr = sbt(sD, "thr", [128, NSB])
        be_f = sbt(sD, "be_f", [128, NSB])
        idxf = sbt(sD, "idxf", [128, NSB])
        OHK = [("OH", t) for t in range(NTT)]
        for t in range(NTT):
            bk = t // HB
            cs = slice((t % HB) * 32, (t % HB) * 32 + 32)
            for t2 in range(t):
                P.op("pe", I("matmul", psum[bk][:, cs], lhsT=ones_bf[:, :], rhs=OH[:, t2, :], start=(t2 == 0), stop=False,
                             skip_group_check=True), reads=[OHK[t2], "ones_bf"], writes=[PS[bk]])
            P.op("pe", I("matmul", psum[bk][:, cs], lhsT=Ustr[:, :], rhs=OH[:, t, :], start=(t == 0), stop=True, skip_group_check=True),
                 reads=[OHK[t], "Ustr"], writes=[PS[bk]])
        for t in range(NTT):
            P.op("pe", I("matmul", psum[2][:, 0:32], lhsT=ones_bf[:, :], rhs=OH[:, t, :], start=(t == 0), stop=(t == NTT - 1)),
                 reads=[OHK[t], "ones_bf"], writes=[PS[2]])
        P.op("dve", I("tensor_scalar", out=cnt_i[:], in0=psum[2][:, 0:32], scalar1=255.0, scalar2=None, op0=ALU.add),
             reads=[PS[2]], writes=["cnt_i"])
        P.op("dve", I("tensor_scalar", out=cnt_i[:], in0=cnt_i[:], scalar1=8, scalar2=8, op0=ALU.arith_shift_right,
                      op1=ALU.logical_shift_left), reads=["cnt_i"], writes=["cnt_i2"])
        P.op("dve", I("tensor_copy", out=pad_f[:], in_=cnt_i[:]), reads=["cnt_i2"], writes=["pad_f"])
        P.op("dve", I("tensor_tensor_scan", out=pend[:], data0=ones_f[:, 0:32], data1=pad_f[:], initial=0.0, op0=ALU.mult, op1=ALU.add),
             reads=["pad_f", "ones_f"], writes=["pend"])
        P.op("dve", I("tensor_tensor", out=pstart[:], in0=pend[:], in1=pad_f[:], op=ALU.subtract), reads=["pend", "pad_f"], writes=["pstart"])
        for bk in range(2):
            P.op("dve", I("tensor_tensor", out=dtmp[:], in0=psum[bk][:, 0:HB * 32].rearrange("p (a b) -> p a b", b=32),
                          in1=pstart[:, 0:32].unsqueeze(1).to_broadcast([128, HB, 32]), op=ALU.add),
                 reads=[PS[bk], "pstart"], writes=["dtmp"])
            P.op("dve", I("tensor_tensor", out=dtmp[:], in0=dtmp[:], in1=OH[:, bk * HB:(bk + 1) * HB, :], op=ALU.mult),
                 reads=["dtmp"] + OHK, writes=["dtmp"])
            P.op("dve", I("tensor_reduce", out=dest_f[:, bk * HB:(bk + 1) * HB], in_=dtmp[:], axis=AX.X, op=ALU.add),
                 reads=["dtmp"], writes=[("dest_f", bk)])
        P.op("dve", I("tensor_copy", out=dest_i[:], in_=dest_f[:]), reads=[("dest_f", 0), ("dest_f", 1)], writes=["dest_i"])
        P.op("pool", I("iota", iot2[:, 0:NSB], pattern=[[256, NSB]], base=0, channel_multiplier=0), writes=["iot2"])
        P.op("dve", I("tensor_copy", out=thr[:], in_=iot2[:, 0:NSB]), reads=["iot2"], writes=["thr"])
        P.op("dve", I("tensor_tensor", out=cmpb[:], in0=pend[:, 0:32].unsqueeze(1).to_broadcast([128, NSB, 32]),
                      in1=thr[:, 0:NSB].unsqueeze(2).to_broadcast([128, NSB, 32]), op=ALU.is_le),
             reads=["pend", "thr"], writes=["cmpb"])
        P.op("dve", I("tensor_reduce", out=be_f[:], in_=cmpb[:], axis=AX.X, op=ALU.add), reads=["cmpb"], writes=["be_f"])
        P.op("pool", I("iota", iot2[:, 0:1], pattern=[[0, 1]], base=0, channel_multiplier=1), reads=["thr"], writes=["iot2"])
        P.op("dve", I("tensor_copy", out=thr[:, 0:1], in_=iot2[:, 0:1]), reads=["iot2", "cmpb"], writes=["thr"])
        P.op("dve", I("scalar_tensor_tensor", out=idxf[:],
                      in0=be_f[:], scalar=128.0, in1=thr[:, 0:1].to_broadcast([128, NSB]), op0=ALU.mult, op1=ALU.add),
             reads=["be_f", "thr"], writes=["idxf"])
        P.op("dve", I("tensor_copy", out=idxw[:], in_=idxf[:]), reads=["idxf"], writes=["idxw"])
        P.op("dve", I("tensor_scalar", out=be_f[:], in0=be_f[:], scalar1=float(NEXP - 1), scalar2=None, op0=ALU.min),
             reads=["be_f"], writes=["be_f2"])
        P.op("dve", I("tensor_copy", out=be_i[:], in_=be_f[:]), reads=["be_f2"], writes=["be_i"])

        if stage == "disp":
            dst = sb("dbg_st", [128, 256])
            dv = dbg.ap().rearrange("(t p) c -> p t c", p=128)
            P.op("dve", I("tensor_copy", out=dst[:, 0:NTT], in_=dest_f[:]), reads=[("dest_f", 0), ("dest_f", 1)], writes=["dbg_st"])
            P.op("dve", I("tensor_copy", out=dst[:, 64:64 + NSB], in_=be_f[:]), reads=["be_f2"], writes=["dbg_st"])
            P.op("dve", I("tensor_copy", out=dst[:, 160:192], in_=pend[:]), reads=["pend"], writes=["dbg_st"])
            P.dma("sp", I("dma_start", out=dv[:, 0, 0:256], in_=dst[:]), reads=["dbg_st"], writes=[("OUT", 0)], sem="out")
            return finish()

        xs_rows = xs_s.ap()
        sE = ExitStack()
        st.enter_context(sE)
        xsc = [sbt(sE, f"xsc{i}", [128, D], BF16) for i in range(2)]
        for T in range(NT):
            sl = T % 2
            P.dma("sp", I("dma_start", out=xsc[sl][:], in_=xn_v[:, T, :]), reads=[("xn_s", T)], writes=[("xsc", sl)], sem=f"xsc{sl}")
            for k in range(2):
                t = k * NT + T
                P.dma("pool", I("indirect_dma_start", out=xs_rows[:, :],
                                out_offset=bass.IndirectOffsetOnAxis(ap=dest_i[:, t:t + 1], axis=0),
                                in_=xsc[sl][:, :], in_offset=None),
                      reads=[("xsc", sl), "dest_i"] + XSZ, writes=[("xs_sc", t)], sem=f"scat{sl}{k}")

        wgb = [sbt(sE, f"wgb{i}", [128, 8, HID], BF16) for i in range(2)]
        wub = [sbt(sE, f"wub{i}", [128, 8, HID], BF16) for i in range(2)]
        wdb = [sbt(sE, f"wdb{i}", [128, 4, D], BF16) for i in range(2)]
        xsb = [sbt(sE, f"xsb{i}", [128, D], BF16) for i in range(2)]
        xsT = [sbt(sE, f"xsT{i}", [128, 8, 128], BF16) for i in range(2)]
        sgt = [sbt(sE, f"sgt{i}", [128, HID]) for i in range(2)]
        hb = [sbt(sE, f"hb{i}", [128, HID], BF16) for i in range(2)]
        hT = [sbt(sE, f"hT{i}", [128, 4, 128], BF16) for i in range(2)]
        ysb = [sbt(sE, f"ysb{i}", [128, D]) for i in range(2)]

        def wload(sb_, which):
            if sb_ >= NSB:
                return
            sl = sb_ % 2
            for (dst_t, src_t, nm) in which:
                def gth(e, dst_ap=dst_t[sl][:, :, :].rearrange("p k n -> p (k n)"), src_ap=src_t.ap()[:, :], ix=idxw[:, sb_:sb_ + 1]):
                    return e.indirect_dma_start(out=dst_ap, out_offset=None, in_=src_ap,
                                                in_offset=bass.IndirectOffsetOnAxis(ap=ix, axis=0),
                                                bounds_check=P.bcreg, oob_is_err=False)
                P.dma("pool", gth, reads=["idxw"], writes=[(nm, sl)], sem=f"{nm}{sl}")

        WGU = ((wgb, wg, "wgb"), (wub, wu, "wub"))
        WD = ((wdb, wd, "wdb"),)

        def xload(b):
            sl = b % 2
            P.dma("sp", I("dma_start", out=xsb[sl][:], in_=xs_rows[b * 128:(b + 1) * 128, :]),
                  reads=[("xs_sc", t_) for t_ in range(2 * NT)], writes=[("xsb", sl)], sem=f"xsb{sl}")

        def S1(b):
            sl = b % 2
            for kc in range(8):
                P.op("pe", I("transpose", out=psbf[sl][:, kc * 128:(kc + 1) * 128], in_=xsb[sl][:, kc * 128:(kc + 1) * 128],
                             identity=ident_bf[:]), reads=[("xsb", sl), "ident_bf"], writes=[PS[sl]])
            P.op("act", I("activation", out=xsT[sl][:].rearrange("p a b -> p (a b)"), in_=psbf[sl][:, :], func=AF.Copy),
                 reads=[PS[sl]], writes=[("xsT", sl)])

        def S2(b):
            sl = b % 2
            for kc in range(8):
                P.op("pe", I("matmul", psum[2][:, :], lhsT=xsT[sl][:, kc, :], rhs=wgb[(b // 2) % 2][:, kc, :], start=(kc == 0), stop=(kc == 7)),
                     reads=[("xsT", sl), ("wgb", (b // 2) % 2)], writes=[PS[2]])
            for kc in range(8):
                P.op("pe", I("matmul", psum[3][:, :], lhsT=xsT[sl][:, kc, :], rhs=wub[(b // 2) % 2][:, kc, :], start=(kc == 0), stop=(kc == 7)),
                     reads=[("xsT", sl), ("wub", (b // 2) % 2)], writes=[PS[3]])
            P.op("act", I("activation", out=sgt[sl][:], in_=psum[2][:, :], func=AF.Silu), reads=[PS[2]], writes=[("sgt", sl)])
            P.op("dve", I("tensor_tensor", out=hb[sl][:], in0=psum[3][:, :], in1=sgt[sl][:], op=ALU.mult),
                 reads=[PS[3], ("sgt", sl)], writes=[("hb", sl)])

        def S3(b):
            sl = b % 2
            for hc in range(4):
                P.op("pe", I("transpose", out=psbf[6][:, hc * 128:(hc + 1) * 128], in_=hb[sl][:, hc * 128:(hc + 1) * 128],
                             identity=ident_bf[:]), reads=[("hb", sl), "ident_bf"], writes=[PS[6]])
            P.op("dve", I("tensor_copy", out=hT[sl][:].rearrange("p a b -> p (a b)"), in_=psbf[6][:, 0:512]), reads=[PS[6]],
                 writes=[("hT", sl)])

        def S4(b):
            sl = b % 2
            for half in range(2):
                for hc in range(4):
                    P.op("pe", I("matmul", psum[4 + half][:, :], lhsT=hT[sl][:, hc, :], rhs=wdb[(b // 2) % 2][:, hc, half * 512:(half + 1) * 512],
                                 start=(hc == 0), stop=(hc == 3)), reads=[("hT", sl), ("wdb", (b // 2) % 2)], writes=[PS[4 + half]])
                if half == 0:
                    P.op("act", I("activation", out=ysb[sl][:, 0:512], in_=psum[4][:, :], func=AF.Copy), reads=[PS[4]], writes=[("ysb", sl, 0)])
                else:
                    P.op("dve", I("tensor_copy", out=ysb[sl][:, 512:1024], in_=psum[5][:, :]), reads=[PS[5]], writes=[("ysb", sl, 1)])
            P.dma("sp", I("dma_start", out=ys_s.ap()[b * 128:(b + 1) * 128, :], in_=ysb[sl][:]),
                  reads=[("ysb", sl, 0), ("ysb", sl, 1)], writes=[("ys_s", b)], sem=f"ysst{sl}")

        for b in range(min(2, NBLK)):
            xload(b)
        for sb_ in range(2):
            wload(sb_, WGU)
            wload(sb_, WD)
        for i in range(NBLK + 3):
            if i < NBLK:
                S1(i)
                if i + 2 < NBLK:
                    xload(i + 2)
            if 0 <= i - 1 < NBLK:
                S2(i - 1)
                if (i - 1) % 2 == 1:
                    wload((i - 1) // 2 + 2, WGU)
            if 0 <= i - 2 < NBLK:
                S3(i - 2)
            if 0 <= i - 3 < NBLK:
                S4(i - 3)
                if (i - 3) % 2 == 1:
                    wload((i - 3) // 2 + 2, WD)

        P.barrier()
        sE.close()
        y0 = [sbt(sD, f"y0_{i}", [128, D]) for i in range(2)]
        y1 = [sbt(sD, f"y1_{i}", [128, D]) for i in range(2)]
        hh_ = [sbt(sD, f"hh_{i}", [128, D]) for i in range(2)]
        oo = [sbt(sD, f"oo{i}", [128, D]) for i in range(2)]
        junk2 = sbt(sD, "junk2", [128, D], BF16)
        fr = sbt(sD, "fr", [128, 4])
        for T in range(NT):
            sl = T % 2
            P.dma("sp", I("dma_start", out=hh_[sl][:], in_=h1_v[:, T, :]), reads=[("h1_s", T)], writes=[("hh", sl)], sem=f"hh{sl}")
            for k, yb in ((0, y0), (1, y1)):
                t = k * NT + T
                P.dma("pool", I("indirect_dma_start", out=yb[sl][:, :], out_offset=None, in_=ys_s.ap()[:, :],
                                in_offset=bass.IndirectOffsetOnAxis(ap=dest_i[:, t:t + 1], axis=0)),
                      reads=[("ys_s", b_) for b_ in range(NBLK)] + ["dest_i"], writes=[("y", k, sl)], sem=f"yg{k}{sl}")
            P.op("dve", I("scalar_tensor_tensor", out=oo[sl][:], in0=y0[sl][:], scalar=wts[:, T, 0:1], in1=hh_[sl][:], op0=ALU.mult, op1=ALU.add),
                 reads=[("y", 0, sl), ("hh", sl), ("wts", T)], writes=[("oo", sl)])
            P.op("dve", I("scalar_tensor_tensor", out=oo[sl][:], in0=y1[sl][:], scalar=wts[:, T, 1:2], in1=oo[sl][:], op0=ALU.mult, op1=ALU.add),
                 reads=[("y", 1, sl), ("oo", sl), ("wts", T)], writes=[("oo", sl)])
            P.op("act", I("activation", out=junk2[:], in_=oo[sl][:], func=AF.Square, accum_out=fr[:, 0:1]), reads=[("oo", sl)],
                 writes=["junk2", "fr0"])
            rstd_from(fr[:, 0:1], fr[:, 1:2], 1, "fr0", "fr1", 1.0 / D)
            P.op("dve", I("scalar_tensor_tensor", out=oo[sl][:], in0=oo[sl][:], scalar=fr[:, 1:2], in1=gfbc[:], op0=ALU.mult, op1=ALU.mult),
                 reads=[("oo", sl), "fr1", ("gfbc", 0), ("gfbc", 1)], writes=[("oo", sl)])
            P.dma("sp", I("dma_start", out=out_v[:, T, :], in_=oo[sl][:]), reads=[("oo", sl)], writes=[("OUT", T)], sem=f"out{sl}")
        return finish()


def make_inputs(inputs, S):
    x = np.asarray(inputs["x"], np.float32).reshape(S, D)
    xT = np.ascontiguousarray(x.T)
    common = dict(
        xT=xT,
        w_in=np.ascontiguousarray(inputs["w_in"][0]),
        g1=np.ascontiguousarray(np.asarray(inputs["attn_norm_g"][0]).reshape(8, 128).T),
        lamv=np.stack([inputs["da_lambda_q1"][0], inputs["da_lambda_k1"][0], inputs["da_lambda_q2"][0],
                       inputs["da_lambda_k2"][0]])[None].astype(np.float32),
        w_out=np.ascontiguousarray(inputs["w_out"][0]),
        sg=np.ascontiguousarray(np.asarray(inputs["da_subln_g"][0]).reshape(128, 1)),
        g2=np.asarray(inputs["ffn_norm_g"][0]).reshape(1, D),
        gf=np.asarray(inputs["final_norm_g"]).reshape(1, D),
        wr=np.ascontiguousarray(np.concatenate([inputs["router_group_w"][0], inputs["router_expert_w"][0]], axis=1)),
        br=np.concatenate([inputs["router_group_b"][0], inputs["router_expert_b"][0]])[None].astype(np.float32),
        wg=np.ascontiguousarray(np.asarray(inputs["expert_w_gate"][0]).reshape(NEXP, 8, 128, HID).transpose(0, 2, 1, 3)).reshape(NEXP * 128, 8 * HID),
        wu=np.ascontiguousarray(np.asarray(inputs["expert_w_up"][0]).reshape(NEXP, 8, 128, HID).transpose(0, 2, 1, 3)).reshape(NEXP * 128, 8 * HID),
        wd=np.ascontiguousarray(np.asarray(inputs["expert_w_down"][0]).reshape(NEXP, 4, 128, D).transpose(0, 2, 1, 3)).reshape(NEXP * 128, 4 * D),
    )
    maps = []
    own = []
    for c in range(NCORES):
        i = np.arange(S // 8)
        pos = 64 * (i // 8) + 8 * c + (i % 8)
        own.append(pos)
        m = dict(common)
        m["xo"] = np.ascontiguousarray(x[pos])
        m["xoT"] = np.ascontiguousarray(xT[:, pos])
        m.update(host_tables(c, S))
        maps.append({k: np.ascontiguousarray(v, dtype=np.float32) for k, v in m.items()})
    return maps, own


_NC_CACHE = {}


def kernel(**inputs):
    S = inputs["x"].shape[1]
    if S not in _NC_CACHE:
        _NC_CACHE[S] = build(S, "full")
    nc = _NC_CACHE[S]
    maps, own = make_inputs(inputs, S)
    res = run_bass_kernel_spmd(nc, maps, core_ids=list(range(NCORES)))
    outp = np.zeros((S, D), np.float32)
    for c in range(NCORES):
        outp[own[c]] = res.results[c]["out"]
    return outp.reshape(1, S, D)
```

```python
import math
from contextlib import ExitStack

import numpy as np
import concourse.bass as bass
import concourse.mybir as mybir
from concourse.bass_utils import run_bass_kernel_spmd

F32 = mybir.dt.float32
BF16 = mybir.dt.bfloat16
I32 = mybir.dt.int32
U32 = mybir.dt.uint32
ALU = mybir.AluOpType
AF = mybir.ActivationFunctionType
AX = mybir.AxisListType

NCORES = 8
D = 1024
H = 4
EPS = 1e-6
NEXP = 32
HID = 512
LAMBDA_INIT = 0.8 - 0.6 * math.exp(-0.3 * 0)
ALIBI_CUT = 134.0


class Prog:
    ENG = ("pe", "act", "dve", "pool", "sp")

    def __init__(self, nc, stack):
        self.nc = nc
        self.stack = stack
        self.sem = {e: stack.enter_context(nc.semaphore("sem_" + e)) for e in self.ENG}
        self.cnt = {e: 0 for e in self.ENG}
        self.stream = {e: [] for e in self.ENG}
        self.seen = {e: {} for e in self.ENG}
        self.dsem = {}
        self.dcnt = {}
        self.lastw = {}
        self.readers = {}
        self.bc_value = 0
        self.bcreg = None

    def _dma_sem(self, name):
        if name not in self.dsem:
            self.dsem[name] = self.stack.enter_context(self.nc.semaphore("dma_" + name))
            self.dcnt[name] = 0
        return self.dsem[name]

    def _need(self, eng, tok):
        if tok is None:
            return
        kind, name, val = tok
        if kind == "eng" and name == eng and eng == "pe":
            return
        key = (kind, name)
        if self.seen[eng].get(key, 0) >= val:
            return
        self.seen[eng][key] = val
        sem = self.sem[name] if kind == "eng" else self.dsem[name]
        self.stream[eng].append(("wait", sem, val))

    def _deps(self, eng, reads, writes):
        toks = []
        for k in reads:
            toks.append(self.lastw.get(k))
        for k in writes:
            toks.append(self.lastw.get(k))
            toks.extend(self.readers.get(k, ()))
        best = {}
        for t in toks:
            if t is None:
                continue
            key = (t[0], t[1])
            if key not in best or best[key][2] < t[2]:
                best[key] = t
        for t in best.values():
            self._need(eng, t)

    def _commit(self, tok, reads, writes):
        for k in reads:
            self.readers.setdefault(k, []).append(tok)
        for k in writes:
            self.lastw[k] = tok
            self.readers[k] = []

    def op(self, eng, fn, reads=(), writes=()):
        self._deps(eng, reads, writes)
        self.cnt[eng] += 1
        tok = ("eng", eng, self.cnt[eng])
        self.stream[eng].append(("inst", fn, self.sem[eng], 1))
        self._commit(tok, reads, writes)

    def dma(self, eng, fn, reads=(), writes=(), sem=None):
        assert eng in ("sp", "pool", "act")
        name = sem
        s = self._dma_sem(name)
        self._deps(eng, reads, writes)
        self.dcnt[name] += 16
        tok = ("dma", name, self.dcnt[name])
        self.stream[eng].append(("inst", fn, s, 16))
        self._commit(tok, reads, writes)

    def raw(self, eng, fn, reads=(), writes=()):
        self._deps(eng, reads, writes)
        self.stream[eng].append(("raw", fn))

    def barrier(self):
        toks = [("eng", e, self.cnt[e]) for e in self.ENG if self.cnt[e] > 0]
        toks += [("dma", n, c) for n, c in self.dcnt.items() if c > 0]
        for e in self.ENG:
            for t in toks:
                self._need(e, t)

    def finish_waits(self, eng="sp"):
        best = {}
        for k, tok in list(self.lastw.items()):
            if k[0] == "OUT":
                key = (tok[0], tok[1])
                if key not in best or best[key][2] < tok[2]:
                    best[key] = tok
        for tok in best.values():
            self._need(eng, tok)

    def emit(self, block):
        nc = self.nc
        engobj = {"pe": "tensor", "act": "scalar", "dve": "vector", "pool": "gpsimd", "sp": "sync"}

        def mk(ename):
            def body(e):
                if ename == "pool":
                    self.bcreg = e.to_reg(self.bc_value)
                    run(e)
                else:
                    run(e)

            def run(e):
                for it in self.stream[ename]:
                    if it[0] == "wait":
                        e.wait_ge(it[1], it[2])
                    elif it[0] == "raw":
                        it[1](e)
                    else:
                        it[1](e).then_inc(it[2], it[3])
            return body

        for ename in self.ENG:
            getattr(block, engobj[ename])(mk(ename))


def I(name, *args, **kw):
    return lambda e: getattr(e, name)(*args, **kw)


def host_tables(c, S):
    NO = S // 8
    slopes = np.exp2(-8.0 * np.arange(1, H + 1) / H)
    i = np.arange(NO)
    pos = 64 * (i // 8) + 8 * c + (i % 8)
    qrel = pos % 4096
    a = (qrel // 64) * 64
    b = qrel % 64
    qaug = np.zeros((2, H, NO), np.float32)
    for h in range(H):
        qaug[0, h] = -8.0 * slopes[h] * a
        qaug[1, h] = -8.0 * slopes[h] * b
    p = np.arange(128)
    biask = np.zeros((128, H, 128), np.float32)
    for h in range(H):
        for idx in range(128):
            biask[:, h, idx] = slopes[h] * (128 * (idx - 96) + p)
    fix = np.zeros((128, H, 16), np.float32)
    for col in range(16):
        u, v = col // 8, col % 8
        qt = 64 * u + 8 * c + v
        for h in range(H):
            for pp in range(128):
                if pp // 64 > u:
                    fix[pp, h, col] = -30000.0
                elif pp > qt:
                    fix[pp, h, col] = -2.0 * slopes[h] * (pp - qt) * 8.0
    lg = np.log1p(-np.exp2(-5.0 - np.arange(H, dtype=np.float64)))
    s = np.arange(64)
    v8 = 8 * c + np.arange(8)
    kdec = np.zeros((128, H, 64), np.float32)
    for h in range(H):
        kd = np.exp(lg[h] * (63 - s)) * 0.125
        kdec[:, h, :] = np.concatenate([kd, kd])[:, None]
    dt = np.zeros((128, 2, 2, 8), np.float32)
    qdec8 = np.zeros((128, 2, 8), np.float32)
    sdec = np.zeros((128, 2), np.float32)
    for h in range(H):
        rel = v8[None, :] - s[:, None]
        dd = np.where(rel >= 0, np.exp(lg[h] * np.maximum(rel, 0)), 0.0) * 0.125
        hh, grp = h % 2, h // 2
        dt[:, hh, grp, :] = np.concatenate([dd, dd], 0)
        qdec8[hh * 64:(hh + 1) * 64, grp, :] = np.exp(lg[h] * (v8 + 1.0))[None, :]
        sdec[hh * 64:(hh + 1) * 64, grp] = np.exp(lg[h] * 64)
    return dict(qaug=qaug, biask=biask, fix=fix, kdec=kdec.reshape(128, H * 64), dt=dt,
                qdec8=qdec8, sdec=sdec)


def build(S, stage="full"):
    NO = S // 8
    NT = NO // 128
    NB = S // 512
    NG = S // 4096
    NSB = NT + NEXP
    NBLK = 2 * NSB
    assert NG >= 1
    nc = bass.Bass("TRN2", target_bir_lowering=False)
    st = ExitStack()
    with st:
        def din(name, shape, dt=F32):
            return nc.dram_tensor(name, list(shape), dt, kind="ExternalInput")

        xT = din("xT", [D, S])
        xoT = din("xoT", [D, NO])
        xo = din("xo", [NO, D])
        w_in = din("w_in", [D, 3072])
        g1 = din("g1", [128, 8])
        qaug_d = din("qaug", [2, H, NO])
        biask_d = din("biask", [128, H, 128])
        fix_d = din("fix", [128, H, 16])
        kdec_d = din("kdec", [128, H * 64])
        dt_d = din("dt", [128, 2, 2, 8])
        qdec8_d = din("qdec8", [128, 2, 8])
        sdec_d = din("sdec", [128, 2])
        lamv = din("lamv", [1, 4, 64])
        w_out = din("w_out", [D, D])
        sg = din("sg", [128, 1])
        g2 = din("g2", [1, D])
        gf = din("gf", [1, D])
        wr = din("wr", [D, 36])
        br = din("br", [1, 36])
        wg = din("wg", [NEXP * 128, 8 * HID])
        wu = din("wu", [NEXP * 128, 8 * HID])
        wd = din("wd", [NEXP * 128, 4 * D])
        out = nc.dram_tensor("out", [NO, D], F32, kind="ExternalOutput")
        dbg = None
        if stage != "full":
            dbg = nc.dram_tensor("dbg", [NO, 2048], F32, kind="ExternalOutput")

        kT_s = nc.dram_tensor("kT_s", [H, 2, 64, S], BF16)
        v_s = nc.dram_tensor("v_s", [S, 512], BF16)
        h1_s = nc.dram_tensor("h1_s", [NO, D], F32)
        xn_s = nc.dram_tensor("xn_s", [NO, D], BF16)
        xs_s = nc.dram_tensor("xs_s", [NBLK * 128, D], BF16)
        ys_s = nc.dram_tensor("ys_s", [NBLK * 128, D], F32)

        P = Prog(nc, st)
        P.bc_value = NEXP * 128 - 1

        def sbt(stack, name, shape, dt=F32):
            return stack.enter_context(nc.sbuf_tensor(name, list(shape), dt))

        def sb(name, shape, dt=F32):
            return sbt(st, name, shape, dt)

        def finish():
            P.finish_waits("sp")
            with nc.Block() as block:
                P.emit(block)
            return nc

        ones_bf = sb("ones_bf", [128, 128], BF16)
        ones_f = sb("ones_f", [128, 128], F32)
        ident_bf = sb("ident_bf", [128, 128], BF16)
        ident_f = sb("ident_f", [128, 128], F32)
        iotf = sb("iotf", [128, 128])
        g1_sb = sb("g1_sb", [128, 8])
        biask = sb("biask_sb", [128, H, 128])
        fix = sb("fix_sb", [128, H, 16])
        kdec = sb("kdec_sb", [128, H * 64])
        dtt = sb("dt_sb", [128, 2, 2, 8])
        qdec8 = sb("qdec8_sb", [128, 2, 8])
        sdec = sb("sdec_sb", [128, 2])
        epsc = sb("epsc", [128, 1])
        gsl = sb("gsl", [128, NT, 512], BF16)
        ret = sb("ret", [128, NT, 512], BF16)
        att = sb("att", [128, NT, 8, 128], BF16)
        psum = [st.enter_context(nc.psum_tensor(f"ps{i}", [128, 512], F32)) for i in range(8)]
        PS = [("ps", i) for i in range(8)]

        P.op("pool", I("memset", ones_bf[:], 1.0), writes=["ones_bf"])
        P.op("pool", I("memset", ones_f[:], 1.0), writes=["ones_f"])
        P.op("pool", I("memset", epsc[:], EPS), writes=["epsc"])
        iot = sb("iot", [128, 128], I32)
        P.op("pool", I("iota", iot[:], pattern=[[1, 128]], base=0, channel_multiplier=-1), writes=["iot"])
        P.op("dve", I("tensor_copy", out=iotf[:], in_=iot[:]), reads=["iot"], writes=["iotf"])
        P.op("dve", I("tensor_single_scalar", out=ident_f[:], in_=iotf[:], scalar=0.0, op=ALU.is_equal),
             reads=["iotf"], writes=["ident_f"])
        P.op("dve", I("tensor_copy", out=ident_bf[:], in_=ident_f[:]), reads=["ident_f"], writes=["ident_bf"])
        for (dst, src, key) in ((g1_sb, g1, "g1"), (biask, biask_d, "biask"), (fix, fix_d, "fix"),
                                (kdec, kdec_d, "kdec"), (dtt, dt_d, "dt"), (qdec8, qdec8_d, "qdec8"),
                                (sdec, sdec_d, "sdec")):
            P.dma("sp", I("dma_start", out=dst[:], in_=src.ap()), writes=[key], sem="c_" + key)

        stM = ExitStack()
        st.enter_context(stM)
        qda = sbt(stM, "qda", [128, 8, NO], BF16)
        qr = sbt(stM, "qr", [128, 2, NO], BF16)
        for h in range(H):
            for m in range(2):
                P.dma("pool", I("dma_start", out=qda[64:66, 2 * h + m, :], in_=qaug_d.ap()[:, h, :]),
                      writes=[("qda", 2 * h + m, "aug")], sem=f"c_qa{2 * h + m}")

        w_in_v = w_in.ap().rearrange("(k p) c -> p k c", p=128)

        def load_w(stack, name, ranges):
            ncols = sum(r[1] - r[0] for r in ranges)
            W = sbt(stack, name, [128, 8, ncols], BF16)
            stg = [sbt(stack, f"{name}_st{i}", [128, 512]) for i in range(2)]
            cnt = 0
            d0 = 0
            for (a0, a1) in ranges:
                for c0 in range(a0, a1, 512):
                    c1 = min(a1, c0 + 512)
                    for kc in range(8):
                        sl = cnt % 2
                        cnt += 1
                        key = (name + "_st", sl)
                        P.dma("sp", I("dma_start", out=stg[sl][:, 0:c1 - c0], in_=w_in_v[:, kc, c0:c1]),
                              writes=[key], sem=f"{name}_st{sl}")
                        P.op("dve", I("tensor_scalar", out=W[:, kc, d0:d0 + c1 - c0], in0=stg[sl][:, 0:c1 - c0],
                                      scalar1=g1_sb[:, kc:kc + 1], scalar2=None, op0=ALU.mult),
                             reads=[key, "g1"], writes=[(name, kc)])
                    d0 += c1 - c0
            return W, [(name, kc) for kc in range(8)]

        def mk_front(stack, tag, nsq=1):
            xb = [sbt(stack, f"xb{tag}{i}", [128, 8, 512], BF16) for i in range(2)]
            sqs = [sbt(stack, f"sq{tag}{i}", [128, 8, 512], BF16) for i in range(nsq)]
            rbc = [sbt(stack, f"rbc{tag}{i}", [128, 512]) for i in range(2)]
            rtk = [sbt(stack, f"rtk{tag}{i}", [128, 4]) for i in range(2)]
            lnb = sbt(stack, f"lnb{tag}", [128, 512])

            issued = set()

            def front_load(src_ap_T, blk, par):
                if blk in issued:
                    return
                issued.add(blk)
                xbk = ("xb" + tag, par)
                xv = src_ap_T.rearrange("(k p) t -> p k t", p=128)
                for half in range(2):
                    P.dma("pool", I("dma_start", out=xb[par][:, 4 * half:4 * half + 4, :],
                                    in_=xv[:, 4 * half:4 * half + 4, blk * 512:(blk + 1) * 512]),
                          writes=[(xbk, half)], sem=f"xb{tag}{par}_{half}")

            done_sq, done_st = set(), set()

            def front_sq(src_ap_T, blk, par):
                if blk in done_sq:
                    return
                done_sq.add(blk)
                front_load(src_ap_T, blk, par)
                xbk = ("xb" + tag, par)
                sq, sqk = sqs[blk % nsq], ("sq" + tag, blk % nsq)
                for half in range(2):
                    P.op("pool" if half else "dve", I("tensor_tensor", out=sq[:, 4 * half:4 * half + 4, :],
                                                      in0=xb[par][:, 4 * half:4 * half + 4, :],
                                                      in1=xb[par][:, 4 * half:4 * half + 4, :], op=ALU.mult),
                         reads=[(xbk, half)], writes=[(sqk, half)])

            def front_stats(src_ap_T, blk, par):
                if blk in done_st:
                    return
                done_st.add(blk)
                front_sq(src_ap_T, blk, par)
                sq, sqk = sqs[blk % nsq], ("sq" + tag, blk % nsq)
                for kc in range(8):
                    P.op("pe", I("matmul", psum[7][:, :], lhsT=ones_bf[:, :], rhs=sq[:, kc, :], start=(kc == 0), stop=(kc == 7)),
                         reads=[(sqk, kc // 4), "ones_bf"], writes=[PS[7]])
                P.op("act", I("activation", out=lnb[:], in_=psum[7][:, :], func=AF.Ln, bias=epsc[:, 0:1], scale=1.0 / D),
                     reads=[PS[7], "epsc"], writes=["lnb" + tag])
                P.op("act", I("activation", out=rbc[par][:], in_=lnb[:], func=AF.Exp, scale=-0.5),
                     reads=["lnb" + tag], writes=[("rbc" + tag, par)])
                for tt in range(4):
                    for kc in range(8):
                        P.op("pe", I("matmul", psum[7][:, tt:tt + 1], lhsT=sq[:, kc, tt * 128:(tt + 1) * 128], rhs=ones_bf[:, 0:1],
                                     start=(kc == 0 and tt == 0), stop=(kc == 7), skip_group_check=True),
                             reads=[(sqk, kc // 4), "ones_bf"], writes=[PS[7]])
                P.op("act", I("activation", out=lnb[:, 0:4], in_=psum[7][:, 0:4], func=AF.Ln, bias=epsc[:, 0:1], scale=1.0 / D),
                     reads=[PS[7], "epsc"], writes=["lnb" + tag])
                P.op("act", I("activation", out=rtk[par][:], in_=lnb[:, 0:4], func=AF.Exp, scale=-0.5),
                     reads=["lnb" + tag], writes=[("rtk" + tag, par)])

            def front(src_ap_T, blk, par, nblk=None):
                front_stats(src_ap_T, blk, par)
                xbk = ("xb" + tag, par)
                return [(xbk, 0), (xbk, 1)]
            front.sq = front_sq
            front.stats = front_stats
            front.load = front_load
            return xb, rbc, rtk, front

        pa = 0

        def proj(XB, WK, lhsT_fn, rhs_fn, M, N):
            nonlocal pa
            pb, pk = psum[pa % 2], PS[pa % 2]
            pa += 1
            for kc in range(8):
                P.op("pe", I("matmul", pb[0:M, 0:N], lhsT=lhsT_fn(kc), rhs=rhs_fn(kc), start=(kc == 0), stop=(kc == 7)),
                     reads=XB + [WK[kc]], writes=[pk])
            return pb, pk

        WA, WKA = load_w(stM, "WA", [(512, 1536), (1792, 2560)])

        with ExitStack() as s0:
            W0, WK0 = load_w(s0, "W0", [(0, 512), (1536, 1792), (2560, 3072)])
            xb, rbc, rtk, front = mk_front(s0, "o")
            for blk in range(NO // 512):
                par = blk % 2
                XB = front(xoT.ap(), blk, par)
                for hm in range(8):
                    c0 = (hm // 2) * 128 + (hm % 2) * 64
                    pb, pk = proj(XB, WK0, lambda kc: W0[:, kc, c0:c0 + 64], lambda kc: xb[par][:, kc, :], 64, 512)
                    P.op("dve", I("tensor_tensor", out=qda[0:64, hm, blk * 512:(blk + 1) * 512], in0=pb[0:64, :],
                                  in1=rbc[par][0:64, :], op=ALU.mult),
                         reads=[pk, ("rbco", par)], writes=[("qda", hm, blk)])
                for grp in range(2):
                    c0 = 512 + grp * 128
                    pb, pk = proj(XB, WK0, lambda kc: W0[:, kc, c0:c0 + 128], lambda kc: xb[par][:, kc, :], 128, 512)
                    P.op("dve", I("tensor_tensor", out=qr[:, grp, blk * 512:(blk + 1) * 512], in0=pb[:, :],
                                  in1=rbc[par][:, :], op=ALU.mult),
                         reads=[pk, ("rbco", par)], writes=[("qr", blk)])
                for tt in range(4):
                    pb, pk = proj(XB, WK0, lambda kc: xb[par][:, kc, tt * 128:(tt + 1) * 128], lambda kc: W0[:, kc, 768:1280], 128, 512)
                    P.op("act", I("activation", out=gsl[:, blk * 4 + tt, :], in_=pb[:, :], func=AF.Silu,
                                  scale=rtk[par][:, tt:tt + 1]),
                         reads=[pk, ("rtko", par)], writes=[("gsl", blk * 4 + tt)])
            P.barrier()

        with ExitStack() as sA:
            xb, rbc, rtk, front = mk_front(sA, "a", nsq=2)
            kst = sbt(sA, "kst", [128, 4, 512], BF16)
            vst = sbt(sA, "vst", [128, 4, 512], BF16)
            krT = sbt(sA, "krT", [128, 2, 512], BF16)
            vr = sbt(sA, "vr", [128, 4, 512], BF16)
            krk = sbt(sA, "krk", [128, 4, 256], BF16)
            NPB = 2
            ADp = [sbt(sA, f"ADp{i}", [128, 2, 2, 248], BF16) for i in range(NPB)]
            Qxp = [sbt(sA, f"Qxp{i}", [128, 2, 248], BF16) for i in range(NPB)]
            Sst = sbt(sA, "Sst", [128, 256])
            Sbf = sbt(sA, "Sbf", [128, 256], BF16)
            rtmp = sbt(sA, "rtmp", [128, 512])
            for i in range(NPB):
                P.op("pool", I("memset", ADp[i][:], 0.0), writes=[("ADp", i)])
                P.op("pool", I("memset", Qxp[i][:], 0.0), writes=[("Qxp", i)])
            P.op("pool", I("memset", Sst[:], 0.0), writes=["Sst"])
            P.op("pool", I("memset", Sbf[:], 0.0), writes=[("Sbf", 0)])
            kT_v = kT_s.ap().rearrange("h m d s -> (m d) h s")
            v_v = v_s.ap().rearrange("(n p) c -> p n c", p=128)
            retfirst = {2: True, 3: True}
            Sbf2 = [Sbf, sbt(sA, "Sbf_b", [128, 256], BF16)]
            P.op("pool", I("memset", Sbf2[1][:], 0.0), writes=[("Sbf", 1)])
            for blk in range(NB):
                par = blk % 2
                XB = front(xT.ap(), blk, par)
                pending_prefetch = (blk + 1 < NB)

                def kv_unit(u, blk=blk, par=par, XB=XB):
                    if u < 4:
                        h = u
                        c0 = h * 128
                        pb, pk = proj(XB, WKA, lambda kc: WA[:, kc, c0:c0 + 128], lambda kc: xb[par][:, kc, :], 128, 512)
                        P.op("dve", I("tensor_tensor", out=kst[:, h, :], in0=pb[:, :], in1=rbc[par][:, :], op=ALU.mult),
                             reads=[pk, ("rbca", par)], writes=[("kst", h)])
                        if u == 3:
                            P.dma("sp", I("dma_start", out=kT_v[:, :, blk * 512:(blk + 1) * 512], in_=kst[:, :, :]),
                                  reads=[("kst", h_) for h_ in range(H)], writes=[("kT_s", blk)], sem="kst")
                    else:
                        tt = u - 4
                        pb, pk = proj(XB, WKA, lambda kc: xb[par][:, kc, tt * 128:(tt + 1) * 128], lambda kc: WA[:, kc, 512:1024], 128, 512)
                        P.op("act", I("activation", out=vst[:, tt, :], in_=pb[:, :], func=AF.Copy, scale=rtk[par][:, tt:tt + 1]),
                             reads=[pk, ("rtka", par)], writes=[("vst", tt)])
                        if u == 7:
                            P.dma("sp", I("dma_start", out=v_v[:, blk * 4:(blk + 1) * 4, :], in_=vst[:, :, :]),
                                  reads=[("vst", t_) for t_ in range(4)], writes=[("v_s", blk)], sem="vst")

                for grp in range(2):
                    c0 = 1024 + grp * 128
                    pb, pk = proj(XB, WKA, lambda kc: WA[:, kc, c0:c0 + 128], lambda kc: xb[par][:, kc, :], 128, 512)
                    P.op("dve", I("tensor_tensor", out=krT[:, grp, :], in0=pb[:, :], in1=rbc[par][:, :], op=ALU.mult),
                         reads=[pk, ("rbca", par)], writes=[("krT", grp)])
                for tt in range(4):
                    pb, pk = proj(XB, WKA, lambda kc: xb[par][:, kc, tt * 128:(tt + 1) * 128], lambda kc: WA[:, kc, 1280:1792], 128, 512)
                    P.op("act", I("activation", out=vr[:, tt, :], in_=pb[:, :], func=AF.Copy, scale=rtk[par][:, tt:tt + 1]),
                         reads=[pk, ("rtka", par)], writes=[("vr", tt)])
                for tt in range(4):
                    pbT, pkT = psum[pa % 2], PS[pa % 2]
                    pa += 1
                    pbTb = pbT.bitcast(BF16)
                    for grp in range(2):
                        P.op("pe", I("transpose", out=pbTb[:, grp * 128:(grp + 1) * 128], in_=krT[:, grp, tt * 128:(tt + 1) * 128],
                                     identity=ident_bf[:]), reads=[("krT", grp), "ident_bf"], writes=[pkT])
                    P.op("dve", I("tensor_tensor", out=krk[:, tt, :], in0=pbTb[:, 0:256], in1=kdec[:, :], op=ALU.mult),
                         reads=[pkT, "kdec"], writes=[("krk", tt)])
                for ch in range(8):
                    n = blk * 8 + ch
                    tt, cp = ch // 2, ch % 2
                    T, w = n // 16, n % 16
                    pbi = n % NPB
                    wo = 120 - 8 * w
                    rows = slice(cp * 64, cp * 64 + 64)
                    scur, snxt = n % 2, (n + 1) % 2
                    for h in range(H):
                        hr = slice((h % 2) * 64, (h % 2) * 64 + 64)
                        bk = 4 if h % 2 == 0 else 6
                        P.op("pe", I("matmul", psum[bk][rows, (h // 2) * 8:(h // 2) * 8 + 8],
                                     lhsT=krT[hr, h // 2, ch * 64:ch * 64 + 64], rhs=qr[hr, h // 2, 8 * n:8 * n + 8],
                                     start=True, stop=True, skip_group_check=True),
                             reads=[("krT", h // 2), ("qr", (8 * n) // 512)], writes=[PS[bk]])
                    for hh in range(2):
                        bk = 4 if hh == 0 else 6
                        P.op("dve", I("tensor_tensor", out=ADp[pbi][rows, hh, :, 120:128],
                                      in0=psum[bk][rows, 0:16].rearrange("p (g c) -> p g c", g=2), in1=dtt[rows, hh, :, :],
                                      op=ALU.mult),
                             reads=[PS[bk], "dt"], writes=[("ADp", pbi)])
                    P.op("pool", I("tensor_tensor", out=Qxp[pbi][:, :, 120:128], in0=qr[:, :, 8 * n:8 * n + 8],
                                   in1=qdec8[:, :, :], op=ALU.mult),
                         reads=[("qr", (8 * n) // 512), "qdec8"], writes=[("Qxp", pbi)])
                    for h in range(H):
                        hr = slice((h % 2) * 64, (h % 2) * 64 + 64)
                        P.op("pe", I("matmul", psum[5][hr, (h // 2) * 128:(h // 2) * 128 + 128],
                                     lhsT=krk[rows, tt, h * 64:h * 64 + 64], rhs=vr[rows, tt, h * 128:(h + 1) * 128],
                                     start=True, stop=True, skip_group_check=True),
                             reads=[("krk", tt), ("vr", tt)], writes=[PS[5]])
                    for grp in range(2):
                        P.op("dve", I("scalar_tensor_tensor", out=Sst[:, grp * 128:(grp + 1) * 128],
                                      in0=Sst[:, grp * 128:(grp + 1) * 128], scalar=sdec[:, grp:grp + 1],
                                      in1=psum[5][:, grp * 128:(grp + 1) * 128], op0=ALU.mult, op1=ALU.add),
                             reads=["Sst", PS[5], "sdec"], writes=["Sst"])
                    P.op("act", I("activation", out=Sbf2[snxt][:, :], in_=Sst[:, :], func=AF.Copy), reads=["Sst"], writes=[("Sbf", snxt)])
                    kv_unit(ch)
                    if pending_prefetch:
                        if ch == 0:
                            front.load(xT.ap(), blk + 1, (blk + 1) % 2)
                        elif ch == 3:
                            front.sq(xT.ap(), blk + 1, (blk + 1) % 2)
                        elif ch == 6:
                            front.stats(xT.ap(), blk + 1, (blk + 1) % 2)
                    if w == 0:
                        retfirst = {2: True, 3: True}
                    for h in range(H):
                        hr = slice((h % 2) * 64, (h % 2) * 64 + 64)
                        bk = 2 + cp
                        P.op("pe", I("matmul", psum[bk][:, h * 128:(h + 1) * 128], lhsT=ADp[pbi][rows, h % 2, h // 2, wo:wo + 128],
                                     rhs=vr[rows, tt, h * 128:(h + 1) * 128], start=retfirst[bk], stop=False, skip_group_check=True),
                             reads=[("ADp", pbi), ("vr", tt)], writes=[PS[bk]])
                        retfirst[bk] = False
                        bk = 2 + (h % 2)
                        P.op("pe", I("matmul", psum[bk][:, h * 128:(h + 1) * 128], lhsT=Qxp[pbi][hr, h // 2, wo:wo + 128],
                                     rhs=Sbf2[scur][hr, (h // 2) * 128:(h // 2) * 128 + 128], start=retfirst[bk], stop=False,
                                     skip_group_check=True),
                             reads=[("Qxp", pbi), ("Sbf", scur)], writes=[PS[bk]])
                        retfirst[bk] = False
                    if w == 15:
                        P.op("act", I("activation", out=rtmp[:, :], in_=psum[3][:, :], func=AF.Copy), reads=[PS[3]], writes=["rtmp"])
                        P.op("dve", I("tensor_tensor", out=ret[:, T, :], in0=psum[2][:, :], in1=rtmp[:, :], op=ALU.add),
                             reads=[PS[2], "rtmp"], writes=[("ret", T)])
            P.barrier()

        if stage == "ret":
            dst = sb("dbg_st", [128, 512])
            dv = dbg.ap().rearrange("(t p) c -> p t c", p=128)
            for t in range(NT):
                P.op("dve", I("tensor_copy", out=dst[:], in_=ret[:, t, :]), reads=[("ret", t)], writes=["dbg_st"])
                P.dma("sp", I("dma_start", out=dv[:, t, 0:512], in_=dst[:]), reads=["dbg_st"], writes=[("OUT", t)], sem="out")
            return finish()

        with ExitStack() as sB:
            KB = 8
            NKB = 3
            kbuf = [sbt(sB, f"kbuf{i}", [128, 2, KB * 128], BF16) for i in range(NKB)]
            vbuf = [sbt(sB, f"vbuf{i}", [128, KB, 132], BF16) for i in range(NKB)]
            eT = [sbt(sB, f"eT{i}", [128, 512], BF16) for i in range(4)]
            rz = sbt(sB, "rz", [128, 8])
            ust = [[sbt(sB, f"ust{g_}{m_}", [128, 516]) for m_ in range(2)] for g_ in range(2)]
            for i in range(NKB):
                P.op("pool", I("memset", kbuf[i][64:66, :, :], 1.0), writes=[("kbuf", i, "ones")])
                P.op("pool", I("memset", vbuf[i][:, :, 128:129], 1.0), writes=[("vbuf", i, "ones")])
            kT_l = kT_s.ap()
            v_l = v_s.ap().rearrange("(n p) c -> p n c", p=128)
            ldc = sc = ec = 0
            SKEW = 2
            ncols_total = [0]
            for h in range(H):
                for G in range(NG):
                    nkt = 32 * (G + 1)
                    steps = []
                    batches = []
                    Wh = ALIBI_CUT / (2.0 ** (-8.0 * (h + 1) / H))
                    chi = {}
                    for kt in range(nkt):
                        jj = kt - 32 * G
                        c0 = 16 * jj if jj >= 0 else 0
                        cq_max = math.floor((Wh + 128 * kt + 127 - 4096 * G) / 64.0)
                        c_hi = min(512, 8 * (cq_max + 1))
                        if c_hi <= c0:
                            continue
                        chi[kt] = min(512, ((c_hi + 127) // 128) * 128)
                    for kb0 in range(0, nkt, KB):
                        kts = [kt for kt in range(kb0, kb0 + KB) if kt in chi]
                        if not kts:
                            continue
                        lb = ldc % NKB
                        ldc += 1
                        batches.append((kb0, lb))
                        for kt in kts:
                            for m in range(2):
                                steps.append((kb0, lb, kt, m))
                    ncols_total[0] += sum(chi[st_[2]] - (16 * max(st_[2] - 32 * G, 0) // 128) * 128 for st_ in steps)
                    loaded = set()
                    started = set()

                    def kv_load(bi):
                        if bi >= len(batches) or bi in loaded:
                            return
                        loaded.add(bi)
                        kb0, lb = batches[bi]
                        P.dma("sp", I("dma_start", out=kbuf[lb][0:64, :, :],
                                      in_=kT_l[h, :, :, kb0 * 128:(kb0 + KB) * 128].rearrange("m d s -> d m s")),
                              reads=[("kT_s", b_) for b_ in range(kb0 // 4, (kb0 + KB) // 4)], writes=[("kbuf", lb)], sem=f"kbuf{lb}")
                        P.dma("sp", I("dma_start", out=vbuf[lb][:, :, 0:128], in_=v_l[:, kb0:kb0 + KB, h * 128:(h + 1) * 128]),
                              reads=[("v_s", b_) for b_ in range(kb0 // 4, (kb0 + KB) // 4)], writes=[("vbuf", lb)], sem=f"vbuf{lb}")

                    bidx = {kb0_: i_ for i_, (kb0_, _) in enumerate(batches)}
                    seenb = set()

                    def s_front(kb0, lb, kt, m, sp_, spk, eb, ek):
                        if kb0 not in seenb:
                            seenb.add(kb0)
                            kv_load(bidx[kb0])
                            kv_load(bidx[kb0] + 1)
                        ce = chi[kt]
                        jj = kt - 32 * G
                        c0 = 16 * jj if jj >= 0 else 0
                        cc0 = (c0 // 128) * 128
                        kk = kt - kb0
                        hm = 2 * h + m
                        P.op("pe", I("matmul", sp_[:, cc0:ce], lhsT=kbuf[lb][0:66, m, kk * 128:(kk + 1) * 128],
                                     rhs=qda[0:66, hm, G * 512 + cc0:G * 512 + ce], start=True, stop=True),
                             reads=[("kbuf", lb), ("kbuf", lb, "ones"), ("qda", hm, G), ("qda", hm, "aug")], writes=[spk])
                        if jj >= 0:
                            P.op("dve", I("tensor_tensor", out=sp_[:, c0:c0 + 16], in0=sp_[:, c0:c0 + 16],
                                          in1=fix[:, h, :], op=ALU.add), reads=[spk, "fix"], writes=[spk])
                            if c0 > cc0:
                                P.op("pool", I("memset", eb[:, cc0:c0], 0.0), writes=[ek])
                        P.op("act", I("activation", out=eb[:, c0:ce], in_=sp_[:, c0:ce], func=AF.Exp,
                                      bias=biask[:, h, kt - 32 * G + 96:kt - 32 * G + 97], scale=0.125),
                             reads=[spk, "biask"], writes=[ek])

                    def s_back(kb0, lb, kt, m, eb, ek):
                        jj = kt - 32 * G
                        c0 = 16 * jj if jj >= 0 else 0
                        cc0 = (c0 // 128) * 128
                        kk = kt - kb0
                        for j in range(cc0 // 128, chi[kt] // 128):
                            bank = 4 + 2 * m + (1 if j == 3 else 0)
                            col = 0 if j == 3 else j * 129
                            first = bank not in started
                            started.add(bank)
                            P.op("pe", I("matmul", psum[bank][:, col:col + 129], lhsT=eb[:, j * 128:(j + 1) * 128],
                                         rhs=vbuf[lb][:, kk, 0:129], start=first, stop=(kt == nkt - 1), skip_group_check=True),
                                 reads=[ek, ("vbuf", lb), ("vbuf", lb, "ones")], writes=[PS[bank]])

                    bufs = []
                    for i in range(len(steps) + SKEW):
                        if i < len(steps):
                            sp_, spk = psum[sc % 4], PS[sc % 4]
                            sc += 1
                            eb, ek = eT[ec % 4], ("eT", ec % 4)
                            ec += 1
                            bufs.append((eb, ek))
                            s_front(*steps[i], sp_, spk, eb, ek)
                        if i - SKEW >= 0:
                            s_back(*steps[i - SKEW], *bufs[i - SKEW])
                    gp_ = (h * NG + G) % 2
                    for m in range(2):
                        P.op("dve", I("tensor_copy", out=ust[gp_][m][:, 0:387], in_=psum[4 + 2 * m][:, 0:387]),
                             reads=[PS[4 + 2 * m]], writes=[("ust", gp_, m, 0)])
                        P.op("act", I("activation", out=ust[gp_][m][:, 387:516], in_=psum[5 + 2 * m][:, 0:129], func=AF.Copy),
                             reads=[PS[5 + 2 * m]], writes=[("ust", gp_, m, 1)])
                    for m in range(2):
                        hm = 2 * h + m
                        for j in range(4):
                            col = j * 129
                            part = 1 if j == 3 else 0
                            P.op("dve", I("reciprocal", out=rz[:, 4 * m + j:4 * m + j + 1], in_=ust[gp_][m][:, col + 128:col + 129]),
                                 reads=[("ust", gp_, m, part)], writes=[("rz", m, j)])
                            P.op("dve", I("tensor_scalar", out=att[:, 4 * G + j, hm, :], in0=ust[gp_][m][:, col:col + 128],
                                          scalar1=rz[:, 4 * m + j:4 * m + j + 1], scalar2=None, op0=ALU.mult),
                                 reads=[("ust", gp_, m, part), ("rz", m, j)], writes=[("att", 4 * G + j)])
            print('attention score columns per core:', ncols_total[0])
            P.barrier()
        stM.close()

        if stage == "attn":
            dst = sb("dbg_st", [128, 1024])
            dv = dbg.ap().rearrange("(t p) c -> p t c", p=128)
            for t in range(NT):
                P.op("dve", I("tensor_copy", out=dst[:], in_=att[:, t, :, :].rearrange("p a b -> p (a b)")),
                     reads=[("att", t)], writes=["dbg_st"])
                P.dma("sp", I("dma_start", out=dv[:, t, 0:1024], in_=dst[:]), reads=["dbg_st"], writes=[("OUT", t)], sem="out")
            return finish()

        sC = ExitStack()
        st.enter_context(sC)
        Wo = sbt(sC, "Wo", [128, 8, D], BF16)
        g2bc = sbt(sC, "g2bc", [128, D])
        gfbc = sbt(sC, "gfbc", [128, D])
        Wr = sbt(sC, "Wr", [128, 8, 36])
        brow = sbt(sC, "brow", [1, 36])
        lam_sb = sbt(sC, "lam_sb", [1, 256])
        lam_t = sbt(sC, "lam_t", [1, 136])
        nlam = sbt(sC, "nlam", [128, 1])
        sgs = sbt(sC, "sgs", [128, 1])
        grow = sbt(sC, "grow", [1, 2 * D])
        iota32 = sbt(sC, "iota32", [128, 64])
        iot2 = sbt(sC, "iot2", [128, 64], I32)
        Ustr = sbt(sC, "Ustr", [128, 128], BF16)
        OH = sbt(sC, "OH", [128, 2 * NT, 32], BF16)
        wts = sbt(sC, "wts", [128, NT, 2])
        dest_f = sbt(sC, "dest_f", [128, 2 * NT])
        dest_i = sbt(sC, "dest_i", [128, 2 * NT], I32)
        be_i = sbt(sC, "be_i", [128, NSB], I32)
        idxw = sbt(sC, "idxw", [128, NSB], I32)
        wost = [sbt(sC, f"wost{i}", [128, D]) for i in range(2)]

        zt = sbt(sC, "zt", [128, D], BF16)
        P.op("pool", I("memset", zt[:], 0.0), writes=["zt"])
        for b in range(NBLK):
            P.dma("sp", I("dma_start", out=xs_s.ap()[b * 128:(b + 1) * 128, :], in_=zt[:]), reads=["zt"], writes=[("xs_z", b)], sem="xsz")
        XSZ = [("xs_z", b) for b in range(NBLK)]
        P.dma("sp", I("dma_start", out=lam_sb[:], in_=lamv.ap().rearrange("a b c -> a (b c)")), writes=["lam_sb"], sem="c_lam")
        P.op("dve", I("tensor_tensor", out=lam_t[0:1, 0:64], in0=lam_sb[0:1, 0:64], in1=lam_sb[0:1, 64:128], op=ALU.mult),
             reads=["lam_sb"], writes=["lam_t"])
        P.op("dve", I("tensor_tensor", out=lam_t[0:1, 64:128], in0=lam_sb[0:1, 128:192], in1=lam_sb[0:1, 192:256], op=ALU.mult),
             reads=["lam_sb"], writes=["lam_t"])
        P.op("dve", I("tensor_reduce", out=lam_t[0:1, 128:130], in_=lam_t[0:1, 0:128].rearrange("p (a b) -> p a b", a=2),
                      axis=AX.X, op=ALU.add), reads=["lam_t"], writes=["lam_t"])
        P.op("act", I("activation", out=lam_t[0:1, 130:132], in_=lam_t[0:1, 128:130], func=AF.Exp), reads=["lam_t"], writes=["lam_t"])
        P.op("dve", I("tensor_tensor", out=lam_t[0:1, 132:133], in0=lam_t[0:1, 131:132], in1=lam_t[0:1, 130:131], op=ALU.subtract),
             reads=["lam_t"], writes=["lam_t"])
        P.op("dve", I("tensor_scalar", out=lam_t[0:1, 133:134], in0=lam_t[0:1, 132:133], scalar1=-LAMBDA_INIT, scalar2=None,
                      op0=ALU.add), reads=["lam_t"], writes=["lam_t"])
        P.op("pe", I("matmul", psum[0][:, 0:1], lhsT=ones_f[0:1, :], rhs=lam_t[0:1, 133:134], start=True, stop=True),
             reads=["lam_t", "ones_f"], writes=[PS[0]])
        P.op("dve", I("tensor_copy", out=nlam[:], in_=psum[0][:, 0:1]), reads=[PS[0]], writes=["nlam"])
        P.dma("sp", I("dma_start", out=sgs[:], in_=sg.ap()), writes=["sgs0"], sem="c_sg")
        P.op("dve", I("tensor_scalar", out=sgs[:], in0=sgs[:], scalar1=1.0 - LAMBDA_INIT, scalar2=None, op0=ALU.mult),
             reads=["sgs0"], writes=["sgs"])
        w_out_v = w_out.ap().rearrange("(k p) c -> p k c", p=128)
        for kc in range(8):
            sl = kc % 2
            P.dma("sp", I("dma_start", out=wost[sl][:], in_=w_out_v[:, kc, :]), writes=[("wost", sl)], sem=f"wost{sl}")
            if kc < 4:
                P.op("dve", I("tensor_scalar", out=Wo[:, kc, :], in0=wost[sl][:], scalar1=sgs[:, 0:1], scalar2=None, op0=ALU.mult),
                     reads=[("wost", sl), "sgs"], writes=[("Wo", kc)])
            else:
                P.op("dve", I("tensor_copy", out=Wo[:, kc, :], in_=wost[sl][:]), reads=[("wost", sl)], writes=[("Wo", kc)])
        WOK = [("Wo", kc) for kc in range(8)]
        P.dma("sp", I("dma_start", out=grow[0:1, 0:D], in_=g2.ap()), writes=["grow_a"], sem="c_g2")
        P.dma("sp", I("dma_start", out=grow[0:1, D:2 * D], in_=gf.ap()), writes=["grow_b"], sem="c_gf")
        for i, (dstt, key) in enumerate(((g2bc, "g2bc"), (gfbc, "gfbc"))):
            for half in range(2):
                P.op("pe", I("matmul", psum[1 + half][:, :], lhsT=ones_f[0:1, :], rhs=grow[0:1, i * D + half * 512:i * D + half * 512 + 512],
                             start=True, stop=True), reads=["grow_a", "grow_b", "ones_f"], writes=[PS[1 + half]])
                P.op("dve", I("tensor_copy", out=dstt[:, half * 512:(half + 1) * 512], in_=psum[1 + half][:, :]),
                     reads=[PS[1 + half]], writes=[(key, half)])
        P.dma("sp", I("dma_start", out=Wr[:], in_=wr.ap().rearrange("(k p) c -> p k c", p=128)), writes=["Wr"], sem="c_wr")
        P.dma("sp", I("dma_start", out=brow[:], in_=br.ap()), writes=["brow"], sem="c_br")
        P.op("pool", I("iota", iot2[:], pattern=[[1, 64]], base=0, channel_multiplier=0), writes=["iot2"])
        P.op("dve", I("tensor_copy", out=iota32[:], in_=iot2[:]), reads=["iot2"], writes=["iota32"])
        P.op("dve", I("tensor_single_scalar", out=Ustr[:], in_=iotf[:], scalar=0.0, op=ALU.is_gt), reads=["iotf"], writes=["Ustr"])

        sT = ExitStack()
        st.enter_context(sT)
        od = [sbt(sT, f"od{i_}", [128, 4, 128]) for i_ in range(2)]
        sqt = [sbt(sT, f"sqt{i_}", [128, 4, 128]) for i_ in range(2)]
        ssr = [sbt(sT, f"ssr{i_}", [128, 16]) for i_ in range(2)]
        mix = [sbt(sT, f"mix{i_}", [128, D], BF16) for i_ in range(2)]
        mixT = [sbt(sT, f"mixT{i_}", [128, 8, 128], BF16) for i_ in range(2)]
        xot = [sbt(sT, f"xot{i_}", [128, D]) for i_ in range(2)]
        h1t = [sbt(sT, f"h1t{i_}", [128, D]) for i_ in range(2)]
        junk = [sbt(sT, f"junk{i_}", [128, D], BF16) for i_ in range(2)]
        xn2f = [sbt(sT, f"xn2f{i_}", [128, D]) for i_ in range(2)]
        xn2b = [sbt(sT, f"xn2b{i_}", [128, D], BF16) for i_ in range(2)]
        xnT = [sbt(sT, f"xnT{i_}", [128, 8, 128]) for i_ in range(2)]
        lg = [sbt(sT, f"lg{i_}", [128, 36]) for i_ in range(2)]
        rt_ = [sbt(sT, f"rt_{i_}", [128, 64]) for i_ in range(2)]
        t8 = [sbt(sT, f"t8{i_}", [128, 8]) for i_ in range(2)]
        i8 = [sbt(sT, f"i8{i_}", [128, 8], U32) for i_ in range(2)]
        psbf = [psum[i].bitcast(BF16) for i in range(8)]
        xo_v = xo.ap().rearrange("(t p) c -> p t c", p=128)
        h1_v = h1_s.ap().rearrange("(t p) c -> p t c", p=128)
        xn_v = xn_s.ap().rearrange("(t p) c -> p t c", p=128)
        out_v = out.ap().rearrange("(t p) c -> p t c", p=128)

        def rstd_from(ss_ap, out_ap, n, key_in, key_out, scale):
            P.op("act", I("activation", out=out_ap, in_=ss_ap, func=AF.Ln, bias=epsc[:, 0:1], scale=scale),
                 reads=[key_in, "epsc"], writes=[key_out])
            P.op("act", I("activation", out=out_ap, in_=out_ap, func=AF.Exp, scale=-0.5), reads=[key_out], writes=[key_out])

        def C1(T):
            pz = T % 2
            attv = att[:, T, :, :].rearrange("p (h m) e -> p h m e", m=2)
            P.op("dve", I("scalar_tensor_tensor", out=od[pz][:], in0=attv[:, :, 1, :], scalar=nlam[:, 0:1], in1=attv[:, :, 0, :],
                          op0=ALU.mult, op1=ALU.add), reads=[("att", T), "nlam"], writes=[("od", pz)])
            P.op("dve", I("tensor_tensor", out=sqt[pz][:], in0=od[pz][:], in1=od[pz][:], op=ALU.mult), reads=[("od", pz)], writes=[("sqt", pz)])
            P.op("dve", I("tensor_reduce", out=ssr[pz][:, 0:4], in_=sqt[pz][:], axis=AX.X, op=ALU.add), reads=[("sqt", pz)], writes=[("ssr", pz)])
            retv = ret[:, T, :].rearrange("p (h e) -> p h e", h=H)
            P.op("dve", I("tensor_tensor", out=sqt[pz][:], in0=retv, in1=retv, op=ALU.mult), reads=[("ret", T), ("ssr", pz)], writes=[("sqt", pz)])
            P.op("dve", I("tensor_reduce", out=ssr[pz][:, 4:8], in_=sqt[pz][:], axis=AX.X, op=ALU.add), reads=[("sqt", pz)], writes=[("ssr", pz)])
            rstd_from(ssr[pz][:, 0:8], ssr[pz][:, 8:16], 8, ("ssr", pz), ("ssr2", pz), 1.0 / 128)
            for h in range(H):
                P.op("dve", I("tensor_scalar", out=mix[pz][:, h * 128:(h + 1) * 128], in0=od[pz][:, h, :], scalar1=ssr[pz][:, 8 + h:9 + h],
                              scalar2=None, op0=ALU.mult), reads=[("od", pz), ("ssr2", pz)], writes=[("mix", pz, h)])
                P.op("dve", I("scalar_tensor_tensor", out=mix[pz][:, 512 + h * 128:512 + (h + 1) * 128], in0=retv[:, h, :],
                              scalar=ssr[pz][:, 12 + h:13 + h], in1=gsl[:, T, h * 128:(h + 1) * 128], op0=ALU.mult, op1=ALU.mult),
                     reads=[("ret", T), ("ssr2", pz), ("gsl", T)], writes=[("mix", pz, 4 + h)])

        def C2(T):
            pz = T % 2
            for kc in range(8):
                P.op("pe", I("transpose", out=psbf[pz][:, kc * 128:(kc + 1) * 128], in_=mix[pz][:, kc * 128:(kc + 1) * 128],
                             identity=ident_bf[:]), reads=[("mix", pz, kc), "ident_bf"], writes=[PS[pz]])
            P.op("act", I("activation", out=mixT[pz][:].rearrange("p a b -> p (a b)"), in_=psbf[pz][:, :], func=AF.Copy),
                 reads=[PS[pz]], writes=[("mixT", pz)])
            P.dma("sp", I("dma_start", out=xot[pz][:], in_=xo_v[:, T, :]), writes=[("xot", pz)], sem=f"xot{pz}")

        def C3(T):
            pz = T % 2
            for half in range(2):
                for kc in range(8):
                    P.op("pe", I("matmul", psum[2 + half][:, :], lhsT=mixT[pz][:, kc, :], rhs=Wo[:, kc, half * 512:(half + 1) * 512],
                                 start=(kc == 0), stop=(kc == 7)), reads=[("mixT", pz), WOK[kc]], writes=[PS[2 + half]])
                P.op("dve", I("tensor_tensor", out=h1t[pz][:, half * 512:(half + 1) * 512], in0=psum[2 + half][:, :],
                              in1=xot[pz][:, half * 512:(half + 1) * 512], op=ALU.add),
                     reads=[PS[2 + half], ("xot", pz)], writes=[("h1t", pz, half)])
            P.dma("sp", I("dma_start", out=h1_v[:, T, :], in_=h1t[pz][:]), reads=[("h1t", pz, 0), ("h1t", pz, 1)], writes=[("h1_s", T)], sem=f"h1st{pz}")
            P.op("act", I("activation", out=junk[pz][:], in_=h1t[pz][:], func=AF.Square, accum_out=rt_[pz][:, 0:1]),
                 reads=[("h1t", pz, 0), ("h1t", pz, 1)], writes=[("junk", pz), ("rt0", pz)])
            rstd_from(rt_[pz][:, 0:1], rt_[pz][:, 1:2], 1, ("rt0", pz), ("rt1", pz), 1.0 / D)
            P.op("dve", I("scalar_tensor_tensor", out=xn2f[pz][:], in0=h1t[pz][:], scalar=rt_[pz][:, 1:2], in1=g2bc[:], op0=ALU.mult, op1=ALU.mult),
                 reads=[("h1t", pz, 0), ("h1t", pz, 1), ("rt1", pz), ("g2bc", 0), ("g2bc", 1)], writes=[("xn2f", pz)])
            P.op("act", I("activation", out=xn2b[pz][:], in_=xn2f[pz][:], func=AF.Copy), reads=[("xn2f", pz)], writes=[("xn2b", pz)])
            P.dma("sp", I("dma_start", out=xn_v[:, T, :], in_=xn2b[pz][:]), reads=[("xn2b", pz)], writes=[("xn_s", T)], sem=f"xnst{pz}")

        def C4(T):
            pz = T % 2
            for kc in range(8):
                bk = 4 + kc // 4
                P.op("pe", I("transpose", out=psum[bk][:, (kc % 4) * 128:(kc % 4 + 1) * 128], in_=xn2f[pz][:, kc * 128:(kc + 1) * 128],
                             identity=ident_f[:]), reads=[("xn2f", pz), "ident_f"], writes=[PS[bk]])
            for half in range(2):
                P.op("act" if half else "dve",
                     I("activation", out=xnT[pz][:, 4 * half:4 * half + 4, :].rearrange("p a b -> p (a b)"), in_=psum[4 + half][:, :], func=AF.Copy)
                     if half else I("tensor_copy", out=xnT[pz][:, 0:4, :].rearrange("p a b -> p (a b)"), in_=psum[4][:, :]),
                     reads=[PS[4 + half]], writes=[("xnT", pz, half)])
            for kc in range(8):
                P.op("pe", I("matmul", psum[6][:, 0:36], lhsT=xnT[pz][:, kc, :], rhs=Wr[:, kc, :], start=(kc == 0), stop=False),
                     reads=[("xnT", pz, kc // 4), "Wr"], writes=[PS[6]])
            P.op("pe", I("matmul", psum[6][:, 0:36], lhsT=ones_f[0:1, :], rhs=brow[0:1, :], start=False, stop=True),
                 reads=["ones_f", "brow"], writes=[PS[6]])
            P.op("dve", I("tensor_copy", out=lg[pz][:], in_=psum[6][:, 0:36]), reads=[PS[6]], writes=[("lg", pz)])

        def C5(T):
            pz = T % 2
            R = ("rt", pz)
            P.op("dve", I("tensor_reduce", out=rt_[pz][:, 2:3], in_=lg[pz][:, 0:4], axis=AX.X, op=ALU.max), reads=[("lg", pz)], writes=[R])
            P.op("dve", I("tensor_scalar", out=rt_[pz][:, 3:4], in0=rt_[pz][:, 2:3], scalar1=-1.0, scalar2=None, op0=ALU.mult), reads=[R], writes=[R])
            P.op("act", I("activation", out=rt_[pz][:, 4:8], in_=lg[pz][:, 0:4], func=AF.Exp, bias=rt_[pz][:, 3:4], scale=1.0, accum_out=rt_[pz][:, 8:9]),
                 reads=[R, ("lg", pz)], writes=[R])
            P.op("dve", I("reciprocal", out=rt_[pz][:, 9:10], in_=rt_[pz][:, 8:9]), reads=[R], writes=[R])
            P.op("dve", I("tensor_scalar", out=rt_[pz][:, 10:14], in0=lg[pz][:, 0:4], scalar1=rt_[pz][:, 2:3], scalar2=None, op0=ALU.is_equal),
                 reads=[R, ("lg", pz)], writes=[R])
            lev = lg[pz][:, 4:36].rearrange("p (g e) -> p g e", g=4)
            P.op("dve", I("tensor_scalar", out=rt_[pz][:, 16:24], in0=lev[:, 0, :], scalar1=rt_[pz][:, 10:11], scalar2=None, op0=ALU.mult),
                 reads=[R, ("lg", pz)], writes=[R])
            for g in range(1, 4):
                P.op("dve", I("scalar_tensor_tensor", out=rt_[pz][:, 16:24], in0=lev[:, g, :], scalar=rt_[pz][:, 10 + g:11 + g],
                              in1=rt_[pz][:, 16:24], op0=ALU.mult, op1=ALU.add), reads=[R, ("lg", pz)], writes=[R])
            P.op("dve", I("tensor_tensor", out=rt_[pz][:, 24:28], in0=rt_[pz][:, 10:14], in1=iota32[:, 0:4], op=ALU.mult),
                 reads=[R, "iota32"], writes=[R])
            P.op("dve", I("tensor_reduce", out=rt_[pz][:, 28:29], in_=rt_[pz][:, 24:28], axis=AX.X, op=ALU.add), reads=[R], writes=[R])
            P.op("dve", I("max", out=t8[pz][:], in_=rt_[pz][:, 16:24]), reads=[R], writes=[("t8", pz)])
            P.op("dve", I("max_index", out=i8[pz][:], in_max=t8[pz][:], in_values=rt_[pz][:, 16:24]), reads=[R, ("t8", pz)], writes=[("i8", pz)])
            P.op("dve", I("tensor_copy", out=rt_[pz][:, 30:32], in_=i8[pz][:, 0:2]), reads=[("i8", pz)], writes=[R])
            for k in range(2):
                P.op("dve", I("scalar_tensor_tensor", out=rt_[pz][:, 32 + k:33 + k], in0=rt_[pz][:, 28:29], scalar=8.0, in1=rt_[pz][:, 30 + k:31 + k],
                              op0=ALU.mult, op1=ALU.add), reads=[R], writes=[R])
                P.op("dve", I("tensor_scalar", out=OH[:, k * NT + T, :], in0=iota32[:, 0:32], scalar1=rt_[pz][:, 32 + k:33 + k], scalar2=None,
                              op0=ALU.is_equal), reads=[R, "iota32"], writes=[("OH", k * NT + T)])
            P.op("dve", I("tensor_tensor", out=rt_[pz][:, 34:35], in0=t8[pz][:, 1:2], in1=t8[pz][:, 0:1], op=ALU.subtract), reads=[("t8", pz), R], writes=[R])
            P.op("act", I("activation", out=rt_[pz][:, 35:36], in_=rt_[pz][:, 34:35], func=AF.Exp), reads=[R], writes=[R])
            P.op("dve", I("tensor_scalar", out=rt_[pz][:, 36:37], in0=rt_[pz][:, 35:36], scalar1=1.0, scalar2=None, op0=ALU.add), reads=[R], writes=[R])
            P.op("dve", I("reciprocal", out=rt_[pz][:, 37:38], in_=rt_[pz][:, 36:37]), reads=[R], writes=[R])
            P.op("dve", I("tensor_tensor", out=wts[:, T, 0:1], in0=rt_[pz][:, 37:38], in1=rt_[pz][:, 9:10], op=ALU.mult), reads=[R], writes=[("wts", T)])
            P.op("dve", I("tensor_tensor", out=wts[:, T, 1:2], in0=wts[:, T, 0:1], in1=rt_[pz][:, 35:36], op=ALU.mult),
                 reads=[R, ("wts", T)], writes=[("wts", T)])

        for i in range(NT + 4):
            if i < NT:
                C1(i)
            if 0 <= i - 1 < NT:
                C2(i - 1)
            if 0 <= i - 2 < NT:
                C3(i - 2)
            if 0 <= i - 3 < NT:
                C4(i - 3)
            if 0 <= i - 4 < NT:
                C5(i - 4)

        if stage == "h1":
            dst = sb("dbg_st", [128, 1024])
            dv = dbg.ap().rearrange("(t p) c -> p t c", p=128)
            for t in range(NT):
                P.dma("sp", I("dma_start", out=dst[:], in_=h1_v[:, t, :]), reads=[("h1_s", t)], writes=["dbg_st"], sem="dbgl")
                P.dma("sp", I("dma_start", out=dv[:, t, 0:1024], in_=dst[:]), reads=["dbg_st"], writes=[("OUT", t)], sem="out")
            dst2 = sb("dbg_st2", [128, 2 * NT, 32])
            P.op("dve", I("tensor_copy", out=dst2[:], in_=OH[:]), reads=[("OH", t) for t in range(2 * NT)], writes=["dbg_st2"])
            P.dma("sp", I("dma_start", out=dv[:, 0, 1024:1024 + 64 * NT].rearrange("p (a b) -> p a b", b=32), in_=dst2[:]),
                  reads=["dbg_st2"], writes=[("OUT", "oh")], sem="out")
            P.dma("sp", I("dma_start", out=dv[:, 1, 1024:1024 + 2 * NT].rearrange("p (a b) -> p a b", b=2), in_=wts[:]),
                  reads=[("wts", t) for t in range(NT)], writes=[("OUT", "w")], sem="out")
            return finish()
        P.barrier()
        sT.close()

        sD = ExitStack()
        st.enter_context(sD)
        NTT = 2 * NT
        HB = NTT // 2
        assert HB * 32 <= 512
        cnt_f = sbt(sD, "cnt_f", [128, 32])
        cnt_i = sbt(sD, "cnt_i", [128, 32], I32)
        pad_f = sbt(sD, "pad_f", [128, 32])
        pend = sbt(sD, "pend", [128, 32])
        pstart = sbt(sD, "pstart", [128, 32])
        dtmp = sbt(sD, "dtmp", [128, HB, 32])
        cmpb = sbt(sD, "cmpb", [128, NSB, 32])
        thr = sbt(sD, "thr", [128, NSB])
        be_f = sbt(sD, "be_f", [128, NSB])
        idxf = sbt(sD, "idxf", [128, NSB])
        OHK = [("OH", t) for t in range(NTT)]
        for t in range(NTT):
            bk = t // HB
            cs = slice((t % HB) * 32, (t % HB) * 32 + 32)
            for t2 in range(t):
                P.op("pe", I("matmul", psum[bk][:, cs], lhsT=ones_bf[:, :], rhs=OH[:, t2, :], start=(t2 == 0), stop=False,
                             skip_group_check=True), reads=[OHK[t2], "ones_bf"], writes=[PS[bk]])
            P.op("pe", I("matmul", psum[bk][:, cs], lhsT=Ustr[:, :], rhs=OH[:, t, :], start=(t == 0), stop=True, skip_group_check=True),
                 reads=[OHK[t], "Ustr"], writes=[PS[bk]])
        for t in range(NTT):
            P.op("pe", I("matmul", psum[2][:, 0:32], lhsT=ones_bf[:, :], rhs=OH[:, t, :], start=(t == 0), stop=(t == NTT - 1)),
                 reads=[OHK[t], "ones_bf"], writes=[PS[2]])
        P.op("dve", I("tensor_scalar", out=cnt_i[:], in0=psum[2][:, 0:32], scalar1=255.0, scalar2=None, op0=ALU.add),
             reads=[PS[2]], writes=["cnt_i"])
        P.op("dve", I("tensor_scalar", out=cnt_i[:], in0=cnt_i[:], scalar1=8, scalar2=8, op0=ALU.arith_shift_right,
                      op1=ALU.logical_shift_left), reads=["cnt_i"], writes=["cnt_i2"])
        P.op("dve", I("tensor_copy", out=pad_f[:], in_=cnt_i[:]), reads=["cnt_i2"], writes=["pad_f"])
        P.op("dve", I("tensor_tensor_scan", out=pend[:], data0=ones_f[:, 0:32], data1=pad_f[:], initial=0.0, op0=ALU.mult, op1=ALU.add),
             reads=["pad_f", "ones_f"], writes=["pend"])
        P.op("dve", I("tensor_tensor", out=pstart[:], in0=pend[:], in1=pad_f[:], op=ALU.subtract), reads=["pend", "pad_f"], writes=["pstart"])
        for bk in range(2):
            P.op("dve", I("tensor_tensor", out=dtmp[:], in0=psum[bk][:, 0:HB * 32].rearrange("p (a b) -> p a b", b=32),
                          in1=pstart[:, 0:32].unsqueeze(1).to_broadcast([128, HB, 32]), op=ALU.add),
                 reads=[PS[bk], "pstart"], writes=["dtmp"])
            P.op("dve", I("tensor_tensor", out=dtmp[:], in0=dtmp[:], in1=OH[:, bk * HB:(bk + 1) * HB, :], op=ALU.mult),
                 reads=["dtmp"] + OHK, writes=["dtmp"])
            P.op("dve", I("tensor_reduce", out=dest_f[:, bk * HB:(bk + 1) * HB], in_=dtmp[:], axis=AX.X, op=ALU.add),
                 reads=["dtmp"], writes=[("dest_f", bk)])
        P.op("dve", I("tensor_copy", out=dest_i[:], in_=dest_f[:]), reads=[("dest_f", 0), ("dest_f", 1)], writes=["dest_i"])
        P.op("pool", I("iota", iot2[:, 0:NSB], pattern=[[256, NSB]], base=0, channel_multiplier=0), writes=["iot2"])
        P.op("dve", I("tensor_copy", out=thr[:], in_=iot2[:, 0:NSB]), reads=["iot2"], writes=["thr"])
        P.op("dve", I("tensor_tensor", out=cmpb[:], in0=pend[:, 0:32].unsqueeze(1).to_broadcast([128, NSB, 32]),
                      in1=thr[:, 0:NSB].unsqueeze(2).to_broadcast([128, NSB, 32]), op=ALU.is_le),
             reads=["pend", "thr"], writes=["cmpb"])
        P.op("dve", I("tensor_reduce", out=be_f[:], in_=cmpb[:], axis=AX.X, op=ALU.add), reads=["cmpb"], writes=["be_f"])
        P.op("pool", I("iota", iot2[:, 0:1], pattern=[[0, 1]], base=0, channel_multiplier=1), reads=["thr"], writes=["iot2"])
        P.op("dve", I("tensor_copy", out=thr[:, 0:1], in_=iot2[:, 0:1]), reads=["iot2", "cmpb"], writes=["thr"])
        P.op("dve", I("scalar_tensor_tensor", out=idxf[:],
                      in0=be_f[:], scalar=128.0, in1=thr[:, 0:1].to_broadcast([128, NSB]), op0=ALU.mult, op1=ALU.add),
             reads=["be_f", "thr"], writes=["idxf"])
        P.op("dve", I("tensor_copy", out=idxw[:], in_=idxf[:]), reads=["idxf"], writes=["idxw"])
        P.op("dve", I("tensor_scalar", out=be_f[:], in0=be_f[:], scalar1=float(NEXP - 1), scalar2=None, op0=ALU.min),
             reads=["be_f"], writes=["be_f2"])
        P.op("dve", I("tensor_copy", out=be_i[:], in_=be_f[:]), reads=["be_f2"], writes=["be_i"])

        if stage == "disp":
            dst = sb("dbg_st", [128, 256])
            dv = dbg.ap().rearrange("(t p) c -> p t c", p=128)
            P.op("dve", I("tensor_copy", out=dst[:, 0:NTT], in_=dest_f[:]), reads=[("dest_f", 0), ("dest_f", 1)], writes=["dbg_st"])
            P.op("dve", I("tensor_copy", out=dst[:, 64:64 + NSB], in_=be_f[:]), reads=["be_f2"], writes=["dbg_st"])
            P.op("dve", I("tensor_copy", out=dst[:, 160:192], in_=pend[:]), reads=["pend"], writes=["dbg_st"])
            P.dma("sp", I("dma_start", out=dv[:, 0, 0:256], in_=dst[:]), reads=["dbg_st"], writes=[("OUT", 0)], sem="out")
            return finish()

        xs_rows = xs_s.ap()
        sE = ExitStack()
        st.enter_context(sE)
        xsc = [sbt(sE, f"xsc{i}", [128, D], BF16) for i in range(2)]
        for T in range(NT):
            sl = T % 2
            P.dma("sp", I("dma_start", out=xsc[sl][:], in_=xn_v[:, T, :]), reads=[("xn_s", T)], writes=[("xsc", sl)], sem=f"xsc{sl}")
            for k in range(2):
                t = k * NT + T
                P.dma("pool", I("indirect_dma_start", out=xs_rows[:, :],
                                out_offset=bass.IndirectOffsetOnAxis(ap=dest_i[:, t:t + 1], axis=0),
                                in_=xsc[sl][:, :], in_offset=None),
                      reads=[("xsc", sl), "dest_i"] + XSZ, writes=[("xs_sc", t)], sem=f"scat{sl}{k}")

        wgb = [sbt(sE, f"wgb{i}", [128, 8, HID], BF16) for i in range(2)]
        wub = [sbt(sE, f"wub{i}", [128, 8, HID], BF16) for i in range(2)]
        wdb = [sbt(sE, f"wdb{i}", [128, 4, D], BF16) for i in range(2)]
        xsb = [sbt(sE, f"xsb{i}", [128, D], BF16) for i in range(2)]
        xsT = [sbt(sE, f"xsT{i}", [128, 8, 128], BF16) for i in range(2)]
        sgt = [sbt(sE, f"sgt{i}", [128, HID]) for i in range(2)]
        hb = [sbt(sE, f"hb{i}", [128, HID], BF16) for i in range(2)]
        hT = [sbt(sE, f"hT{i}", [128, 4, 128], BF16) for i in range(2)]
        ysb = [sbt(sE, f"ysb{i}", [128, D]) for i in range(2)]

        def wload(sb_, which):
            if sb_ >= NSB:
                return
            sl = sb_ % 2
            for (dst_t, src_t, nm) in which:
                def gth(e, dst_ap=dst_t[sl][:, :, :].rearrange("p k n -> p (k n)"), src_ap=src_t.ap()[:, :], ix=idxw[:, sb_:sb_ + 1]):
                    return e.indirect_dma_start(out=dst_ap, out_offset=None, in_=src_ap,
                                                in_offset=bass.IndirectOffsetOnAxis(ap=ix, axis=0),
                                                bounds_check=P.bcreg, oob_is_err=False)
                P.dma("pool", gth, reads=["idxw"], writes=[(nm, sl)], sem=f"{nm}{sl}")

        WGU = ((wgb, wg, "wgb"), (wub, wu, "wub"))
        WD = ((wdb, wd, "wdb"),)

        def xload(b):
            sl = b % 2
            P.dma("sp", I("dma_start", out=xsb[sl][:], in_=xs_rows[b * 128:(b + 1) * 128, :]),
                  reads=[("xs_sc", t_) for t_ in range(2 * NT)], writes=[("xsb", sl)], sem=f"xsb{sl}")

        def S1(b):
            sl = b % 2
            for kc in range(8):
                P.op("pe", I("transpose", out=psbf[sl][:, kc * 128:(kc + 1) * 128], in_=xsb[sl][:, kc * 128:(kc + 1) * 128],
                             identity=ident_bf[:]), reads=[("xsb", sl), "ident_bf"], writes=[PS[sl]])
            P.op("act", I("activation", out=xsT[sl][:].rearrange("p a b -> p (a b)"), in_=psbf[sl][:, :], func=AF.Copy),
                 reads=[PS[sl]], writes=[("xsT", sl)])

        def S2(b):
            sl = b % 2
            for kc in range(8):
                P.op("pe", I("matmul", psum[2][:, :], lhsT=xsT[sl][:, kc, :], rhs=wgb[(b // 2) % 2][:, kc, :], start=(kc == 0), stop=(kc == 7)),
                     reads=[("xsT", sl), ("wgb", (b // 2) % 2)], writes=[PS[2]])
            for kc in range(8):
                P.op("pe", I("matmul", psum[3][:, :], lhsT=xsT[sl][:, kc, :], rhs=wub[(b // 2) % 2][:, kc, :], start=(kc == 0), stop=(kc == 7)),
                     reads=[("xsT", sl), ("wub", (b // 2) % 2)], writes=[PS[3]])
            P.op("act", I("activation", out=sgt[sl][:], in_=psum[2][:, :], func=AF.Silu), reads=[PS[2]], writes=[("sgt", sl)])
            P.op("dve", I("tensor_tensor", out=hb[sl][:], in0=psum[3][:, :], in1=sgt[sl][:], op=ALU.mult),
                 reads=[PS[3], ("sgt", sl)], writes=[("hb", sl)])

        def S3(b):
            sl = b % 2
            for hc in range(4):
                P.op("pe", I("transpose", out=psbf[6][:, hc * 128:(hc + 1) * 128], in_=hb[sl][:, hc * 128:(hc + 1) * 128],
                             identity=ident_bf[:]), reads=[("hb", sl), "ident_bf"], writes=[PS[6]])
            P.op("dve", I("tensor_copy", out=hT[sl][:].rearrange("p a b -> p (a b)"), in_=psbf[6][:, 0:512]), reads=[PS[6]],
                 writes=[("hT", sl)])

        def S4(b):
            sl = b % 2
            for half in range(2):
                for hc in range(4):
                    P.op("pe", I("matmul", psum[4 + half][:, :], lhsT=hT[sl][:, hc, :], rhs=wdb[(b // 2) % 2][:, hc, half * 512:(half + 1) * 512],
                                 start=(hc == 0), stop=(hc == 3)), reads=[("hT", sl), ("wdb", (b // 2) % 2)], writes=[PS[4 + half]])
                if half == 0:
                    P.op("act", I("activation", out=ysb[sl][:, 0:512], in_=psum[4][:, :], func=AF.Copy), reads=[PS[4]], writes=[("ysb", sl, 0)])
                else:
                    P.op("dve", I("tensor_copy", out=ysb[sl][:, 512:1024], in_=psum[5][:, :]), reads=[PS[5]], writes=[("ysb", sl, 1)])
            P.dma("sp", I("dma_start", out=ys_s.ap()[b * 128:(b + 1) * 128, :], in_=ysb[sl][:]),
                  reads=[("ysb", sl, 0), ("ysb", sl, 1)], writes=[("ys_s", b)], sem=f"ysst{sl}")

        for b in range(min(2, NBLK)):
            xload(b)
        for sb_ in range(2):
            wload(sb_, WGU)
            wload(sb_, WD)
        for i in range(NBLK + 3):
            if i < NBLK:
                S1(i)
                if i + 2 < NBLK:
                    xload(i + 2)
            if 0 <= i - 1 < NBLK:
                S2(i - 1)
                if (i - 1) % 2 == 1:
                    wload((i - 1) // 2 + 2, WGU)
            if 0 <= i - 2 < NBLK:
                S3(i - 2)
            if 0 <= i - 3 < NBLK:
                S4(i - 3)
                if (i - 3) % 2 == 1:
                    wload((i - 3) // 2 + 2, WD)

        P.barrier()
        sE.close()
        y0 = [sbt(sD, f"y0_{i}", [128, D]) for i in range(2)]
        y1 = [sbt(sD, f"y1_{i}", [128, D]) for i in range(2)]
        hh_ = [sbt(sD, f"hh_{i}", [128, D]) for i in range(2)]
        oo = [sbt(sD, f"oo{i}", [128, D]) for i in range(2)]
        junk2 = sbt(sD, "junk2", [128, D], BF16)
        fr = sbt(sD, "fr", [128, 4])
        for T in range(NT):
            sl = T % 2
            P.dma("sp", I("dma_start", out=hh_[sl][:], in_=h1_v[:, T, :]), reads=[("h1_s", T)], writes=[("hh", sl)], sem=f"hh{sl}")
            for k, yb in ((0, y0), (1, y1)):
                t = k * NT + T
                P.dma("pool", I("indirect_dma_start", out=yb[sl][:, :], out_offset=None, in_=ys_s.ap()[:, :],
                                in_offset=bass.IndirectOffsetOnAxis(ap=dest_i[:, t:t + 1], axis=0)),
                      reads=[("ys_s", b_) for b_ in range(NBLK)] + ["dest_i"], writes=[("y", k, sl)], sem=f"yg{k}{sl}")
            P.op("dve", I("scalar_tensor_tensor", out=oo[sl][:], in0=y0[sl][:], scalar=wts[:, T, 0:1], in1=hh_[sl][:], op0=ALU.mult, op1=ALU.add),
                 reads=[("y", 0, sl), ("hh", sl), ("wts", T)], writes=[("oo", sl)])
            P.op("dve", I("scalar_tensor_tensor", out=oo[sl][:], in0=y1[sl][:], scalar=wts[:, T, 1:2], in1=oo[sl][:], op0=ALU.mult, op1=ALU.add),
                 reads=[("y", 1, sl), ("oo", sl), ("wts", T)], writes=[("oo", sl)])
            P.op("act", I("activation", out=junk2[:], in_=oo[sl][:], func=AF.Square, accum_out=fr[:, 0:1]), reads=[("oo", sl)],
                 writes=["junk2", "fr0"])
            rstd_from(fr[:, 0:1], fr[:, 1:2], 1, "fr0", "fr1", 1.0 / D)
            P.op("dve", I("scalar_tensor_tensor", out=oo[sl][:], in0=oo[sl][:], scalar=fr[:, 1:2], in1=gfbc[:], op0=ALU.mult, op1=ALU.mult),
                 reads=[("oo", sl), "fr1", ("gfbc", 0), ("gfbc", 1)], writes=[("oo", sl)])
            P.dma("sp", I("dma_start", out=out_v[:, T, :], in_=oo[sl][:]), reads=[("oo", sl)], writes=[("OUT", T)], sem=f"out{sl}")
        return finish()


def make_inputs(inputs, S):
    x = np.asarray(inputs["x"], np.float32).reshape(S, D)
    xT = np.ascontiguousarray(x.T)
    common = dict(
        xT=xT,
        w_in=np.ascontiguousarray(inputs["w_in"][0]),
        g1=np.ascontiguousarray(np.asarray(inputs["attn_norm_g"][0]).reshape(8, 128).T),
        lamv=np.stack([inputs["da_lambda_q1"][0], inputs["da_lambda_k1"][0], inputs["da_lambda_q2"][0],
                       inputs["da_lambda_k2"][0]])[None].astype(np.float32),
        w_out=np.ascontiguousarray(inputs["w_out"][0]),
        sg=np.ascontiguousarray(np.asarray(inputs["da_subln_g"][0]).reshape(128, 1)),
        g2=np.asarray(inputs["ffn_norm_g"][0]).reshape(1, D),
        gf=np.asarray(inputs["final_norm_g"]).reshape(1, D),
        wr=np.ascontiguousarray(np.concatenate([inputs["router_group_w"][0], inputs["router_expert_w"][0]], axis=1)),
        br=np.concatenate([inputs["router_group_b"][0], inputs["router_expert_b"][0]])[None].astype(np.float32),
        wg=np.ascontiguousarray(np.asarray(inputs["expert_w_gate"][0]).reshape(NEXP, 8, 128, HID).transpose(0, 2, 1, 3)).reshape(NEXP * 128, 8 * HID),
        wu=np.ascontiguousarray(np.asarray(inputs["expert_w_up"][0]).reshape(NEXP, 8, 128, HID).transpose(0, 2, 1, 3)).reshape(NEXP * 128, 8 * HID),
        wd=np.ascontiguousarray(np.asarray(inputs["expert_w_down"][0]).reshape(NEXP, 4, 128, D).transpose(0, 2, 1, 3)).reshape(NEXP * 128, 4 * D),
    )
    maps = []
    own = []
    for c in range(NCORES):
        i = np.arange(S // 8)
        pos = 64 * (i // 8) + 8 * c + (i % 8)
        own.append(pos)
        m = dict(common)
        m["xo"] = np.ascontiguousarray(x[pos])
        m["xoT"] = np.ascontiguousarray(xT[:, pos])
        m.update(host_tables(c, S))
        maps.append({k: np.ascontiguousarray(v, dtype=np.float32) for k, v in m.items()})
    return maps, own


_NC_CACHE = {}


def kernel(**inputs):
    S = inputs["x"].shape[1]
    if S not in _NC_CACHE:
        _NC_CACHE[S] = build(S, "full")
    nc = _NC_CACHE[S]
    maps, own = make_inputs(inputs, S)
    res = run_bass_kernel_spmd(nc, maps, core_ids=list(range(NCORES)))
    outp = np.zeros((S, D), np.float32)
    for c in range(NCORES):
        outp[own[c]] = res.results[c]["out"]
    return outp.reshape(1, S, D)
```

```python
import math
from contextlib import ExitStack

import numpy as np
import concourse.bass as bass
import concourse.mybir as mybir
from concourse.bass_utils import run_bass_kernel_spmd

F32 = mybir.dt.float32
BF16 = mybir.dt.bfloat16
I32 = mybir.dt.int32
U32 = mybir.dt.uint32
ALU = mybir.AluOpType
AF = mybir.ActivationFunctionType
AX = mybir.AxisListType

NCORES = 8
D = 1024
H = 4
EPS = 1e-6
NEXP = 32
HID = 512
LAMBDA_INIT = 0.8 - 0.6 * math.exp(-0.3 * 0)
ALIBI_CUT = 134.0


class Prog:
    ENG = ("pe", "act", "dve", "pool", "sp")

    def __init__(self, nc, stack):
        self.nc = nc
        self.stack = stack
        self.sem = {e: stack.enter_context(nc.semaphore("sem_" + e)) for e in self.ENG}
        self.cnt = {e: 0 for e in self.ENG}
        self.stream = {e: [] for e in self.ENG}
        self.seen = {e: {} for e in self.ENG}
        self.dsem = {}
        self.dcnt = {}
        self.lastw = {}
        self.readers = {}
        self.bc_value = 0
        self.bcreg = None

    def _dma_sem(self, name):
        if name not in self.dsem:
            self.dsem[name] = self.stack.enter_context(self.nc.semaphore("dma_" + name))
            self.dcnt[name] = 0
        return self.dsem[name]

    def _need(self, eng, tok):
        if tok is None:
            return
        kind, name, val = tok
        if kind == "eng" and name == eng and eng == "pe":
            return
        key = (kind, name)
        if self.seen[eng].get(key, 0) >= val:
            return
        self.seen[eng][key] = val
        sem = self.sem[name] if kind == "eng" else self.dsem[name]
        self.stream[eng].append(("wait", sem, val))

    def _deps(self, eng, reads, writes):
        toks = []
        for k in reads:
            toks.append(self.lastw.get(k))
        for k in writes:
            toks.append(self.lastw.get(k))
            toks.extend(self.readers.get(k, ()))
        best = {}
        for t in toks:
            if t is None:
                continue
            key = (t[0], t[1])
            if key not in best or best[key][2] < t[2]:
                best[key] = t
        for t in best.values():
            self._need(eng, t)

    def _commit(self, tok, reads, writes):
        for k in reads:
            self.readers.setdefault(k, []).append(tok)
        for k in writes:
            self.lastw[k] = tok
            self.readers[k] = []

    def op(self, eng, fn, reads=(), writes=()):
        self._deps(eng, reads, writes)
        self.cnt[eng] += 1
        tok = ("eng", eng, self.cnt[eng])
        self.stream[eng].append(("inst", fn, self.sem[eng], 1))
        self._commit(tok, reads, writes)

    def dma(self, eng, fn, reads=(), writes=(), sem=None):
        assert eng in ("sp", "pool", "act")
        name = sem
        s = self._dma_sem(name)
        self._deps(eng, reads, writes)
        self.dcnt[name] += 16
        tok = ("dma", name, self.dcnt[name])
        self.stream[eng].append(("inst", fn, s, 16))
        self._commit(tok, reads, writes)

    def raw(self, eng, fn, reads=(), writes=()):
        self._deps(eng, reads, writes)
        self.stream[eng].append(("raw", fn))

    def barrier(self):
        toks = [("eng", e, self.cnt[e]) for e in self.ENG if self.cnt[e] > 0]
        toks += [("dma", n, c) for n, c in self.dcnt.items() if c > 0]
        for e in self.ENG:
            for t in toks:
                self._need(e, t)

    def finish_waits(self, eng="sp"):
        best = {}
        for k, tok in list(self.lastw.items()):
            if k[0] == "OUT":
                key = (tok[0], tok[1])
                if key not in best or best[key][2] < tok[2]:
                    best[key] = tok
        for tok in best.values():
            self._need(eng, tok)

    def emit(self, block):
        nc = self.nc
        engobj = {"pe": "tensor", "act": "scalar", "dve": "vector", "pool": "gpsimd", "sp": "sync"}

        def mk(ename):
            def body(e):
                if ename == "pool":
                    self.bcreg = e.to_reg(self.bc_value)
                    run(e)
                else:
                    run(e)

            def run(e):
                for it in self.stream[ename]:
                    if it[0] == "wait":
                        e.wait_ge(it[1], it[2])
                    elif it[0] == "raw":
                        it[1](e)
                    else:
                        it[1](e).then_inc(it[2], it[3])
            return body

        for ename in self.ENG:
            getattr(block, engobj[ename])(mk(ename))


def I(name, *args, **kw):
    return lambda e: getattr(e, name)(*args, **kw)


def host_tables(c, S):
    NO = S // 8
    slopes = np.exp2(-8.0 * np.arange(1, H + 1) / H)
    i = np.arange(NO)
    pos = 64 * (i // 8) + 8 * c + (i % 8)
    qrel = pos % 4096
    a = (qrel // 64) * 64
    b = qrel % 64
    qaug = np.zeros((2, H, NO), np.float32)
    for h in range(H):
        qaug[0, h] = -8.0 * slopes[h] * a
        qaug[1, h] = -8.0 * slopes[h] * b
    p = np.arange(128)
    biask = np.zeros((128, H, 128), np.float32)
    for h in range(H):
        for idx in range(128):
            biask[:, h, idx] = slopes[h] * (128 * (idx - 96) + p)
    fix = np.zeros((128, H, 16), np.float32)
    for col in range(16):
        u, v = col // 8, col % 8
        qt = 64 * u + 8 * c + v
        for h in range(H):
            for pp in range(128):
                if pp // 64 > u:
                    fix[pp, h, col] = -30000.0
                elif pp > qt:
                    fix[pp, h, col] = -2.0 * slopes[h] * (pp - qt) * 8.0
    lg = np.log1p(-np.exp2(-5.0 - np.arange(H, dtype=np.float64)))
    s = np.arange(64)
    v8 = 8 * c + np.arange(8)
    kdec = np.zeros((128, H, 64), np.float32)
    for h in range(H):
        kd = np.exp(lg[h] * (63 - s)) * 0.125
        kdec[:, h, :] = np.concatenate([kd, kd])[:, None]
    dt = np.zeros((128, 2, 2, 8), np.float32)
    qdec8 = np.zeros((128, 2, 8), np.float32)
    sdec = np.zeros((128, 2), np.float32)
    for h in range(H):
        rel = v8[None, :] - s[:, None]
        dd = np.where(rel >= 0, np.exp(lg[h] * np.maximum(rel, 0)), 0.0) * 0.125
        hh, grp = h % 2, h // 2
        dt[:, hh, grp, :] = np.concatenate([dd, dd], 0)
        qdec8[hh * 64:(hh + 1) * 64, grp, :] = np.exp(lg[h] * (v8 + 1.0))[None, :]
        sdec[hh * 64:(hh + 1) * 64, grp] = np.exp(lg[h] * 64)
    return dict(qaug=qaug, biask=biask, fix=fix, kdec=kdec.reshape(128, H * 64), dt=dt,
                qdec8=qdec8, sdec=sdec)


def build(S, stage="full"):
    NO = S // 8
    NT = NO // 128
    NB = S // 512
    NG = S // 4096
    NSB = NT + NEXP
    NBLK = 2 * NSB
    assert NG >= 1
    nc = bass.Bass("TRN2", target_bir_lowering=False)
    st = ExitStack()
    with st:
        def din(name, shape, dt=F32):
            return nc.dram_tensor(name, list(shape), dt, kind="ExternalInput")

        xT = din("xT", [D, S])
        xoT = din("xoT", [D, NO])
        xo = din("xo", [NO, D])
        w_in = din("w_in", [D, 3072])
        g1 = din("g1", [128, 8])
        qaug_d = din("qaug", [2, H, NO])
        biask_d = din("biask", [128, H, 128])
        fix_d = din("fix", [128, H, 16])
        kdec_d = din("kdec", [128, H * 64])
        dt_d = din("dt", [128, 2, 2, 8])
        qdec8_d = din("qdec8", [128, 2, 8])
        sdec_d = din("sdec", [128, 2])
        lamv = din("lamv", [1, 4, 64])
        w_out = din("w_out", [D, D])
        sg = din("sg", [128, 1])
        g2 = din("g2", [1, D])
        gf = din("gf", [1, D])
        wr = din("wr", [D, 36])
        br = din("br", [1, 36])
        wg = din("wg", [NEXP * 128, 8 * HID])
        wu = din("wu", [NEXP * 128, 8 * HID])
        wd = din("wd", [NEXP * 128, 4 * D])
        out = nc.dram_tensor("out", [NO, D], F32, kind="ExternalOutput")
        dbg = None
        if stage != "full":
            dbg = nc.dram_tensor("dbg", [NO, 2048], F32, kind="ExternalOutput")

        kT_s = nc.dram_tensor("kT_s", [H, 2, 64, S], BF16)
        v_s = nc.dram_tensor("v_s", [S, 512], BF16)
        h1_s = nc.dram_tensor("h1_s", [NO, D], F32)
        xn_s = nc.dram_tensor("xn_s", [NO, D], BF16)
        xs_s = nc.dram_tensor("xs_s", [NBLK * 128, D], BF16)
        ys_s = nc.dram_tensor("ys_s", [NBLK * 128, D], F32)

        P = Prog(nc, st)
        P.bc_value = NEXP * 128 - 1

        def sbt(stack, name, shape, dt=F32):
            return stack.enter_context(nc.sbuf_tensor(name, list(shape), dt))

        def sb(name, shape, dt=F32):
            return sbt(st, name, shape, dt)

        def finish():
            P.finish_waits("sp")
            with nc.Block() as block:
                P.emit(block)
            return nc

        ones_bf = sb("ones_bf", [128, 128], BF16)
        ones_f = sb("ones_f", [128, 128], F32)
        ident_bf = sb("ident_bf", [128, 128], BF16)
        ident_f = sb("ident_f", [128, 128], F32)
        iotf = sb("iotf", [128, 128])
        g1_sb = sb("g1_sb", [128, 8])
        biask = sb("biask_sb", [128, H, 128])
        fix = sb("fix_sb", [128, H, 16])
        kdec = sb("kdec_sb", [128, H * 64])
        dtt = sb("dt_sb", [128, 2, 2, 8])
        qdec8 = sb("qdec8_sb", [128, 2, 8])
        sdec = sb("sdec_sb", [128, 2])
        epsc = sb("epsc", [128, 1])
        gsl = sb("gsl", [128, NT, 512], BF16)
        ret = sb("ret", [128, NT, 512], BF16)
        att = sb("att", [128, NT, 8, 128], BF16)
        psum = [st.enter_context(nc.psum_tensor(f"ps{i}", [128, 512], F32)) for i in range(8)]
        PS = [("ps", i) for i in range(8)]

        P.op("pool", I("memset", ones_bf[:], 1.0), writes=["ones_bf"])
        P.op("pool", I("memset", ones_f[:], 1.0), writes=["ones_f"])
        P.op("pool", I("memset", epsc[:], EPS), writes=["epsc"])
        iot = sb("iot", [128, 128], I32)
        P.op("pool", I("iota", iot[:], pattern=[[1, 128]], base=0, channel_multiplier=-1), writes=["iot"])
        P.op("dve", I("tensor_copy", out=iotf[:], in_=iot[:]), reads=["iot"], writes=["iotf"])
        P.op("dve", I("tensor_single_scalar", out=ident_f[:], in_=iotf[:], scalar=0.0, op=ALU.is_equal),
             reads=["iotf"], writes=["ident_f"])
        P.op("dve", I("tensor_copy", out=ident_bf[:], in_=ident_f[:]), reads=["ident_f"], writes=["ident_bf"])
        for (dst, src, key) in ((g1_sb, g1, "g1"), (biask, biask_d, "biask"), (fix, fix_d, "fix"),
                                (kdec, kdec_d, "kdec"), (dtt, dt_d, "dt"), (qdec8, qdec8_d, "qdec8"),
                                (sdec, sdec_d, "sdec")):
            P.dma("sp", I("dma_start", out=dst[:], in_=src.ap()), writes=[key], sem="c_" + key)

        stM = ExitStack()
        st.enter_context(stM)
        qda = sbt(stM, "qda", [128, 8, NO], BF16)
        qr = sbt(stM, "qr", [128, 2, NO], BF16)
        for h in range(H):
            for m in range(2):
                P.dma("pool", I("dma_start", out=qda[64:66, 2 * h + m, :], in_=qaug_d.ap()[:, h, :]),
                      writes=[("qda", 2 * h + m, "aug")], sem=f"c_qa{2 * h + m}")

        w_in_v = w_in.ap().rearrange("(k p) c -> p k c", p=128)

        def load_w(stack, name, ranges):
            ncols = sum(r[1] - r[0] for r in ranges)
            W = sbt(stack, name, [128, 8, ncols], BF16)
            stg = [sbt(stack, f"{name}_st{i}", [128, 512]) for i in range(2)]
            cnt = 0
            d0 = 0
            for (a0, a1) in ranges:
                for c0 in range(a0, a1, 512):
                    c1 = min(a1, c0 + 512)
                    for kc in range(8):
                        sl = cnt % 2
                        cnt += 1
                        key = (name + "_st", sl)
                        P.dma("sp", I("dma_start", out=stg[sl][:, 0:c1 - c0], in_=w_in_v[:, kc, c0:c1]),
                              writes=[key], sem=f"{name}_st{sl}")
                        P.op("dve", I("tensor_scalar", out=W[:, kc, d0:d0 + c1 - c0], in0=stg[sl][:, 0:c1 - c0],
                                      scalar1=g1_sb[:, kc:kc + 1], scalar2=None, op0=ALU.mult),
                             reads=[key, "g1"], writes=[(name, kc)])
                    d0 += c1 - c0
            return W, [(name, kc) for kc in range(8)]

        def mk_front(stack, tag, nsq=1):
            xb = [sbt(stack, f"xb{tag}{i}", [128, 8, 512], BF16) for i in range(2)]
            sqs = [sbt(stack, f"sq{tag}{i}", [128, 8, 512], BF16) for i in range(nsq)]
            rbc = [sbt(stack, f"rbc{tag}{i}", [128, 512]) for i in range(2)]
            rtk = [sbt(stack, f"rtk{tag}{i}", [128, 4]) for i in range(2)]
            lnb = sbt(stack, f"lnb{tag}", [128, 512])

            issued = set()

            def front_load(src_ap_T, blk, par):
                if blk in issued:
                    return
                issued.add(blk)
                xbk = ("xb" + tag, par)
                xv = src_ap_T.rearrange("(k p) t -> p k t", p=128)
                for half in range(2):
                    P.dma("pool", I("dma_start", out=xb[par][:, 4 * half:4 * half + 4, :],
                                    in_=xv[:, 4 * half:4 * half + 4, blk * 512:(blk + 1) * 512]),
                          writes=[(xbk, half)], sem=f"xb{tag}{par}_{half}")

            done_sq, done_st = set(), set()

            def front_sq(src_ap_T, blk, par):
                if blk in done_sq:
                    return
                done_sq.add(blk)
                front_load(src_ap_T, blk, par)
                xbk = ("xb" + tag, par)
                sq, sqk = sqs[blk % nsq], ("sq" + tag, blk % nsq)
                for half in range(2):
                    P.op("pool" if half else "dve", I("tensor_tensor", out=sq[:, 4 * half:4 * half + 4, :],
                                                      in0=xb[par][:, 4 * half:4 * half + 4, :],
                                                      in1=xb[par][:, 4 * half:4 * half + 4, :], op=ALU.mult),
                         reads=[(xbk, half)], writes=[(sqk, half)])

            def front_stats(src_ap_T, blk, par):
                if blk in done_st:
                    return
                done_st.add(blk)
                front_sq(src_ap_T, blk, par)
                sq, sqk = sqs[blk % nsq], ("sq" + tag, blk % nsq)
                for kc in range(8):
                    P.op("pe", I("matmul", psum[7][:, :], lhsT=ones_bf[:, :], rhs=sq[:, kc, :], start=(kc == 0), stop=(kc == 7)),
                         reads=[(sqk, kc // 4), "ones_bf"], writes=[PS[7]])
                P.op("act", I("activation", out=lnb[:], in_=psum[7][:, :], func=AF.Ln, bias=epsc[:, 0:1], scale=1.0 / D),
                     reads=[PS[7], "epsc"], writes=["lnb" + tag])
                P.op("act", I("activation", out=rbc[par][:], in_=lnb[:], func=AF.Exp, scale=-0.5),
                     reads=["lnb" + tag], writes=[("rbc" + tag, par)])
                for tt in range(4):
                    for kc in range(8):
                        P.op("pe", I("matmul", psum[7][:, tt:tt + 1], lhsT=sq[:, kc, tt * 128:(tt + 1) * 128], rhs=ones_bf[:, 0:1],
                                     start=(kc == 0 and tt == 0), stop=(kc == 7), skip_group_check=True),
                             reads=[(sqk, kc // 4), "ones_bf"], writes=[PS[7]])
                P.op("act", I("activation", out=lnb[:, 0:4], in_=psum[7][:, 0:4], func=AF.Ln, bias=epsc[:, 0:1], scale=1.0 / D),
                     reads=[PS[7], "epsc"], writes=["lnb" + tag])
                P.op("act", I("activation", out=rtk[par][:], in_=lnb[:, 0:4], func=AF.Exp, scale=-0.5),
                     reads=["lnb" + tag], writes=[("rtk" + tag, par)])

            def front(src_ap_T, blk, par, nblk=None):
                front_stats(src_ap_T, blk, par)
                xbk = ("xb" + tag, par)
                return [(xbk, 0), (xbk, 1)]
            front.sq = front_sq
            front.stats = front_stats
            front.load = front_load
            return xb, rbc, rtk, front

        pa = 0

        def proj(XB, WK, lhsT_fn, rhs_fn, M, N):
            nonlocal pa
            pb, pk = psum[pa % 2], PS[pa % 2]
            pa += 1
            for kc in range(8):
                P.op("pe", I("matmul", pb[0:M, 0:N], lhsT=lhsT_fn(kc), rhs=rhs_fn(kc), start=(kc == 0), stop=(kc == 7)),
                     reads=XB + [WK[kc]], writes=[pk])
            return pb, pk

        WA, WKA = load_w(stM, "WA", [(512, 1536), (1792, 2560)])

        with ExitStack() as s0:
            W0, WK0 = load_w(s0, "W0", [(0, 512), (1536, 1792), (2560, 3072)])
            xb, rbc, rtk, front = mk_front(s0, "o")
            for blk in range(NO // 512):
                par = blk % 2
                XB = front(xoT.ap(), blk, par)
                for hm in range(8):
                    c0 = (hm // 2) * 128 + (hm % 2) * 64
                    pb, pk = proj(XB, WK0, lambda kc: W0[:, kc, c0:c0 + 64], lambda kc: xb[par][:, kc, :], 64, 512)
                    P.op("dve", I("tensor_tensor", out=qda[0:64, hm, blk * 512:(blk + 1) * 512], in0=pb[0:64, :],
                                  in1=rbc[par][0:64, :], op=ALU.mult),
                         reads=[pk, ("rbco", par)], writes=[("qda", hm, blk)])
                for grp in range(2):
                    c0 = 512 + grp * 128
                    pb, pk = proj(XB, WK0, lambda kc: W0[:, kc, c0:c0 + 128], lambda kc: xb[par][:, kc, :], 128, 512)
                    P.op("dve", I("tensor_tensor", out=qr[:, grp, blk * 512:(blk + 1) * 512], in0=pb[:, :],
                                  in1=rbc[par][:, :], op=ALU.mult),
                         reads=[pk, ("rbco", par)], writes=[("qr", blk)])
                for tt in range(4):
                    pb, pk = proj(XB, WK0, lambda kc: xb[par][:, kc, tt * 128:(tt + 1) * 128], lambda kc: W0[:, kc, 768:1280], 128, 512)
                    P.op("act", I("activation", out=gsl[:, blk * 4 + tt, :], in_=pb[:, :], func=AF.Silu,
                                  scale=rtk[par][:, tt:tt + 1]),
                         reads=[pk, ("rtko", par)], writes=[("gsl", blk * 4 + tt)])
            P.barrier()

        with ExitStack() as sA:
            xb, rbc, rtk, front = mk_front(sA, "a", nsq=2)
            kst = sbt(sA, "kst", [128, 4, 512], BF16)
            vst = sbt(sA, "vst", [128, 4, 512], BF16)
            krT = sbt(sA, "krT", [128, 2, 512], BF16)
            vr = sbt(sA, "vr", [128, 4, 512], BF16)
            krk = sbt(sA, "krk", [128, 4, 256], BF16)
            NPB = 2
            ADp = [sbt(sA, f"ADp{i}", [128, 2, 2, 248], BF16) for i in range(NPB)]
            Qxp = [sbt(sA, f"Qxp{i}", [128, 2, 248], BF16) for i in range(NPB)]
            Sst = sbt(sA, "Sst", [128, 256])
            Sbf = sbt(sA, "Sbf", [128, 256], BF16)
            rtmp = sbt(sA, "rtmp", [128, 512])
            for i in range(NPB):
                P.op("pool", I("memset", ADp[i][:], 0.0), writes=[("ADp", i)])
                P.op("pool", I("memset", Qxp[i][:], 0.0), writes=[("Qxp", i)])
            P.op("pool", I("memset", Sst[:], 0.0), writes=["Sst"])
            P.op("pool", I("memset", Sbf[:], 0.0), writes=[("Sbf", 0)])
            kT_v = kT_s.ap().rearrange("h m d s -> (m d) h s")
            v_v = v_s.ap().rearrange("(n p) c -> p n c", p=128)
            retfirst = {2: True, 3: True}
            Sbf2 = [Sbf, sbt(sA, "Sbf_b", [128, 256], BF16)]
            P.op("pool", I("memset", Sbf2[1][:], 0.0), writes=[("Sbf", 1)])
            for blk in range(NB):
                par = blk % 2
                XB = front(xT.ap(), blk, par)
                pending_prefetch = (blk + 1 < NB)

                def kv_unit(u, blk=blk, par=par, XB=XB):
                    if u < 4:
                        h = u
                        c0 = h * 128
                        pb, pk = proj(XB, WKA, lambda kc: WA[:, kc, c0:c0 + 128], lambda kc: xb[par][:, kc, :], 128, 512)
                        P.op("dve", I("tensor_tensor", out=kst[:, h, :], in0=pb[:, :], in1=rbc[par][:, :], op=ALU.mult),
                             reads=[pk, ("rbca", par)], writes=[("kst", h)])
                        if u == 3:
                            P.dma("sp", I("dma_start", out=kT_v[:, :, blk * 512:(blk + 1) * 512], in_=kst[:, :, :]),
                                  reads=[("kst", h_) for h_ in range(H)], writes=[("kT_s", blk)], sem="kst")
                    else:
                        tt = u - 4
                        pb, pk = proj(XB, WKA, lambda kc: xb[par][:, kc, tt * 128:(tt + 1) * 128], lambda kc: WA[:, kc, 512:1024], 128, 512)
                        P.op("act", I("activation", out=vst[:, tt, :], in_=pb[:, :], func=AF.Copy, scale=rtk[par][:, tt:tt + 1]),
                             reads=[pk, ("rtka", par)], writes=[("vst", tt)])
                        if u == 7:
                            P.dma("sp", I("dma_start", out=v_v[:, blk * 4:(blk + 1) * 4, :], in_=vst[:, :, :]),
                                  reads=[("vst", t_) for t_ in range(4)], writes=[("v_s", blk)], sem="vst")

                for grp in range(2):
                    c0 = 1024 + grp * 128
                    pb, pk = proj(XB, WKA, lambda kc: WA[:, kc, c0:c0 + 128], lambda kc: xb[par][:, kc, :], 128, 512)
                    P.op("dve", I("tensor_tensor", out=krT[:, grp, :], in0=pb[:, :], in1=rbc[par][:, :], op=ALU.mult),
                         reads=[pk, ("rbca", par)], writes=[("krT", grp)])
                for tt in range(4):
                    pb, pk = proj(XB, WKA, lambda kc: xb[par][:, kc, tt * 128:(tt + 1) * 128], lambda kc: WA[:, kc, 1280:1792], 128, 512)
                    P.op("act", I("activation", out=vr[:, tt, :], in_=pb[:, :], func=AF.Copy, scale=rtk[par][:, tt:tt + 1]),
                         reads=[pk, ("rtka", par)], writes=[("vr", tt)])
                for tt in range(4):
                    pbT, pkT = psum[pa % 2], PS[pa % 2]
                    pa += 1
                    pbTb = pbT.bitcast(BF16)
                    for grp in range(2):
                        P.op("pe", I("transpose", out=pbTb[:, grp * 128:(grp + 1) * 128], in_=krT[:, grp, tt * 128:(tt + 1) * 128],
                                     identity=ident_bf[:]), reads=[("krT", grp), "ident_bf"], writes=[pkT])
                    P.op("dve", I("tensor_tensor", out=krk[:, tt, :], in0=pbTb[:, 0:256], in1=kdec[:, :], op=ALU.mult),
                         reads=[pkT, "kdec"], writes=[("krk", tt)])
                for ch in range(8):
                    n = blk * 8 + ch
                    tt, cp = ch // 2, ch % 2
                    T, w = n // 16, n % 16
                    pbi = n % NPB
                    wo = 120 - 8 * w
                    rows = slice(cp * 64, cp * 64 + 64)
                    scur, snxt = n % 2, (n + 1) % 2
                    for h in range(H):
                        hr = slice((h % 2) * 64, (h % 2) * 64 + 64)
                        bk = 4 if h % 2 == 0 else 6
                        P.op("pe", I("matmul", psum[bk][rows, (h // 2) * 8:(h // 2) * 8 + 8],
                                     lhsT=krT[hr, h // 2, ch * 64:ch * 64 + 64], rhs=qr[hr, h // 2, 8 * n:8 * n + 8],
                                     start=True, stop=True, skip_group_check=True),
                             reads=[("krT", h // 2), ("qr", (8 * n) // 512)], writes=[PS[bk]])
                    for hh in range(2):
                        bk = 4 if hh == 0 else 6
                        P.op("dve", I("tensor_tensor", out=ADp[pbi][rows, hh, :, 120:128],
                                      in0=psum[bk][rows, 0:16].rearrange("p (g c) -> p g c", g=2), in1=dtt[rows, hh, :, :],
                                      op=ALU.mult),
                             reads=[PS[bk], "dt"], writes=[("ADp", pbi)])
                    P.op("pool", I("tensor_tensor", out=Qxp[pbi][:, :, 120:128], in0=qr[:, :, 8 * n:8 * n + 8],
                                   in1=qdec8[:, :, :], op=ALU.mult),
                         reads=[("qr", (8 * n) // 512), "qdec8"], writes=[("Qxp", pbi)])
                    for h in range(H):
                        hr = slice((h % 2) * 64, (h % 2) * 64 + 64)
                        P.op("pe", I("matmul", psum[5][hr, (h // 2) * 128:(h // 2) * 128 + 128],
                                     lhsT=krk[rows, tt, h * 64:h * 64 + 64], rhs=vr[rows, tt, h * 128:(h + 1) * 128],
                                     start=True, stop=True, skip_group_check=True),
                             reads=[("krk", tt), ("vr", tt)], writes=[PS[5]])
                    for grp in range(2):
                        P.op("dve", I("scalar_tensor_tensor", out=Sst[:, grp * 128:(grp + 1) * 128],
                                      in0=Sst[:, grp * 128:(grp + 1) * 128], scalar=sdec[:, grp:grp + 1],
                                      in1=psum[5][:, grp * 128:(grp + 1) * 128], op0=ALU.mult, op1=ALU.add),
                             reads=["Sst", PS[5], "sdec"], writes=["Sst"])
                    P.op("act", I("activation", out=Sbf2[snxt][:, :], in_=Sst[:, :], func=AF.Copy), reads=["Sst"], writes=[("Sbf", snxt)])
                    kv_unit(ch)
                    if pending_prefetch:
                        if ch == 0:
                            front.load(xT.ap(), blk + 1, (blk + 1) % 2)
                        elif ch == 3:
                            front.sq(xT.ap(), blk + 1, (blk + 1) % 2)
                        elif ch == 6:
                            front.stats(xT.ap(), blk + 1, (blk + 1) % 2)
                    if w == 0:
                        retfirst = {2: True, 3: True}
                    for h in range(H):
                        hr = slice((h % 2) * 64, (h % 2) * 64 + 64)
                        bk = 2 + cp
                        P.op("pe", I("matmul", psum[bk][:, h * 128:(h + 1) * 128], lhsT=ADp[pbi][rows, h % 2, h // 2, wo:wo + 128],
                                     rhs=vr[rows, tt, h * 128:(h + 1) * 128], start=retfirst[bk], stop=False, skip_group_check=True),
                             reads=[("ADp", pbi), ("vr", tt)], writes=[PS[bk]])
                        retfirst[bk] = False
                        bk = 2 + (h % 2)
                        P.op("pe", I("matmul", psum[bk][:, h * 128:(h + 1) * 128], lhsT=Qxp[pbi][hr, h // 2, wo:wo + 128],
                                     rhs=Sbf2[scur][hr, (h // 2) * 128:(h // 2) * 128 + 128], start=retfirst[bk], stop=False,
                                     skip_group_check=True),
                             reads=[("Qxp", pbi), ("Sbf", scur)], writes=[PS[bk]])
                        retfirst[bk] = False
                    if w == 15:
                        P.op("act", I("activation", out=rtmp[:, :], in_=psum[3][:, :], func=AF.Copy), reads=[PS[3]], writes=["rtmp"])
                        P.op("dve", I("tensor_tensor", out=ret[:, T, :], in0=psum[2][:, :], in1=rtmp[:, :], op=ALU.add),
                             reads=[PS[2], "rtmp"], writes=[("ret", T)])
            P.barrier()

        if stage == "ret":
            dst = sb("dbg_st", [128, 512])
            dv = dbg.ap().rearrange("(t p) c -> p t c", p=128)
            for t in range(NT):
                P.op("dve", I("tensor_copy", out=dst[:], in_=ret[:, t, :]), reads=[("ret", t)], writes=["dbg_st"])
                P.dma("sp", I("dma_start", out=dv[:, t, 0:512], in_=dst[:]), reads=["dbg_st"], writes=[("OUT", t)], sem="out")
            return finish()

        with ExitStack() as sB:
            KB = 16
            NKB = 3
            kbuf = [sbt(sB, f"kbuf{i}", [128, 2, KB * 128], BF16) for i in range(NKB)]
            vbuf = [sbt(sB, f"vbuf{i}", [128, KB, 132], BF16) for i in range(NKB)]
            eT = [sbt(sB, f"eT{i}", [128, 512], BF16) for i in range(4)]
            rz = sbt(sB, "rz", [128, 8])
            ust = [[sbt(sB, f"ust{g_}{m_}", [128, 516]) for m_ in range(2)] for g_ in range(2)]
            for i in range(NKB):
                P.op("pool", I("memset", kbuf[i][64:66, :, :], 1.0), writes=[("kbuf", i, "ones")])
                P.op("pool", I("memset", vbuf[i][:, :, 128:129], 1.0), writes=[("vbuf", i, "ones")])
            kT_l = kT_s.ap()
            v_l = v_s.ap().rearrange("(n p) c -> p n c", p=128)
            ldc = sc = ec = 0
            SKEW = 2
            ncols_total = [0]
            for h in range(H):
                for G in range(NG):
                    nkt = 32 * (G + 1)
                    steps = []
                    batches = []
                    Wh = ALIBI_CUT / (2.0 ** (-8.0 * (h + 1) / H))
                    chi = {}
                    for kt in range(nkt):
                        jj = kt - 32 * G
                        c0 = 16 * jj if jj >= 0 else 0
                        cq_max = math.floor((Wh + 128 * kt + 127 - 4096 * G) / 64.0)
                        c_hi = min(512, 8 * (cq_max + 1))
                        if c_hi <= c0:
                            continue
                        chi[kt] = min(512, ((c_hi + 127) // 128) * 128)
                    for kb0 in range(0, nkt, KB):
                        kts = [kt for kt in range(kb0, kb0 + KB) if kt in chi]
                        if not kts:
                            continue
                        lb = ldc % NKB
                        ldc += 1
                        batches.append((kb0, lb))
                        for kt in kts:
                            for m in range(2):
                                steps.append((kb0, lb, kt, m))
                    ncols_total[0] += sum(chi[st_[2]] - (16 * max(st_[2] - 32 * G, 0) // 128) * 128 for st_ in steps)
                    loaded = set()
                    started = set()

                    def kv_load(bi):
                        if bi >= len(batches) or bi in loaded:
                            return
                        loaded.add(bi)
                        kb0, lb = batches[bi]
                        P.dma("sp", I("dma_start", out=kbuf[lb][0:64, :, :],
                                      in_=kT_l[h, :, :, kb0 * 128:(kb0 + KB) * 128].rearrange("m d s -> d m s")),
                              reads=[("kT_s", b_) for b_ in range(kb0 // 4, (kb0 + KB) // 4)], writes=[("kbuf", lb)], sem=f"kbuf{lb}")
                        P.dma("sp", I("dma_start", out=vbuf[lb][:, :, 0:128], in_=v_l[:, kb0:kb0 + KB, h * 128:(h + 1) * 128]),
                              reads=[("v_s", b_) for b_ in range(kb0 // 4, (kb0 + KB) // 4)], writes=[("vbuf", lb)], sem=f"vbuf{lb}")

                    bidx = {kb0_: i_ for i_, (kb0_, _) in enumerate(batches)}
                    seenb = set()

                    def s_front(kb0, lb, kt, m, sp_, spk, eb, ek):
                        if kb0 not in seenb:
                            seenb.add(kb0)
                            kv_load(bidx[kb0])
                            kv_load(bidx[kb0] + 1)
                        ce = chi[kt]
                        jj = kt - 32 * G
                        c0 = 16 * jj if jj >= 0 else 0
                        cc0 = (c0 // 128) * 128
                        kk = kt - kb0
                        hm = 2 * h + m
                        P.op("pe", I("matmul", sp_[:, cc0:ce], lhsT=kbuf[lb][0:66, m, kk * 128:(kk + 1) * 128],
                                     rhs=qda[0:66, hm, G * 512 + cc0:G * 512 + ce], start=True, stop=True),
                             reads=[("kbuf", lb), ("kbuf", lb, "ones"), ("qda", hm, G), ("qda", hm, "aug")], writes=[spk])
                        if jj >= 0:
                            P.op("dve", I("tensor_tensor", out=sp_[:, c0:c0 + 16], in0=sp_[:, c0:c0 + 16],
                                          in1=fix[:, h, :], op=ALU.add), reads=[spk, "fix"], writes=[spk])
                            if c0 > cc0:
                                P.op("pool", I("memset", eb[:, cc0:c0], 0.0), writes=[ek])
                        P.op("act", I("activation", out=eb[:, c0:ce], in_=sp_[:, c0:ce], func=AF.Exp,
                                      bias=biask[:, h, kt - 32 * G + 96:kt - 32 * G + 97], scale=0.125),
                             reads=[spk, "biask"], writes=[ek])

                    def s_back(kb0, lb, kt, m, eb, ek):
                        jj = kt - 32 * G
                        c0 = 16 * jj if jj >= 0 else 0
                        cc0 = (c0 // 128) * 128
                        kk = kt - kb0
                        for j in range(cc0 // 128, chi[kt] // 128):
                            bank = 4 + 2 * m + (1 if j == 3 else 0)
                            col = 0 if j == 3 else j * 129
                            first = bank not in started
                            started.add(bank)
                            P.op("pe", I("matmul", psum[bank][:, col:col + 129], lhsT=eb[:, j * 128:(j + 1) * 128],
                                         rhs=vbuf[lb][:, kk, 0:129], start=first, stop=(kt == nkt - 1), skip_group_check=True),
                                 reads=[ek, ("vbuf", lb), ("vbuf", lb, "ones")], writes=[PS[bank]])

                    bufs = []
                    for i in range(len(steps) + SKEW):
                        if i < len(steps):
                            sp_, spk = psum[sc % 4], PS[sc % 4]
                            sc += 1
                            eb, ek = eT[ec % 4], ("eT", ec % 4)
                            ec += 1
                            bufs.append((eb, ek))
                            s_front(*steps[i], sp_, spk, eb, ek)
                        if i - SKEW >= 0:
                            s_back(*steps[i - SKEW], *bufs[i - SKEW])
                    gp_ = (h * NG + G) % 2
                    for m in range(2):
                        P.op("dve", I("tensor_copy", out=ust[gp_][m][:, 0:387], in_=psum[4 + 2 * m][:, 0:387]),
                             reads=[PS[4 + 2 * m]], writes=[("ust", gp_, m, 0)])
                        P.op("act", I("activation", out=ust[gp_][m][:, 387:516], in_=psum[5 + 2 * m][:, 0:129], func=AF.Copy),
                             reads=[PS[5 + 2 * m]], writes=[("ust", gp_, m, 1)])
                    for m in range(2):
                        hm = 2 * h + m
                        for j in range(4):
                            col = j * 129
                            part = 1 if j == 3 else 0
                            P.op("dve", I("reciprocal", out=rz[:, 4 * m + j:4 * m + j + 1], in_=ust[gp_][m][:, col + 128:col + 129]),
                                 reads=[("ust", gp_, m, part)], writes=[("rz", m, j)])
                            P.op("dve", I("tensor_scalar", out=att[:, 4 * G + j, hm, :], in0=ust[gp_][m][:, col:col + 128],
                                          scalar1=rz[:, 4 * m + j:4 * m + j + 1], scalar2=None, op0=ALU.mult),
                                 reads=[("ust", gp_, m, part), ("rz", m, j)], writes=[("att", 4 * G + j)])
            print('attention score columns per core:', ncols_total[0])
            P.barrier()
        stM.close()

        if stage == "attn":
            dst = sb("dbg_st", [128, 1024])
            dv = dbg.ap().rearrange("(t p) c -> p t c", p=128)
            for t in range(NT):
                P.op("dve", I("tensor_copy", out=dst[:], in_=att[:, t, :, :].rearrange("p a b -> p (a b)")),
                     reads=[("att", t)], writes=["dbg_st"])
                P.dma("sp", I("dma_start", out=dv[:, t, 0:1024], in_=dst[:]), reads=["dbg_st"], writes=[("OUT", t)], sem="out")
            return finish()

        sC = ExitStack()
        st.enter_context(sC)
        Wo = sbt(sC, "Wo", [128, 8, D], BF16)
        g2bc = sbt(sC, "g2bc", [128, D])
        gfbc = sbt(sC, "gfbc", [128, D])
        Wr = sbt(sC, "Wr", [128, 8, 36])
        brow = sbt(sC, "brow", [1, 36])
        lam_sb = sbt(sC, "lam_sb", [1, 256])
        lam_t = sbt(sC, "lam_t", [1, 136])
        nlam = sbt(sC, "nlam", [128, 1])
        sgs = sbt(sC, "sgs", [128, 1])
        grow = sbt(sC, "grow", [1, 2 * D])
        iota32 = sbt(sC, "iota32", [128, 64])
        iot2 = sbt(sC, "iot2", [128, 64], I32)
        Ustr = sbt(sC, "Ustr", [128, 128], BF16)
        OH = sbt(sC, "OH", [128, 2 * NT, 32], BF16)
        wts = sbt(sC, "wts", [128, NT, 2])
        dest_f = sbt(sC, "dest_f", [128, 2 * NT])
        dest_i = sbt(sC, "dest_i", [128, 2 * NT], I32)
        be_i = sbt(sC, "be_i", [128, NSB], I32)
        idxw = sbt(sC, "idxw", [128, NSB], I32)
        wost = [sbt(sC, f"wost{i}", [128, D]) for i in range(2)]

        zt = sbt(sC, "zt", [128, D], BF16)
        P.op("pool", I("memset", zt[:], 0.0), writes=["zt"])
        for b in range(NBLK):
            P.dma("sp", I("dma_start", out=xs_s.ap()[b * 128:(b + 1) * 128, :], in_=zt[:]), reads=["zt"], writes=[("xs_z", b)], sem="xsz")
        XSZ = [("xs_z", b) for b in range(NBLK)]
        P.dma("sp", I("dma_start", out=lam_sb[:], in_=lamv.ap().rearrange("a b c -> a (b c)")), writes=["lam_sb"], sem="c_lam")
        P.op("dve", I("tensor_tensor", out=lam_t[0:1, 0:64], in0=lam_sb[0:1, 0:64], in1=lam_sb[0:1, 64:128], op=ALU.mult),
             reads=["lam_sb"], writes=["lam_t"])
        P.op("dve", I("tensor_tensor", out=lam_t[0:1, 64:128], in0=lam_sb[0:1, 128:192], in1=lam_sb[0:1, 192:256], op=ALU.mult),
             reads=["lam_sb"], writes=["lam_t"])
        P.op("dve", I("tensor_reduce", out=lam_t[0:1, 128:130], in_=lam_t[0:1, 0:128].rearrange("p (a b) -> p a b", a=2),
                      axis=AX.X, op=ALU.add), reads=["lam_t"], writes=["lam_t"])
        P.op("act", I("activation", out=lam_t[0:1, 130:132], in_=lam_t[0:1, 128:130], func=AF.Exp), reads=["lam_t"], writes=["lam_t"])
        P.op("dve", I("tensor_tensor", out=lam_t[0:1, 132:133], in0=lam_t[0:1, 131:132], in1=lam_t[0:1, 130:131], op=ALU.subtract),
             reads=["lam_t"], writes=["lam_t"])
        P.op("dve", I("tensor_scalar", out=lam_t[0:1, 133:134], in0=lam_t[0:1, 132:133], scalar1=-LAMBDA_INIT, scalar2=None,
                      op0=ALU.add), reads=["lam_t"], writes=["lam_t"])
        P.op("pe", I("matmul", psum[0][:, 0:1], lhsT=ones_f[0:1, :], rhs=lam_t[0:1, 133:134], start=True, stop=True),
             reads=["lam_t", "ones_f"], writes=[PS[0]])
        P.op("dve", I("tensor_copy", out=nlam[:], in_=psum[0][:, 0:1]), reads=[PS[0]], writes=["nlam"])
        P.dma("sp", I("dma_start", out=sgs[:], in_=sg.ap()), writes=["sgs0"], sem="c_sg")
        P.op("dve", I("tensor_scalar", out=sgs[:], in0=sgs[:], scalar1=1.0 - LAMBDA_INIT, scalar2=None, op0=ALU.mult),
             reads=["sgs0"], writes=["sgs"])
        w_out_v = w_out.ap().rearrange("(k p) c -> p k c", p=128)
        for kc in range(8):
            sl = kc % 2
            P.dma("sp", I("dma_start", out=wost[sl][:], in_=w_out_v[:, kc, :]), writes=[("wost", sl)], sem=f"wost{sl}")
            if kc < 4:
                P.op("dve", I("tensor_scalar", out=Wo[:, kc, :], in0=wost[sl][:], scalar1=sgs[:, 0:1], scalar2=None, op0=ALU.mult),
                     reads=[("wost", sl), "sgs"], writes=[("Wo", kc)])
            else:
                P.op("dve", I("tensor_copy", out=Wo[:, kc, :], in_=wost[sl][:]), reads=[("wost", sl)], writes=[("Wo", kc)])
        WOK = [("Wo", kc) for kc in range(8)]
        P.dma("sp", I("dma_start", out=grow[0:1, 0:D], in_=g2.ap()), writes=["grow_a"], sem="c_g2")
        P.dma("sp", I("dma_start", out=grow[0:1, D:2 * D], in_=gf.ap()), writes=["grow_b"], sem="c_gf")
        for i, (dstt, key) in enumerate(((g2bc, "g2bc"), (gfbc, "gfbc"))):
            for half in range(2):
                P.op("pe", I("matmul", psum[1 + half][:, :], lhsT=ones_f[0:1, :], rhs=grow[0:1, i * D + half * 512:i * D + half * 512 + 512],
                             start=True, stop=True), reads=["grow_a", "grow_b", "ones_f"], writes=[PS[1 + half]])
                P.op("dve", I("tensor_copy", out=dstt[:, half * 512:(half + 1) * 512], in_=psum[1 + half][:, :]),
                     reads=[PS[1 + half]], writes=[(key, half)])
        P.dma("sp", I("dma_start", out=Wr[:], in_=wr.ap().rearrange("(k p) c -> p k c", p=128)), writes=["Wr"], sem="c_wr")
        P.dma("sp", I("dma_start", out=brow[:], in_=br.ap()), writes=["brow"], sem="c_br")
        P.op("pool", I("iota", iot2[:], pattern=[[1, 64]], base=0, channel_multiplier=0), writes=["iot2"])
        P.op("dve", I("tensor_copy", out=iota32[:], in_=iot2[:]), reads=["iot2"], writes=["iota32"])
        P.op("dve", I("tensor_single_scalar", out=Ustr[:], in_=iotf[:], scalar=0.0, op=ALU.is_gt), reads=["iotf"], writes=["Ustr"])

        sT = ExitStack()
        st.enter_context(sT)
        od = [sbt(sT, f"od{i_}", [128, 4, 128]) for i_ in range(2)]
        sqt = [sbt(sT, f"sqt{i_}", [128, 4, 128]) for i_ in range(2)]
        ssr = [sbt(sT, f"ssr{i_}", [128, 16]) for i_ in range(2)]
        mix = [sbt(sT, f"mix{i_}", [128, D], BF16) for i_ in range(2)]
        mixT = [sbt(sT, f"mixT{i_}", [128, 8, 128], BF16) for i_ in range(2)]
        xot = [sbt(sT, f"xot{i_}", [128, D]) for i_ in range(2)]
        h1t = [sbt(sT, f"h1t{i_}", [128, D]) for i_ in range(2)]
        junk = [sbt(sT, f"junk{i_}", [128, D], BF16) for i_ in range(2)]
        xn2f = [sbt(sT, f"xn2f{i_}", [128, D]) for i_ in range(2)]
        xn2b = [sbt(sT, f"xn2b{i_}", [128, D], BF16) for i_ in range(2)]
        xnT = [sbt(sT, f"xnT{i_}", [128, 8, 128]) for i_ in range(2)]
        lg = [sbt(sT, f"lg{i_}", [128, 36]) for i_ in range(2)]
        rt_ = [sbt(sT, f"rt_{i_}", [128, 64]) for i_ in range(2)]
        t8 = [sbt(sT, f"t8{i_}", [128, 8]) for i_ in range(2)]
        i8 = [sbt(sT, f"i8{i_}", [128, 8], U32) for i_ in range(2)]
        psbf = [psum[i].bitcast(BF16) for i in range(8)]
        xo_v = xo.ap().rearrange("(t p) c -> p t c", p=128)
        h1_v = h1_s.ap().rearrange("(t p) c -> p t c", p=128)
        xn_v = xn_s.ap().rearrange("(t p) c -> p t c", p=128)
        out_v = out.ap().rearrange("(t p) c -> p t c", p=128)

        def rstd_from(ss_ap, out_ap, n, key_in, key_out, scale):
            P.op("act", I("activation", out=out_ap, in_=ss_ap, func=AF.Ln, bias=epsc[:, 0:1], scale=scale),
                 reads=[key_in, "epsc"], writes=[key_out])
            P.op("act", I("activation", out=out_ap, in_=out_ap, func=AF.Exp, scale=-0.5), reads=[key_out], writes=[key_out])

        def C1(T):
            pz = T % 2
            attv = att[:, T, :, :].rearrange("p (h m) e -> p h m e", m=2)
            P.op("dve", I("scalar_tensor_tensor", out=od[pz][:], in0=attv[:, :, 1, :], scalar=nlam[:, 0:1], in1=attv[:, :, 0, :],
                          op0=ALU.mult, op1=ALU.add), reads=[("att", T), "nlam"], writes=[("od", pz)])
            P.op("dve", I("tensor_tensor", out=sqt[pz][:], in0=od[pz][:], in1=od[pz][:], op=ALU.mult), reads=[("od", pz)], writes=[("sqt", pz)])
            P.op("dve", I("tensor_reduce", out=ssr[pz][:, 0:4], in_=sqt[pz][:], axis=AX.X, op=ALU.add), reads=[("sqt", pz)], writes=[("ssr", pz)])
            retv = ret[:, T, :].rearrange("p (h e) -> p h e", h=H)
            P.op("dve", I("tensor_tensor", out=sqt[pz][:], in0=retv, in1=retv, op=ALU.mult), reads=[("ret", T), ("ssr", pz)], writes=[("sqt", pz)])
            P.op("dve", I("tensor_reduce", out=ssr[pz][:, 4:8], in_=sqt[pz][:], axis=AX.X, op=ALU.add), reads=[("sqt", pz)], writes=[("ssr", pz)])
            rstd_from(ssr[pz][:, 0:8], ssr[pz][:, 8:16], 8, ("ssr", pz), ("ssr2", pz), 1.0 / 128)
            for h in range(H):
                P.op("dve", I("tensor_scalar", out=mix[pz][:, h * 128:(h + 1) * 128], in0=od[pz][:, h, :], scalar1=ssr[pz][:, 8 + h:9 + h],
                              scalar2=None, op0=ALU.mult), reads=[("od", pz), ("ssr2", pz)], writes=[("mix", pz, h)])
                P.op("dve", I("scalar_tensor_tensor", out=mix[pz][:, 512 + h * 128:512 + (h + 1) * 128], in0=retv[:, h, :],
                              scalar=ssr[pz][:, 12 + h:13 + h], in1=gsl[:, T, h * 128:(h + 1) * 128], op0=ALU.mult, op1=ALU.mult),
                     reads=[("ret", T), ("ssr2", pz), ("gsl", T)], writes=[("mix", pz, 4 + h)])

        def C2(T):
            pz = T % 2
            for kc in range(8):
                P.op("pe", I("transpose", out=psbf[pz][:, kc * 128:(kc + 1) * 128], in_=mix[pz][:, kc * 128:(kc + 1) * 128],
                             identity=ident_bf[:]), reads=[("mix", pz, kc), "ident_bf"], writes=[PS[pz]])
            P.op("act", I("activation", out=mixT[pz][:].rearrange("p a b -> p (a b)"), in_=psbf[pz][:, :], func=AF.Copy),
                 reads=[PS[pz]], writes=[("mixT", pz)])
            P.dma("sp", I("dma_start", out=xot[pz][:], in_=xo_v[:, T, :]), writes=[("xot", pz)], sem=f"xot{pz}")

        def C3(T):
            pz = T % 2
            for half in range(2):
                for kc in range(8):
                    P.op("pe", I("matmul", psum[2 + half][:, :], lhsT=mixT[pz][:, kc, :], rhs=Wo[:, kc, half * 512:(half + 1) * 512],
                                 start=(kc == 0), stop=(kc == 7)), reads=[("mixT", pz), WOK[kc]], writes=[PS[2 + half]])
                P.op("dve", I("tensor_tensor", out=h1t[pz][:, half * 512:(half + 1) * 512], in0=psum[2 + half][:, :],
                              in1=xot[pz][:, half * 512:(half + 1) * 512], op=ALU.add),
                     reads=[PS[2 + half], ("xot", pz)], writes=[("h1t", pz, half)])
            P.dma("sp", I("dma_start", out=h1_v[:, T, :], in_=h1t[pz][:]), reads=[("h1t", pz, 0), ("h1t", pz, 1)], writes=[("h1_s", T)], sem=f"h1st{pz}")
            P.op("act", I("activation", out=junk[pz][:], in_=h1t[pz][:], func=AF.Square, accum_out=rt_[pz][:, 0:1]),
                 reads=[("h1t", pz, 0), ("h1t", pz, 1)], writes=[("junk", pz), ("rt0", pz)])
            rstd_from(rt_[pz][:, 0:1], rt_[pz][:, 1:2], 1, ("rt0", pz), ("rt1", pz), 1.0 / D)
            P.op("dve", I("scalar_tensor_tensor", out=xn2f[pz][:], in0=h1t[pz][:], scalar=rt_[pz][:, 1:2], in1=g2bc[:], op0=ALU.mult, op1=ALU.mult),
                 reads=[("h1t", pz, 0), ("h1t", pz, 1), ("rt1", pz), ("g2bc", 0), ("g2bc", 1)], writes=[("xn2f", pz)])
            P.op("act", I("activation", out=xn2b[pz][:], in_=xn2f[pz][:], func=AF.Copy), reads=[("xn2f", pz)], writes=[("xn2b", pz)])
            P.dma("sp", I("dma_start", out=xn_v[:, T, :], in_=xn2b[pz][:]), reads=[("xn2b", pz)], writes=[("xn_s", T)], sem=f"xnst{pz}")

        def C4(T):
            pz = T % 2
            for kc in range(8):
                bk = 4 + kc // 4
                P.op("pe", I("transpose", out=psum[bk][:, (kc % 4) * 128:(kc % 4 + 1) * 128], in_=xn2f[pz][:, kc * 128:(kc + 1) * 128],
                             identity=ident_f[:]), reads=[("xn2f", pz), "ident_f"], writes=[PS[bk]])
            for half in range(2):
                P.op("act" if half else "dve",
                     I("activation", out=xnT[pz][:, 4 * half:4 * half + 4, :].rearrange("p a b -> p (a b)"), in_=psum[4 + half][:, :], func=AF.Copy)
                     if half else I("tensor_copy", out=xnT[pz][:, 0:4, :].rearrange("p a b -> p (a b)"), in_=psum[4][:, :]),
                     reads=[PS[4 + half]], writes=[("xnT", pz, half)])
            for kc in range(8):
                P.op("pe", I("matmul", psum[6][:, 0:36], lhsT=xnT[pz][:, kc, :], rhs=Wr[:, kc, :], start=(kc == 0), stop=False),
                     reads=[("xnT", pz, kc // 4), "Wr"], writes=[PS[6]])
            P.op("pe", I("matmul", psum[6][:, 0:36], lhsT=ones_f[0:1, :], rhs=brow[0:1, :], start=False, stop=True),
                 reads=["ones_f", "brow"], writes=[PS[6]])
            P.op("dve", I("tensor_copy", out=lg[pz][:], in_=psum[6][:, 0:36]), reads=[PS[6]], writes=[("lg", pz)])

        def C5(T):
            pz = T % 2
            R = ("rt", pz)
            P.op("dve", I("tensor_reduce", out=rt_[pz][:, 2:3], in_=lg[pz][:, 0:4], axis=AX.X, op=ALU.max), reads=[("lg", pz)], writes=[R])
            P.op("dve", I("tensor_scalar", out=rt_[pz][:, 3:4], in0=rt_[pz][:, 2:3], scalar1=-1.0, scalar2=None, op0=ALU.mult), reads=[R], writes=[R])
            P.op("act", I("activation", out=rt_[pz][:, 4:8], in_=lg[pz][:, 0:4], func=AF.Exp, bias=rt_[pz][:, 3:4], scale=1.0, accum_out=rt_[pz][:, 8:9]),
                 reads=[R, ("lg", pz)], writes=[R])
            P.op("dve", I("reciprocal", out=rt_[pz][:, 9:10], in_=rt_[pz][:, 8:9]), reads=[R], writes=[R])
            P.op("dve", I("tensor_scalar", out=rt_[pz][:, 10:14], in0=lg[pz][:, 0:4], scalar1=rt_[pz][:, 2:3], scalar2=None, op0=ALU.is_equal),
                 reads=[R, ("lg", pz)], writes=[R])
            lev = lg[pz][:, 4:36].rearrange("p (g e) -> p g e", g=4)
            P.op("dve", I("tensor_scalar", out=rt_[pz][:, 16:24], in0=lev[:, 0, :], scalar1=rt_[pz][:, 10:11], scalar2=None, op0=ALU.mult),
                 reads=[R, ("lg", pz)], writes=[R])
            for g in range(1, 4):
                P.op("dve", I("scalar_tensor_tensor", out=rt_[pz][:, 16:24], in0=lev[:, g, :], scalar=rt_[pz][:, 10 + g:11 + g],
                              in1=rt_[pz][:, 16:24], op0=ALU.mult, op1=ALU.add), reads=[R, ("lg", pz)], writes=[R])
            P.op("dve", I("tensor_tensor", out=rt_[pz][:, 24:28], in0=rt_[pz][:, 10:14], in1=iota32[:, 0:4], op=ALU.mult),
                 reads=[R, "iota32"], writes=[R])
            P.op("dve", I("tensor_reduce", out=rt_[pz][:, 28:29], in_=rt_[pz][:, 24:28], axis=AX.X, op=ALU.add), reads=[R], writes=[R])
            P.op("dve", I("max", out=t8[pz][:], in_=rt_[pz][:, 16:24]), reads=[R], writes=[("t8", pz)])
            P.op("dve", I("max_index", out=i8[pz][:], in_max=t8[pz][:], in_values=rt_[pz][:, 16:24]), reads=[R, ("t8", pz)], writes=[("i8", pz)])
            P.op("dve", I("tensor_copy", out=rt_[pz][:, 30:32], in_=i8[pz][:, 0:2]), reads=[("i8", pz)], writes=[R])
            for k in range(2):
                P.op("dve", I("scalar_tensor_tensor", out=rt_[pz][:, 32 + k:33 + k], in0=rt_[pz][:, 28:29], scalar=8.0, in1=rt_[pz][:, 30 + k:31 + k],
                              op0=ALU.mult, op1=ALU.add), reads=[R], writes=[R])
                P.op("dve", I("tensor_scalar", out=OH[:, k * NT + T, :], in0=iota32[:, 0:32], scalar1=rt_[pz][:, 32 + k:33 + k], scalar2=None,
                              op0=ALU.is_equal), reads=[R, "iota32"], writes=[("OH", k * NT + T)])
            P.op("dve", I("tensor_tensor", out=rt_[pz][:, 34:35], in0=t8[pz][:, 1:2], in1=t8[pz][:, 0:1], op=ALU.subtract), reads=[("t8", pz), R], writes=[R])
            P.op("act", I("activation", out=rt_[pz][:, 35:36], in_=rt_[pz][:, 34:35], func=AF.Exp), reads=[R], writes=[R])
            P.op("dve", I("tensor_scalar", out=rt_[pz][:, 36:37], in0=rt_[pz][:, 35:36], scalar1=1.0, scalar2=None, op0=ALU.add), reads=[R], writes=[R])
            P.op("dve", I("reciprocal", out=rt_[pz][:, 37:38], in_=rt_[pz][:, 36:37]), reads=[R], writes=[R])
            P.op("dve", I("tensor_tensor", out=wts[:, T, 0:1], in0=rt_[pz][:, 37:38], in1=rt_[pz][:, 9:10], op=ALU.mult), reads=[R], writes=[("wts", T)])
            P.op("dve", I("tensor_tensor", out=wts[:, T, 1:2], in0=wts[:, T, 0:1], in1=rt_[pz][:, 35:36], op=ALU.mult),
                 reads=[R, ("wts", T)], writes=[("wts", T)])

        for i in range(NT + 4):
            if i < NT:
                C1(i)
            if 0 <= i - 1 < NT:
                C2(i - 1)
            if 0 <= i - 2 < NT:
                C3(i - 2)
            if 0 <= i - 3 < NT:
                C4(i - 3)
            if 0 <= i - 4 < NT:
                C5(i - 4)

        if stage == "h1":
            dst = sb("dbg_st", [128, 1024])
            dv = dbg.ap().rearrange("(t p) c -> p t c", p=128)
            for t in range(NT):
                P.dma("sp", I("dma_start", out=dst[:], in_=h1_v[:, t, :]), reads=[("h1_s", t)], writes=["dbg_st"], sem="dbgl")
                P.dma("sp", I("dma_start", out=dv[:, t, 0:1024], in_=dst[:]), reads=["dbg_st"], writes=[("OUT", t)], sem="out")
            dst2 = sb("dbg_st2", [128, 2 * NT, 32])
            P.op("dve", I("tensor_copy", out=dst2[:], in_=OH[:]), reads=[("OH", t) for t in range(2 * NT)], writes=["dbg_st2"])
            P.dma("sp", I("dma_start", out=dv[:, 0, 1024:1024 + 64 * NT].rearrange("p (a b) -> p a b", b=32), in_=dst2[:]),
                  reads=["dbg_st2"], writes=[("OUT", "oh")], sem="out")
            P.dma("sp", I("dma_start", out=dv[:, 1, 1024:1024 + 2 * NT].rearrange("p (a b) -> p a b", b=2), in_=wts[:]),
                  reads=[("wts", t) for t in range(NT)], writes=[("OUT", "w")], sem="out")
            return finish()
        P.barrier()
        sT.close()

        sD = ExitStack()
        st.enter_context(sD)
        NTT = 2 * NT
        HB = NTT // 2
        assert HB * 32 <= 512
        cnt_f = sbt(sD, "cnt_f", [128, 32])
        cnt_i = sbt(sD, "cnt_i", [128, 32], I32)
        pad_f = sbt(sD, "pad_f", [128, 32])
        pend = sbt(sD, "pend", [128, 32])
        pstart = sbt(sD, "pstart", [128, 32])
        dtmp = sbt(sD, "dtmp", [128, HB, 32])
        cmpb = sbt(sD, "cmpb", [128, NSB, 32])
        thr = sbt(sD, "thr", [128, NSB])
        be_f = sbt(sD, "be_f", [128, NSB])
        idxf = sbt(sD, "idxf", [128, NSB])
        OHK = [("OH", t) for t in range(NTT)]
        for t in range(NTT):
            bk = t // HB
            cs = slice((t % HB) * 32, (t % HB) * 32 + 32)
            for t2 in range(t):
                P.op("pe", I("matmul", psum[bk][:, cs], lhsT=ones_bf[:, :], rhs=OH[:, t2, :], start=(t2 == 0), stop=False,
                             skip_group_check=True), reads=[OHK[t2], "ones_bf"], writes=[PS[bk]])
            P.op("pe", I("matmul", psum[bk][:, cs], lhsT=Ustr[:, :], rhs=OH[:, t, :], start=(t == 0), stop=True, skip_group_check=True),
                 reads=[OHK[t], "Ustr"], writes=[PS[bk]])
        for t in range(NTT):
            P.op("pe", I("matmul", psum[2][:, 0:32], lhsT=ones_bf[:, :], rhs=OH[:, t, :], start=(t == 0), stop=(t == NTT - 1)),
                 reads=[OHK[t], "ones_bf"], writes=[PS[2]])
        P.op("dve", I("tensor_scalar", out=cnt_i[:], in0=psum[2][:, 0:32], scalar1=255.0, scalar2=None, op0=ALU.add),
             reads=[PS[2]], writes=["cnt_i"])
        P.op("dve", I("tensor_scalar", out=cnt_i[:], in0=cnt_i[:], scalar1=8, scalar2=8, op0=ALU.arith_shift_right,
                      op1=ALU.logical_shift_left), reads=["cnt_i"], writes=["cnt_i2"])
        P.op("dve", I("tensor_copy", out=pad_f[:], in_=cnt_i[:]), reads=["cnt_i2"], writes=["pad_f"])
        P.op("dve", I("tensor_tensor_scan", out=pend[:], data0=ones_f[:, 0:32], data1=pad_f[:], initial=0.0, op0=ALU.mult, op1=ALU.add),
             reads=["pad_f", "ones_f"], writes=["pend"])
        P.op("dve", I("tensor_tensor", out=pstart[:], in0=pend[:], in1=pad_f[:], op=ALU.subtract), reads=["pend", "pad_f"], writes=["pstart"])
        for bk in range(2):
            P.op("dve", I("tensor_tensor", out=dtmp[:], in0=psum[bk][:, 0:HB * 32].rearrange("p (a b) -> p a b", b=32),
                          in1=pstart[:, 0:32].unsqueeze(1).to_broadcast([128, HB, 32]), op=ALU.add),
                 reads=[PS[bk], "pstart"], writes=["dtmp"])
            P.op("dve", I("tensor_tensor", out=dtmp[:], in0=dtmp[:], in1=OH[:, bk * HB:(bk + 1) * HB, :], op=ALU.mult),
                 reads=["dtmp"] + OHK, writes=["dtmp"])
            P.op("dve", I("tensor_reduce", out=dest_f[:, bk * HB:(bk + 1) * HB], in_=dtmp[:], axis=AX.X, op=ALU.add),
                 reads=["dtmp"], writes=[("dest_f", bk)])
        P.op("dve", I("tensor_copy", out=dest_i[:], in_=dest_f[:]), reads=[("dest_f", 0), ("dest_f", 1)], writes=["dest_i"])
        P.op("pool", I("iota", iot2[:, 0:NSB], pattern=[[256, NSB]], base=0, channel_multiplier=0), writes=["iot2"])
        P.op("dve", I("tensor_copy", out=thr[:], in_=iot2[:, 0:NSB]), reads=["iot2"], writes=["thr"])
        P.op("dve", I("tensor_tensor", out=cmpb[:], in0=pend[:, 0:32].unsqueeze(1).to_broadcast([128, NSB, 32]),
                      in1=thr[:, 0:NSB].unsqueeze(2).to_broadcast([128, NSB, 32]), op=ALU.is_le),
             reads=["pend", "thr"], writes=["cmpb"])
        P.op("dve", I("tensor_reduce", out=be_f[:], in_=cmpb[:], axis=AX.X, op=ALU.add), reads=["cmpb"], writes=["be_f"])
        P.op("pool", I("iota", iot2[:, 0:1], pattern=[[0, 1]], base=0, channel_multiplier=1), reads=["thr"], writes=["iot2"])
        P.op("dve", I("tensor_copy", out=thr[:, 0:1], in_=iot2[:, 0:1]), reads=["iot2", "cmpb"], writes=["thr"])
        P.op("dve", I("scalar_tensor_tensor", out=idxf[:],
                      in0=be_f[:], scalar=128.0, in1=thr[:, 0:1].to_broadcast([128, NSB]), op0=ALU.mult, op1=ALU.add),
             reads=["be_f", "thr"], writes=["idxf"])
        P.op("dve", I("tensor_copy", out=idxw[:], in_=idxf[:]), reads=["idxf"], writes=["idxw"])
        P.op("dve", I("tensor_scalar", out=be_f[:], in0=be_f[:], scalar1=float(NEXP - 1), scalar2=None, op0=ALU.min),
             reads=["be_f"], writes=["be_f2"])
        P.op("dve", I("tensor_copy", out=be_i[:], in_=be_f[:]), reads=["be_f2"], writes=["be_i"])

        if stage == "disp":
            dst = sb("dbg_st", [128, 256])
            dv = dbg.ap().rearrange("(t p) c -> p t c", p=128)
            P.op("dve", I("tensor_copy", out=dst[:, 0:NTT], in_=dest_f[:]), reads=[("dest_f", 0), ("dest_f", 1)], writes=["dbg_st"])
            P.op("dve", I("tensor_copy", out=dst[:, 64:64 + NSB], in_=be_f[:]), reads=["be_f2"], writes=["dbg_st"])
            P.op("dve", I("tensor_copy", out=dst[:, 160:192], in_=pend[:]), reads=["pend"], writes=["dbg_st"])
            P.dma("sp", I("dma_start", out=dv[:, 0, 0:256], in_=dst[:]), reads=["dbg_st"], writes=[("OUT", 0)], sem="out")
            return finish()

        xs_rows = xs_s.ap()
        sE = ExitStack()
        st.enter_context(sE)
        xsc = [sbt(sE, f"xsc{i}", [128, D], BF16) for i in range(2)]
        for T in range(NT):
            sl = T % 2
            P.dma("sp", I("dma_start", out=xsc[sl][:], in_=xn_v[:, T, :]), reads=[("xn_s", T)], writes=[("xsc", sl)], sem=f"xsc{sl}")
            for k in range(2):
                t = k * NT + T
                P.dma("pool", I("indirect_dma_start", out=xs_rows[:, :],
                                out_offset=bass.IndirectOffsetOnAxis(ap=dest_i[:, t:t + 1], axis=0),
                                in_=xsc[sl][:, :], in_offset=None),
                      reads=[("xsc", sl), "dest_i"] + XSZ, writes=[("xs_sc", t)], sem=f"scat{sl}{k}")

        wgb = [sbt(sE, f"wgb{i}", [128, 8, HID], BF16) for i in range(2)]
        wub = [sbt(sE, f"wub{i}", [128, 8, HID], BF16) for i in range(2)]
        wdb = [sbt(sE, f"wdb{i}", [128, 4, D], BF16) for i in range(2)]
        xsb = [sbt(sE, f"xsb{i}", [128, D], BF16) for i in range(2)]
        xsT = [sbt(sE, f"xsT{i}", [128, 8, 128], BF16) for i in range(2)]
        sgt = [sbt(sE, f"sgt{i}", [128, HID]) for i in range(2)]
        hb = [sbt(sE, f"hb{i}", [128, HID], BF16) for i in range(2)]
        hT = [sbt(sE, f"hT{i}", [128, 4, 128], BF16) for i in range(2)]
        ysb = [sbt(sE, f"ysb{i}", [128, D]) for i in range(2)]

        def wload(sb_, which):
            if sb_ >= NSB:
                return
            sl = sb_ % 2
            for (dst_t, src_t, nm) in which:
                def gth(e, dst_ap=dst_t[sl][:, :, :].rearrange("p k n -> p (k n)"), src_ap=src_t.ap()[:, :], ix=idxw[:, sb_:sb_ + 1]):
                    return e.indirect_dma_start(out=dst_ap, out_offset=None, in_=src_ap,
                                                in_offset=bass.IndirectOffsetOnAxis(ap=ix, axis=0),
                                                bounds_check=P.bcreg, oob_is_err=False)
                P.dma("pool", gth, reads=["idxw"], writes=[(nm, sl)], sem=f"{nm}{sl}")

        WGU = ((wgb, wg, "wgb"), (wub, wu, "wub"))
        WD = ((wdb, wd, "wdb"),)

        def xload(b):
            sl = b % 2
            P.dma("sp", I("dma_start", out=xsb[sl][:], in_=xs_rows[b * 128:(b + 1) * 128, :]),
                  reads=[("xs_sc", t_) for t_ in range(2 * NT)], writes=[("xsb", sl)], sem=f"xsb{sl}")

        def S1(b):
            sl = b % 2
            for kc in range(8):
                P.op("pe", I("transpose", out=psbf[sl][:, kc * 128:(kc + 1) * 128], in_=xsb[sl][:, kc * 128:(kc + 1) * 128],
                             identity=ident_bf[:]), reads=[("xsb", sl), "ident_bf"], writes=[PS[sl]])
            P.op("act", I("activation", out=xsT[sl][:].rearrange("p a b -> p (a b)"), in_=psbf[sl][:, :], func=AF.Copy),
                 reads=[PS[sl]], writes=[("xsT", sl)])

        def S2(b):
            sl = b % 2
            for kc in range(8):
                P.op("pe", I("matmul", psum[2][:, :], lhsT=xsT[sl][:, kc, :], rhs=wgb[(b // 2) % 2][:, kc, :], start=(kc == 0), stop=(kc == 7)),
                     reads=[("xsT", sl), ("wgb", (b // 2) % 2)], writes=[PS[2]])
            for kc in range(8):
                P.op("pe", I("matmul", psum[3][:, :], lhsT=xsT[sl][:, kc, :], rhs=wub[(b // 2) % 2][:, kc, :], start=(kc == 0), stop=(kc == 7)),
                     reads=[("xsT", sl), ("wub", (b // 2) % 2)], writes=[PS[3]])
            P.op("act", I("activation", out=sgt[sl][:], in_=psum[2][:, :], func=AF.Silu), reads=[PS[2]], writes=[("sgt", sl)])
            P.op("dve", I("tensor_tensor", out=hb[sl][:], in0=psum[3][:, :], in1=sgt[sl][:], op=ALU.mult),
                 reads=[PS[3], ("sgt", sl)], writes=[("hb", sl)])

        def S3(b):
            sl = b % 2
            for hc in range(4):
                P.op("pe", I("transpose", out=psbf[6][:, hc * 128:(hc + 1) * 128], in_=hb[sl][:, hc * 128:(hc + 1) * 128],
                             identity=ident_bf[:]), reads=[("hb", sl), "ident_bf"], writes=[PS[6]])
            P.op("dve", I("tensor_copy", out=hT[sl][:].rearrange("p a b -> p (a b)"), in_=psbf[6][:, 0:512]), reads=[PS[6]],
                 writes=[("hT", sl)])

        def S4(b):
            sl = b % 2
            for half in range(2):
                for hc in range(4):
                    P.op("pe", I("matmul", psum[4 + half][:, :], lhsT=hT[sl][:, hc, :], rhs=wdb[(b // 2) % 2][:, hc, half * 512:(half + 1) * 512],
                                 start=(hc == 0), stop=(hc == 3)), reads=[("hT", sl), ("wdb", (b // 2) % 2)], writes=[PS[4 + half]])
                if half == 0:
                    P.op("act", I("activation", out=ysb[sl][:, 0:512], in_=psum[4][:, :], func=AF.Copy), reads=[PS[4]], writes=[("ysb", sl, 0)])
                else:
                    P.op("dve", I("tensor_copy", out=ysb[sl][:, 512:1024], in_=psum[5][:, :]), reads=[PS[5]], writes=[("ysb", sl, 1)])
            P.dma("sp", I("dma_start", out=ys_s.ap()[b * 128:(b + 1) * 128, :], in_=ysb[sl][:]),
                  reads=[("ysb", sl, 0), ("ysb", sl, 1)], writes=[("ys_s", b)], sem=f"ysst{sl}")

        for b in range(min(2, NBLK)):
            xload(b)
        for sb_ in range(2):
            wload(sb_, WGU)
            wload(sb_, WD)
        for i in range(NBLK + 3):
            if i < NBLK:
                S1(i)
                if i + 2 < NBLK:
                    xload(i + 2)
            if 0 <= i - 1 < NBLK:
                S2(i - 1)
                if (i - 1) % 2 == 1:
                    wload((i - 1) // 2 + 2, WGU)
            if 0 <= i - 2 < NBLK:
                S3(i - 2)
            if 0 <= i - 3 < NBLK:
                S4(i - 3)
                if (i - 3) % 2 == 1:
                    wload((i - 3) // 2 + 2, WD)

        P.barrier()
        sE.close()
        y0 = [sbt(sD, f"y0_{i}", [128, D]) for i in range(2)]
        y1 = [sbt(sD, f"y1_{i}", [128, D]) for i in range(2)]
        hh_ = [sbt(sD, f"hh_{i}", [128, D]) for i in range(2)]
        oo = [sbt(sD, f"oo{i}", [128, D]) for i in range(2)]
        junk2 = sbt(sD, "junk2", [128, D], BF16)
        fr = sbt(sD, "fr", [128, 4])
        for T in range(NT):
            sl = T % 2
            P.dma("sp", I("dma_start", out=hh_[sl][:], in_=h1_v[:, T, :]), reads=[("h1_s", T)], writes=[("hh", sl)], sem=f"hh{sl}")
            for k, yb in ((0, y0), (1, y1)):
                t = k * NT + T
                P.dma("pool", I("indirect_dma_start", out=yb[sl][:, :], out_offset=None, in_=ys_s.ap()[:, :],
                                in_offset=bass.IndirectOffsetOnAxis(ap=dest_i[:, t:t + 1], axis=0)),
                      reads=[("ys_s", b_) for b_ in range(NBLK)] + ["dest_i"], writes=[("y", k, sl)], sem=f"yg{k}{sl}")
            P.op("dve", I("scalar_tensor_tensor", out=oo[sl][:], in0=y0[sl][:], scalar=wts[:, T, 0:1], in1=hh_[sl][:], op0=ALU.mult, op1=ALU.add),
                 reads=[("y", 0, sl), ("hh", sl), ("wts", T)], writes=[("oo", sl)])
            P.op("dve", I("scalar_tensor_tensor", out=oo[sl][:], in0=y1[sl][:], scalar=wts[:, T, 1:2], in1=oo[sl][:], op0=ALU.mult, op1=ALU.add),
                 reads=[("y", 1, sl), ("oo", sl), ("wts", T)], writes=[("oo", sl)])
            P.op("act", I("activation", out=junk2[:], in_=oo[sl][:], func=AF.Square, accum_out=fr[:, 0:1]), reads=[("oo", sl)],
                 writes=["junk2", "fr0"])
            rstd_from(fr[:, 0:1], fr[:, 1:2], 1, "fr0", "fr1", 1.0 / D)
            P.op("dve", I("scalar_tensor_tensor", out=oo[sl][:], in0=oo[sl][:], scalar=fr[:, 1:2], in1=gfbc[:], op0=ALU.mult, op1=ALU.mult),
                 reads=[("oo", sl), "fr1", ("gfbc", 0), ("gfbc", 1)], writes=[("oo", sl)])
            P.dma("sp", I("dma_start", out=out_v[:, T, :], in_=oo[sl][:]), reads=[("oo", sl)], writes=[("OUT", T)], sem=f"out{sl}")
        return finish()


def make_inputs(inputs, S):
    x = np.asarray(inputs["x"], np.float32).reshape(S, D)
    xT = np.ascontiguousarray(x.T)
    common = dict(
        xT=xT,
        w_in=np.ascontiguousarray(inputs["w_in"][0]),
        g1=np.ascontiguousarray(np.asarray(inputs["attn_norm_g"][0]).reshape(8, 128).T),
        lamv=np.stack([inputs["da_lambda_q1"][0], inputs["da_lambda_k1"][0], inputs["da_lambda_q2"][0],
                       inputs["da_lambda_k2"][0]])[None].astype(np.float32),
        w_out=np.ascontiguousarray(inputs["w_out"][0]),
        sg=np.ascontiguousarray(np.asarray(inputs["da_subln_g"][0]).reshape(128, 1)),
        g2=np.asarray(inputs["ffn_norm_g"][0]).reshape(1, D),
        gf=np.asarray(inputs["final_norm_g"]).reshape(1, D),
        wr=np.ascontiguousarray(np.concatenate([inputs["router_group_w"][0], inputs["router_expert_w"][0]], axis=1)),
        br=np.concatenate([inputs["router_group_b"][0], inputs["router_expert_b"][0]])[None].astype(np.float32),
        wg=np.ascontiguousarray(np.asarray(inputs["expert_w_gate"][0]).reshape(NEXP, 8, 128, HID).transpose(0, 2, 1, 3)).reshape(NEXP * 128, 8 * HID),
        wu=np.ascontiguousarray(np.asarray(inputs["expert_w_up"][0]).reshape(NEXP, 8, 128, HID).transpose(0, 2, 1, 3)).reshape(NEXP * 128, 8 * HID),
        wd=np.ascontiguousarray(np.asarray(inputs["expert_w_down"][0]).reshape(NEXP, 4, 128, D).transpose(0, 2, 1, 3)).reshape(NEXP * 128, 4 * D),
    )
    maps = []
    own = []
    for c in range(NCORES):
        i = np.arange(S // 8)
        pos = 64 * (i // 8) + 8 * c + (i % 8)
        own.append(pos)
        m = dict(common)
        m["xo"] = np.ascontiguousarray(x[pos])
        m["xoT"] = np.ascontiguousarray(xT[:, pos])
        m.update(host_tables(c, S))
        maps.append({k: np.ascontiguousarray(v, dtype=np.float32) for k, v in m.items()})
    return maps, own


_NC_CACHE = {}


def kernel(**inputs):
    S = inputs["x"].shape[1]
    if S not in _NC_CACHE:
        _NC_CACHE[S] = build(S, "full")
    nc = _NC_CACHE[S]
    maps, own = make_inputs(inputs, S)
    res = run_bass_kernel_spmd(nc, maps, core_ids=list(range(NCORES)))
    outp = np.zeros((S, D), np.float32)
    for c in range(NCORES):
        outp[own[c]] = res.results[c]["out"]
    return outp.reshape(1, S, D)
```
